# Optimizing a Trainium2 kernel written in Bass

```python
import jax, jax.numpy as jnp
from jax import lax
import numpy as np

D_MODEL = 1024
BATCH = 8
SEQ = 4096
DEPTH = 2

D_MIX = D_MODEL
ATTN_WIDTH = D_MIX // 2
REC_WIDTH = D_MIX - ATTN_WIDTH
HEAD_DIM = 64
N_HEADS = ATTN_WIDTH // HEAD_DIM
ROT_DIM = HEAD_DIM // 4
ROPE_THETA = 500000.0
MOBA_BLOCK = 256
MOBA_TOPK = 3
Q_CHUNK = 32
REC_BLOCKS = 8
REC_BLOCK = REC_WIDTH // REC_BLOCKS
CONV_WIDTH = 4
LRU_C = 8.0
N_GROUPS = 4
EXPERTS_PER_GROUP = 4
N_EXPERTS = N_GROUPS * EXPERTS_PER_GROUP
MOE_TOPK = 2
D_EXPERT = 512
IN_PROJ_WIDTH = 3 * ATTN_WIDTH + 2 * REC_WIDTH
DEEPNORM_ALPHA = (2 * DEPTH) ** 0.25
DEEPNORM_BETA = (8 * DEPTH) ** -0.25
LN_EPS = 1e-5
RMS_EPS = 1e-6

kernel_name = 'hymba_moba_rglru_hmoe_deepnorm'

F32 = jnp.float32


def _layer_norm(x, g, b):
    xf = x.astype(F32)
    mu = jnp.mean(xf, axis=-1, keepdims=True)
    var = jnp.mean(jnp.square(xf - mu), axis=-1, keepdims=True)
    return ((xf - mu) * lax.rsqrt(var + LN_EPS) * g.astype(F32) + b.astype(F32)).astype(x.dtype)


def _rms_norm(x, g):
    xf = x.astype(F32)
    return (xf * lax.rsqrt(jnp.mean(xf * xf, axis=-1, keepdims=True) + RMS_EPS) * g.astype(F32)).astype(x.dtype)


def _partial_rope(t, pos):
    half = ROT_DIM // 2
    inv_freq = ROPE_THETA ** (-jnp.arange(half, dtype=F32) * 2.0 / ROT_DIM)
    ang = pos.astype(F32)[:, None] * inv_freq[None, :]
    cos = jnp.cos(ang).astype(t.dtype)
    sin = jnp.sin(ang).astype(t.dtype)
    t1 = t[..., :half]
    t2 = t[..., half:ROT_DIM]
    rest = t[..., ROT_DIM:]
    return jnp.concatenate([t1 * cos - t2 * sin, t2 * cos + t1 * sin, rest], axis=-1)


def _moba_attention(q, k, v):
    B, H, S, Dh = q.shape
    nb = S // MOBA_BLOCK
    k_sel = min(MOBA_TOPK, nb)
    kb = k.reshape(B, H, nb, MOBA_BLOCK, Dh)
    vb = v.reshape(B, H, nb, MOBA_BLOCK, Dh)
    kmean = jnp.mean(kb.astype(F32), axis=3).astype(k.dtype)
    scale = HEAD_DIM ** -0.5
    b_ix = jnp.arange(B)[:, None, None, None]
    h_ix = jnp.arange(H)[None, :, None, None]
    blk_ids = jnp.arange(nb)
    rank = jnp.arange(k_sel)

    def chunk(c):
        q0 = c * Q_CHUNK
        bi = q0 // MOBA_BLOCK
        qc = lax.dynamic_slice_in_dim(q, q0, Q_CHUNK, axis=2)
        qpos = q0 + jnp.arange(Q_CHUNK)
        k_own = lax.dynamic_slice_in_dim(k, bi * MOBA_BLOCK, MOBA_BLOCK, axis=2)
        v_own = lax.dynamic_slice_in_dim(v, bi * MOBA_BLOCK, MOBA_BLOCK, axis=2)
        kpos = bi * MOBA_BLOCK + jnp.arange(MOBA_BLOCK)
        s_own = jnp.einsum('bhqd,bhkd->bhqk', qc, k_own).astype(F32) * scale
        s_own = jnp.where(kpos[None, :] <= qpos[:, None], s_own, -jnp.inf)
        gate = jnp.einsum('bhqd,bhnd->bhqn', qc, kmean).astype(F32)
        gate = jnp.where(blk_ids < bi, gate, -jnp.inf)
        _, sel = lax.top_k(gate, k_sel)
        k_g = kb[b_ix, h_ix, sel]
        v_g = vb[b_ix, h_ix, sel]
        s_past = jnp.einsum('bhqd,bhqjkd->bhqjk', qc, k_g).astype(F32) * scale
        s_past = jnp.where((rank < bi)[:, None], s_past, -jnp.inf)
        s_past = s_past.reshape(B, H, Q_CHUNK, k_sel * MOBA_BLOCK)
        p = jax.nn.softmax(jnp.concatenate([s_past, s_own], axis=-1), axis=-1).astype(v.dtype)
        p_past = p[..., :k_sel * MOBA_BLOCK].reshape(B, H, Q_CHUNK, k_sel, MOBA_BLOCK)
        p_own = p[..., k_sel * MOBA_BLOCK:]
        return (jnp.einsum('bhqjk,bhqjkd->bhqd', p_past, v_g)
                + jnp.einsum('bhqk,bhkd->bhqd', p_own, v_own))

    out = lax.map(chunk, jnp.arange(S // Q_CHUNK))
    return jnp.moveaxis(out, 0, 2).reshape(B, H, S, Dh)


def _rglru_branch(xr, gr, conv_w, conv_b, w_a, b_a, w_i, b_i, lam):
    B, S, R = xr.shape
    xc = lax.conv_general_dilated(
        xr, conv_w[:, None, :], window_strides=(1,), padding=[(CONV_WIDTH - 1, 0)],
        dimension_numbers=('NWC', 'WIO', 'NWC'), feature_group_count=R) + conv_b
    xb = xc.reshape(B, S, REC_BLOCKS, REC_BLOCK)
    r = jax.nn.sigmoid(jnp.einsum('bsnc,ncd->bsnd', xb, w_a).reshape(B, S, R) + b_a)
    i = jax.nn.sigmoid(jnp.einsum('bsnc,ncd->bsnd', xb, w_i).reshape(B, S, R) + b_i)
    log_a = (-LRU_C * r.astype(F32)) * jax.nn.softplus(-lam.astype(F32))
    a = jnp.exp(log_a)
    u = jnp.sqrt(-jnp.expm1(2.0 * log_a)) * (i * xc).astype(F32)

    def combine(e1, e2):
        a1, b1 = e1
        a2, b2 = e2
        return a1 * a2, a2 * b1 + b2

    _, h = lax.associative_scan(combine, (a, u), axis=1)
    return h.astype(xr.dtype) * jax.nn.gelu(gr)


def _hybrid_mixer(x, w_in, conv_w, conv_b, w_rg_a, b_rg_a, w_rg_i, b_rg_i, lru_lambda,
                  g_attn_norm, g_rec_norm, w_out):
    B, S, _ = x.shape
    proj = x @ w_in
    q, k, v, xr, gr = jnp.split(
        proj, [ATTN_WIDTH, 2 * ATTN_WIDTH, 3 * ATTN_WIDTH, 3 * ATTN_WIDTH + REC_WIDTH], axis=-1)
    s_pad = -(-S // MOBA_BLOCK) * MOBA_BLOCK

    def heads(t):
        t = t.reshape(B, S, N_HEADS, HEAD_DIM).transpose(0, 2, 1, 3)
        return jnp.pad(t, ((0, 0), (0, 0), (0, s_pad - S), (0, 0)))

    pos = jnp.arange(s_pad)
    qh = _partial_rope(heads(q), pos)
    kh = _partial_rope(heads(k), pos)
    vh = heads(v)
    attn = _moba_attention(qh, kh, vh)[:, :, :S].transpose(0, 2, 1, 3).reshape(B, S, ATTN_WIDTH)
    rec = _rglru_branch(xr, gr, conv_w, conv_b, w_rg_a, b_rg_a, w_rg_i, b_rg_i, lru_lambda)
    mixed = jnp.concatenate([_rms_norm(attn, g_attn_norm), _rms_norm(rec, g_rec_norm)], axis=-1)
    return mixed @ w_out


def _hier_moe(x, w_router_group, b_router_group, w_router_expert, b_router_expert,
              w_gate, w_up, w_down):
    B, S, D = x.shape
    xt = x.reshape(-1, D)
    T = xt.shape[0]
    g_prob = jax.nn.softmax((xt @ w_router_group).astype(F32) + b_router_group.astype(F32), axis=-1)
    g_p, g_idx = lax.top_k(g_prob, 1)
    e_logits = ((xt @ w_router_expert).astype(F32) + b_router_expert.astype(F32)).reshape(
        T, N_GROUPS, EXPERTS_PER_GROUP)
    e_logits = e_logits[jnp.arange(T), g_idx[:, 0]]
    e_p, e_idx = lax.top_k(jax.nn.softmax(e_logits, axis=-1), MOE_TOPK)
    wts = g_p * e_p / jnp.sum(e_p, axis=-1, keepdims=True)
    eid = g_idx * EXPERTS_PER_GROUP + e_idx
    comb = jnp.sum(jax.nn.one_hot(eid, N_EXPERTS, dtype=F32) * wts[..., None], axis=1).astype(x.dtype)
    y = jnp.zeros_like(xt)
    for e in range(N_EXPERTS):
        h = jax.nn.silu(xt @ w_gate[e]) * (xt @ w_up[e])
        y = y + comb[:, e:e + 1] * (h @ w_down[e])
    return y.reshape(B, S, D)


def setup_inputs(seed: int = 0) -> dict:
    key = jax.random.key(seed)
    ks = jax.random.split(key, 24)
    nrm = lambda k, shape, s: jax.random.normal(k, shape, F32) * s
    a_c = jax.random.uniform(ks[8], (DEPTH, REC_WIDTH), F32, minval=0.9, maxval=0.999)
    s_lam = a_c ** (1.0 / LRU_C)
    return {
        'x': nrm(ks[0], (BATCH, SEQ, D_MODEL), 1.0),
        'w_in': nrm(ks[1], (DEPTH, D_MODEL, IN_PROJ_WIDTH), D_MODEL ** -0.5),
        'conv_w': nrm(ks[2], (DEPTH, CONV_WIDTH, REC_WIDTH), CONV_WIDTH ** -0.5),
        'conv_b': nrm(ks[3], (DEPTH, REC_WIDTH), 0.01),
        'w_rg_a': nrm(ks[4], (DEPTH, REC_BLOCKS, REC_BLOCK, REC_BLOCK), REC_BLOCK ** -0.5),
        'b_rg_a': nrm(ks[5], (DEPTH, REC_WIDTH), 0.01),
        'w_rg_i': nrm(ks[6], (DEPTH, REC_BLOCKS, REC_BLOCK, REC_BLOCK), REC_BLOCK ** -0.5),
        'b_rg_i': nrm(ks[7], (DEPTH, REC_WIDTH), 0.01),
        'lru_lambda': jnp.log(s_lam) - jnp.log1p(-s_lam),
        'g_attn_norm': 1.0 + nrm(ks[9], (DEPTH, ATTN_WIDTH), 0.02),
        'g_rec_norm': 1.0 + nrm(ks[10], (DEPTH, REC_WIDTH), 0.02),
        'w_out': nrm(ks[11], (DEPTH, D_MIX, D_MODEL), D_MIX ** -0.5 * DEEPNORM_BETA),
        'ln1_g': 1.0 + nrm(ks[12], (DEPTH, D_MODEL), 0.02),
        'ln1_b': nrm(ks[13], (DEPTH, D_MODEL), 0.01),
        'w_router_group': nrm(ks[14], (DEPTH, D_MODEL, N_GROUPS), D_MODEL ** -0.5),
        'b_router_group': nrm(ks[15], (DEPTH, N_GROUPS), 0.01),
        'w_router_expert': nrm(ks[16], (DEPTH, D_MODEL, N_EXPERTS), D_MODEL ** -0.5),
        'b_router_expert': nrm(ks[17], (DEPTH, N_EXPERTS), 0.01),
        'w_gate': nrm(ks[18], (DEPTH, N_EXPERTS, D_MODEL, D_EXPERT), D_MODEL ** -0.5),
        'w_up': nrm(ks[19], (DEPTH, N_EXPERTS, D_MODEL, D_EXPERT), D_MODEL ** -0.5),
        'w_down': nrm(ks[20], (DEPTH, N_EXPERTS, D_EXPERT, D_MODEL), D_EXPERT ** -0.5 * DEEPNORM_BETA),
        'ln2_g': 1.0 + nrm(ks[21], (DEPTH, D_MODEL), 0.02),
        'ln2_b': nrm(ks[22], (DEPTH, D_MODEL), 0.01),
    }


def reference(x, w_in, conv_w, conv_b, w_rg_a, b_rg_a, w_rg_i, b_rg_i, lru_lambda,
              g_attn_norm, g_rec_norm, w_out, ln1_g, ln1_b, w_router_group, b_router_group,
              w_router_expert, b_router_expert, w_gate, w_up, w_down, ln2_g, ln2_b):
    for l in range(DEPTH):
        h = _hybrid_mixer(x, w_in[l], conv_w[l], conv_b[l], w_rg_a[l], b_rg_a[l], w_rg_i[l],
                          b_rg_i[l], lru_lambda[l], g_attn_norm[l], g_rec_norm[l], w_out[l])
        x = _layer_norm(DEEPNORM_ALPHA * x + h, ln1_g[l], ln1_b[l])
        h = _hier_moe(x, w_router_group[l], b_router_group[l], w_router_expert[l],
                      b_router_expert[l], w_gate[l], w_up[l], w_down[l])
        x = _layer_norm(DEEPNORM_ALPHA * x + h, ln2_g[l], ln2_b[l])
    return x
```

```python
import numpy as np
from contextlib import ExitStack
import concourse.bass as bass
import concourse.mybir as mybir
from concourse.bass_utils import run_bass_kernel_spmd

F32 = mybir.dt.float32
BF16 = mybir.dt.bfloat16
I32 = mybir.dt.int32
AF = mybir.ActivationFunctionType
ALU = mybir.AluOpType
AX = mybir.AxisListType

D = 1024
NH = 8
HD = 64
NEXP = 16
DE = 512
ALPHA = float((2 * 2) ** 0.25)
LN_EPS = 1e-5
RMS_EPS = 1e-6
NEG = -60000.0
COMPUTE = ('pe', 'act', 'dve', 'pool', 'sp')
NDMASEM = 24


class Buf:
    __slots__ = ('name', 'lw', 'rd_eng', 'rd_dma')

    def __init__(self, name=''):
        self.name = name
        self.lw = None
        self.rd_eng = {}
        self.rd_dma = []


class Op:
    __slots__ = ('idx', 'eng', 'fn', 'deps', 'is_dma', 'dq', 'dsem', 'dtarget', 'sig', 'sigval')

    def __init__(self):
        self.sig = False
        self.sigval = 0
        self.is_dma = False
        self.dq = 0


class Prog:
    def __init__(self, nc):
        self.nc = nc
        self.ops = []
        self.dma_count = {'sp': 0, 'act': 0, 'pool': 0}
        self.last = {}
        self.pending_dma = []

    def op(self, eng, fn, r=(), w=(), dma=False):
        ops = self.ops
        o = Op()
        o.idx = len(ops)
        o.eng = eng
        o.fn = fn
        o.is_dma = dma
        deps = set()
        raw_src = set()
        for b in r:
            if b.lw is not None:
                deps.add(b.lw)
                raw_src.add(b.lw)
        for b in w:
            if b.lw is not None:
                deps.add(b.lw)
            deps.update(b.rd_eng.values())
            deps.update(b.rd_dma)
        best = {}
        final = []
        for d in deps:
            p = ops[d]
            if p.is_dma:
                final.append(d)
                continue
            if p.eng == eng and not dma:
                if eng == 'pe':
                    continue
            if p.eng not in best or best[p.eng] < d:
                best[p.eng] = d
        final.extend(best.values())
        o.deps = final
        for d in final:
            ops[d].sig = True
        if dma:
            o.dq = self.dma_count[eng]
            self.dma_count[eng] += 1
            o.sig = True
            self.pending_dma.append(o.idx)
        else:
            self.last[eng] = o.idx
        for b in r:
            if dma:
                b.rd_dma.append(o.idx)
            else:
                b.rd_eng[eng] = o.idx
        for b in w:
            b.lw = o.idx
            b.rd_eng = {}
            b.rd_dma = []
        ops.append(o)
        return o

    def pe(self, fn, r=(), w=()):
        return self.op('pe', fn, r, w)

    def act(self, fn, r=(), w=()):
        return self.op('act', fn, r, w)

    def dve(self, fn, r=(), w=()):
        return self.op('dve', fn, r, w)

    def pool(self, fn, r=(), w=()):
        return self.op('pool', fn, r, w)

    def dma(self, q, fn, r=(), w=()):
        return self.op(q, fn, r, w, dma=True)

    def barrier(self):
        ops = self.ops
        last = dict(self.last)
        pend = list(self.pending_dma)
        for e in ('pe', 'act', 'dve', 'pool', 'sp'):
            o = Op()
            o.idx = len(ops)
            o.eng = e
            o.fn = lambda eng: eng.nop()
            o.deps = [v for k, v in last.items() if k != e] + pend
            for d in o.deps:
                ops[d].sig = True
            ops.append(o)
            if e != 'sp':
                self.last[e] = o.idx
        self.pending_dma = []

    def emit(self, stack):
        nc = self.nc
        ops = self.ops
        sems = {}
        for e in COMPUTE:
            sems[e] = stack.enter_context(nc.semaphore('s_' + e))
        dsems = {}
        for q in ('sp', 'act', 'pool'):
            if self.dma_count[q]:
                dsems[q] = [stack.enter_context(nc.semaphore('d_%s%d' % (q, i)))
                            for i in range(min(NDMASEM, self.dma_count[q]))]
        cnt = {e: 0 for e in COMPUTE}
        for o in ops:
            if o.is_dma:
                n = len(dsems[o.eng])
                o.dsem = dsems[o.eng][o.dq % n]
                o.dtarget = 16 * (o.dq // n + 1)
            elif o.sig:
                cnt[o.eng] += 1
                o.sigval = cnt[o.eng]
        self.sig_counts = dict(cnt)
        streams = {e: [] for e in ('pe', 'act', 'dve', 'pool', 'sp')}
        for o in ops:
            streams[o.eng].append(o)
        block = stack.enter_context(nc.Block())

        def run(engname, eng):
            waited = {}

            def wait(sem, val):
                k = sem.num
                if waited.get(k, 0) >= val:
                    return
                waited[k] = val
                eng.wait_ge(sem, val)

            for o in streams[engname]:
                for d in o.deps:
                    p = ops[d]
                    if p.is_dma:
                        wait(p.dsem, p.dtarget)
                    else:
                        wait(sems[p.eng], p.sigval)
                if o.is_dma:
                    if o.dtarget > 16:
                        wait(o.dsem, o.dtarget - 16)
                    o.fn(eng).then_inc(o.dsem, 16)
                else:
                    ins = o.fn(eng)
                    if o.sig:
                        ins.then_inc(sems[o.eng], 1)

        @block.sync
        def _(e):
            run('sp', e)

        @block.scalar
        def _(e):
            run('act', e)

        @block.vector
        def _(e):
            run('dve', e)

        @block.gpsimd
        def _(e):
            run('pool', e)

        @block.tensor
        def _(e):
            run('pe', e)


class Arena:
    def __init__(self, ap, total):
        self.ap = ap
        self.total = total
        self.top = 0
        self.peak = 0

    def alloc(self, free_shape, dtype, parts=128):
        n = int(np.prod(free_shape))
        four = dtype in (F32, I32)
        nf = n if four else (n + 1) // 2
        assert self.top + nf <= self.total, ('SBUF arena overflow', self.top, nf, self.total)
        v = self.ap[0:parts, self.top:self.top + nf]
        self.top += nf
        self.peak = max(self.peak, self.top)
        if dtype != F32:
            v = v.bitcast(dtype)
            if (not four) and n % 2:
                v = v[:, 0:n]
        if len(free_shape) == 2:
            v = v.rearrange("p (a b) -> p a b", b=free_shape[1])
        elif len(free_shape) == 3:
            v = v.rearrange("p (a b c) -> p a b c", b=free_shape[1], c=free_shape[2])
        elif len(free_shape) == 4:
            v = v.rearrange("p (a b c d) -> p a b c d", b=free_shape[1], c=free_shape[2], d=free_shape[3])
        return v

    def mark(self):
        return self.top

    def release(self, m):
        self.top = m


ARENA_F32 = 53000
DBG_LAYER = 0
SKIP1 = ''
NLAYERS_RUN = None


def build(S, depth, dbg=False):
    NT = S // 128
    NG = S // 512
    NB = S // 256
    nc = bass.Bass("TRN2", target_bir_lowering=False)

    def din(name, shape, dt=F32):
        return nc.dram_tensor(name, list(shape), dt, kind="ExternalInput").ap()

    def dscr(name, shape, dt):
        return nc.dram_tensor(name, list(shape), dt).ap()

    x_d = din("x", [S, D])
    w_in_d = din("w_in", [depth, D, 2560])
    convw_d = din("conv_w", [depth, 128, 4, 4])
    convb_d = din("conv_b", [depth, 128, 4])
    wa_d = din("w_rg_a", [depth, 4, 128, 128])
    ba_d = din("b_rg_a", [depth, 128, 4])
    wi_d = din("w_rg_i", [depth, 4, 128, 128])
    bi_d = din("b_rg_i", [depth, 128, 4])
    lam_d = din("lru_lambda", [depth, 128, 4])
    gattn_d = din("g_attn_norm", [depth, 128, 512])
    grec_d = din("g_rec_norm", [depth, 128, 4])
    w_out_d = din("w_out", [depth, D, D])
    ln1g_d = din("ln1_g", [depth, 128, D])
    ln1b_d = din("ln1_b", [depth, 128, D])
    wr_d = din("w_router", [depth, D, 20])
    br_d = din("b_router", [depth, 128, 20])
    wg_d = din("w_gate", [depth, NEXP, D, DE])
    wu_d = din("w_up", [depth, NEXP, D, DE])
    wd_d = din("w_down", [depth, NEXP, DE, D])
    ln2g_d = din("ln2_g", [depth, 128, D])
    ln2b_d = din("ln2_b", [depth, 128, D])
    ident_d = din("c_ident", [128, 128])
    cos_d = din("c_cos", [128, NT, 8])
    sin_d = din("c_sin", [128, NT, 8])
    gcaus_d = din("c_gcaus", [128, NT, 16])
    gown_d = din("c_gown", [128, NT, 16])
    tri_d = din("c_tri", [128, 128])
    khot_d = din("c_khot", [16, S])
    shift_d = din("c_shift", [128, 128])
    ltri_d = din("c_ltri", [128, 128])
    wbase_d = din("c_wbase", [128, 8])
    jstart_d = din("c_jstart", [128, 32, 16])
    out_d = nc.dram_tensor("out", [S, D], F32, kind="ExternalOutput").ap()
    dbg_d = None
    if dbg:
        dbg_d = nc.dram_tensor("dbg_x1", [S, D], F32, kind="ExternalOutput").ap()
        dbg2_d = nc.dram_tensor("dbg_x2", [S, D], F32, kind="ExternalOutput").ap()

    xres_d = dscr("xres", [S, D], F32)
    qTa_d = dscr("qTa", [NH, 80, S], BF16)
    kT_d = dscr("kT", [NH, 64, S], BF16)
    recT_d = dscr("recT", [4, 128, S], BF16)
    NSLOT = 2 * S + 16 * 512
    NTILE = NSLOT // 512
    assert NTILE <= 32
    xs_d = dscr("xs", [NSLOT, D], BF16)
    wgb_d = [dscr("wgb%d" % i, [NEXP, 128, 8 * DE], BF16) for i in range(depth)]
    wub_d = [dscr("wub%d" % i, [NEXP, 128, 8 * DE], BF16) for i in range(depth)]
    wdb_d = [dscr("wdb%d" % i, [NEXP, 128, 4 * D], BF16) for i in range(depth)]
    ys_d = dscr("ys", [NSLOT, D], F32)

    P = Prog(nc)
    with ExitStack() as st:
        arena_t = st.enter_context(nc.sbuf_tensor("arena", [128, ARENA_F32], F32))
        ar = Arena(arena_t, ARENA_F32)
        ps_all = st.enter_context(nc.psum_tensor("psum_all", [128, 8 * 512], F32))
        banks = [ps_all[:, i * 512:(i + 1) * 512] for i in range(8)]
        bb = [Buf('bank%d' % i) for i in range(8)]

        def bank_bf(i):
            return banks[i][:, :].bitcast(BF16)

        identf = ar.alloc([128], F32)
        idb = ar.alloc([128], BF16)
        trif = ar.alloc([128], F32)
        trib = ar.alloc([128], BF16)
        ones_f = ar.alloc([128], F32)
        b_identf, b_idb, b_trif, b_trib, b_ones = Buf(), Buf(), Buf(), Buf(), Buf()
        P.dma('sp', lambda e: e.dma_start(out=identf, in_=ident_d[:, :]), w=[b_identf])
        P.dma('sp', lambda e: e.dma_start(out=trif, in_=tri_d[:, :]), w=[b_trif])
        P.dve(lambda e: e.tensor_copy(out=idb, in_=identf), r=[b_identf], w=[b_idb])
        P.dve(lambda e: e.tensor_copy(out=trib, in_=trif), r=[b_trif], w=[b_trib])
        P.dve(lambda e: e.memset(ones_f, 1.0), w=[b_ones])
        shiftf = ar.alloc([128], F32)
        shiftb = ar.alloc([128], BF16)
        b_shiftf, b_shift = Buf(), Buf()
        P.dma('sp', lambda e: e.dma_start(out=shiftf, in_=shift_d[:, :]), w=[b_shiftf])
        P.dve(lambda e: e.tensor_copy(out=shiftb, in_=shiftf), r=[b_shiftf], w=[b_shift])
        comb_sb = ar.alloc([NT, 16], F32)
        b_comb = [Buf() for _ in range(NT)]
        ltrif = ar.alloc([128], F32)
        ltrib = ar.alloc([128], BF16)
        onesb = ar.alloc([128], BF16)
        b_ltrif, b_ltri, b_onesb = Buf(), Buf(), Buf()
        P.dma('sp', lambda e: e.dma_start(out=ltrif, in_=ltri_d[:, :]), w=[b_ltrif])
        P.dve(lambda e: e.tensor_copy(out=ltrib, in_=ltrif), r=[b_ltrif], w=[b_ltri])
        P.dve(lambda e: e.memset(onesb, 1.0), w=[b_onesb])
        lg_sb = ar.alloc([NT, 20], F32)
        b_lg = [Buf() for _ in range(NT)]
        sel_sb = ar.alloc([NT, 16], BF16)
        b_sel = [Buf() for _ in range(NT)]
        rank_sb = ar.alloc([NT, 16], F32)
        b_rank = [Buf() for _ in range(NT)]
        carry_sb = ar.alloc([16], F32)
        b_carry = Buf()
        idx_i = ar.alloc([NT, 2], I32)
        wts_sb = ar.alloc([NT, 2], F32)
        b_idx = Buf()
        ej_i = ar.alloc([32], I32)
        b_ej = Buf()
        wbase_sb = ar.alloc([8], F32)
        b_wbase = Buf()
        P.dma('sp', lambda e: e.dma_start(out=wbase_sb, in_=wbase_d[:, :]), w=[b_wbase])
        idxw = ar.alloc([32], I32)
        ejc = ar.alloc([32], F32)
        ejs = ar.alloc([32], F32)
        base_mark = ar.mark()
        vsb = ar.alloc([NT, NH, 65], BF16)
        b_v = [Buf('v%d' % t) for t in range(NT)]
        v_mark = ar.mark()
        attn_sb = ar.alloc([NT, 512], BF16)
        b_attn = [[Buf() for _ in range(NH)] for _ in range(NT)]
        mid_mark = ar.mark()
        b_out_done = Buf('out_done')
        b_xres = [Buf() for _ in range(NT)]
        b_qTa = [[Buf() for _ in range(NG)] for _ in range(NH)]
        b_kT = [[Buf() for _ in range(NG)] for _ in range(NH)]
        b_recT = [Buf() for _ in range(NG)]
        b_xs_w, b_xs, b_ys_w, b_ys = Buf(), Buf(), Buf(), Buf()
        b_wc_w, b_wc = Buf(), Buf()
        b_xs_z = [Buf() for _ in range(NSLOT // 1024)]

        def do_layer(l):
            xsrc = x_d if l == 0 else xres_d
            xdst2 = out_d if l == depth - 1 else xres_d

            if l >= 1 and '1' in SKIP1:
                return
            ar.release(v_mark)
            w_in_sb = ar.alloc([8, 2560], BF16)
            b_win = [Buf(), Buf()]
            for hh in range(2):
                P.dma('pool', lambda e, hh=hh: e.dma_start(
                    out=w_in_sb[:, :, hh * 1280:(hh + 1) * 1280],
                    in_=w_in_d[l].rearrange("(k p) n -> p k n", p=128)[:, :, hh * 1280:(hh + 1) * 1280]),
                    w=[b_win[hh]])
            cos_sb = ar.alloc([NT, 8], F32)
            sin_sb = ar.alloc([NT, 8], F32)
            gcaus_sb = ar.alloc([NT, 16], F32)
            gown_sb = ar.alloc([NT, 16], F32)
            b_tab = Buf()
            P.dma('sp', lambda e: e.dma_start(out=cos_sb, in_=cos_d[:, :, :]), w=[b_tab])
            b_tab2, b_tab3, b_tab4 = Buf(), Buf(), Buf()
            P.dma('sp', lambda e: e.dma_start(out=sin_sb, in_=sin_d[:, :, :]), w=[b_tab2])
            P.dma('sp', lambda e: e.dma_start(out=gcaus_sb, in_=gcaus_d[:, :, :]), w=[b_tab3])
            P.dma('sp', lambda e: e.dma_start(out=gown_sb, in_=gown_d[:, :, :]), w=[b_tab4])
            convw_sb = ar.alloc([4, 4], F32)
            convb_sb = ar.alloc([4], F32)
            ba_sb = ar.alloc([4], F32)
            bi_sb = ar.alloc([4], F32)
            lam_sb = ar.alloc([4], F32)
            cdec_sb = ar.alloc([4], F32)
            grec_sb = ar.alloc([4], F32)
            wa_sb = ar.alloc([4, 128], BF16)
            wi_sb = ar.alloc([4, 128], BF16)
            b_par = Buf()
            b_wa, b_wi, b_waf, b_wif, b_cdec, b_lam = Buf(), Buf(), Buf(), Buf(), Buf(), Buf()
            b_p = [Buf() for _ in range(6)]
            P.dma('sp', lambda e: e.dma_start(out=convw_sb, in_=convw_d[l]), w=[b_p[0]])
            P.dma('sp', lambda e: e.dma_start(out=convb_sb, in_=convb_d[l]), w=[b_p[1]])
            P.dma('sp', lambda e: e.dma_start(out=ba_sb, in_=ba_d[l]), w=[b_p[2]])
            P.dma('sp', lambda e: e.dma_start(out=bi_sb, in_=bi_d[l]), w=[b_p[3]])
            P.dma('sp', lambda e: e.dma_start(out=lam_sb, in_=lam_d[l]), w=[b_lam])
            P.dma('sp', lambda e: e.dma_start(out=grec_sb, in_=grec_d[l]), w=[b_p[5]])
            P.dma('pool', lambda e: e.dma_start(out=wa_sb, in_=wa_d[l].rearrange("c p n -> p c n")), w=[b_wa])
            P.dma('pool', lambda e: e.dma_start(out=wi_sb, in_=wi_d[l].rearrange("c p n -> p c n")), w=[b_wi])
            P.act(lambda e: e.activation(out=cdec_sb, in_=lam_sb, func=AF.Exp, scale=-1.0), r=[b_lam], w=[b_cdec])
            P.act(lambda e: e.activation(out=cdec_sb, in_=cdec_sb, func=AF.Ln, bias=1.0), r=[b_cdec], w=[b_cdec])
            P.dve(lambda e: e.tensor_scalar(out=cdec_sb, in0=cdec_sb, scalar1=-8.0, scalar2=None, op0=ALU.mult),
                  r=[b_cdec], w=[b_cdec])
            b_par_all = b_p + [b_cdec]

            kmeanBD = ar.alloc([4, 2, 16], BF16)
            b_kmean = Buf()
            P.dve(lambda e: e.memset(kmeanBD, 0.0), w=[b_kmean])

            xbf = ar.alloc([4, D], BF16)
            b_xbf = [Buf() for _ in range(4)]
            oddk = ar.alloc([8], BF16, parts=64)
            b_oddk = Buf()
            xT2 = [ar.alloc([8, 512], BF16) for _ in range(2)]
            b_xT2 = [[Buf() for _ in range(4)] for _ in range(2)]
            qb = ar.alloc([4, NH, 80], BF16)
            b_qb = [Buf() for _ in range(4)]
            kb = ar.alloc([4, NH, 64], BF16)
            b_kb = [Buf() for _ in range(4)]
            qTp = ar.alloc([4, 512], BF16)
            b_qTp = [Buf() for _ in range(4)]
            kTh = ar.alloc([NH, 512], BF16, parts=64)
            b_kTh = [Buf() for _ in range(4)]
            qTa = ar.alloc([NH, 512], BF16, parts=80)
            b_qTah = [Buf() for _ in range(4)]
            ksum = ar.alloc([NH, 2], F32, parts=64)
            b_ksum = Buf()
            rt = [ar.alloc([NH, 8], F32) for _ in range(4)]
            b_rt = [Buf() for _ in range(4)]
            gm = ar.alloc([4, NH], F32)
            b_gm = Buf()
            xr_sb = ar.alloc([4, 515], F32)
            b_xr = [Buf() for _ in range(4)]
            P.dve(lambda e: e.memset(xr_sb, 0.0), w=b_xr)
            hst = ar.alloc([4, 1], F32)
            b_h = [Buf() for _ in range(4)]
            XC = [ar.alloc([512], F32) for _ in range(3)]
            b_XC = [Buf() for _ in range(3)]
            RRb = [ar.alloc([512], F32) for _ in range(2)]
            b_RR = [Buf() for _ in range(2)]
            IIb = [ar.alloc([512], F32) for _ in range(2)]
            b_II = [Buf() for _ in range(2)]
            AAb = [ar.alloc([512], F32) for _ in range(2)]
            b_AA = [Buf() for _ in range(2)]
            HHb = ar.alloc([512], F32)
            b_HH = Buf()
            RQb = ar.alloc([512], F32)
            b_RQ = Buf()
            GEb = [ar.alloc([512], BF16) for _ in range(5)]
            b_GE = [Buf() for _ in range(5)]
            g0 = ar.alloc([4, NH, 16], F32)
            g1 = ar.alloc([4, NH, 16], F32)
            ge = ar.alloc([4, NH, 16], F32)
            b_g0, b_g1, b_ge = Buf(), Buf(), Buf()
            xcb = [ar.alloc([512], BF16) for _ in range(2)]
            b_xcb = [Buf(), Buf()]
            recf = ar.alloc([4, 512], F32)
            b_recf = [Buf() for _ in range(4)]
            recn = ar.alloc([4, 512], BF16)
            b_recn = Buf()
            rstd = ar.alloc([512], F32)
            b_rstd = Buf()
            gsb = ar.alloc([512], F32)
            b_gsb = Buf()

            def p1_xload(g, j):
                t_ = 4 * g + j
                P.dma('pool', lambda e, t_=t_, j=j: e.dma_start(out=xbf[:, j, :], in_=xsrc[t_ * 128:(t_ + 1) * 128, :]),
                      r=[b_xres[t_]] if l > 0 else [], w=[b_xbf[j]])

            def A_tile(g, j):
                xi = xbf
                bxi = b_xbf[j]
                t = 4 * g + j
                xT = xT2[g % 2]
                b_xT = b_xT2[g % 2]
                pb = bank_bf(0)
                for k in range(8):
                    P.pe(lambda e, j=j, k=k, pb=pb, xi=xi: e.transpose(out=pb[:, k * 128:(k + 1) * 128],
                                                                     in_=xi[:, j, k * 128:(k + 1) * 128], identity=idb),
                         r=[bxi, b_idb], w=[bb[0]])
                P.act(lambda e, j=j, pb=pb, xT=xT: e.copy(out=xT[:, :, j * 128:(j + 1) * 128],
                                                         in_=pb.rearrange("p (k t) -> p k t", t=128)),
                      r=[bb[0]], w=[b_xT[j]])
                if g + 1 < NG:
                    p1_xload(g + 1, j)
                for which in range(3):
                    bk = 2 + which
                    for k in range(8):
                        P.pe(lambda e, j=j, k=k, which=which, bk=bk: e.matmul(
                            banks[bk][:, :], lhsT=xT[:, k, j * 128:(j + 1) * 128],
                            rhs=w_in_sb[:, k, which * 512:(which + 1) * 512], start=(k == 0), stop=(k == 7)),
                            r=[b_xT[j]] + b_win, w=[bb[bk]])
                for which, dst in ((0, qb), (1, kb)):
                    bk = 2 + which
                    pv = banks[bk][:, :].rearrange("p (h d) -> p h d", d=64)
                    t1 = pv[:, :, 0:8]
                    t2 = pv[:, :, 8:16]
                    cs = cos_sb[:, t, :].unsqueeze(1).to_broadcast([128, NH, 8])
                    sn = sin_sb[:, t, :].unsqueeze(1).to_broadcast([128, NH, 8])
                    bdst = b_qb[j] if which == 0 else b_kb[j]
                    P.dve(lambda e, t1=t1, cs=cs: e.tensor_tensor(out=rt[0], in0=t1, in1=cs, op=ALU.mult),
                          r=[bb[bk], b_tab], w=[b_rt[0]])
                    P.dve(lambda e, t2=t2, sn=sn: e.tensor_tensor(out=rt[1], in0=t2, in1=sn, op=ALU.mult),
                          r=[bb[bk], b_tab2], w=[b_rt[1]])
                    P.dve(lambda e, t2=t2, cs=cs: e.tensor_tensor(out=rt[2], in0=t2, in1=cs, op=ALU.mult),
                          r=[bb[bk], b_tab], w=[b_rt[2]])
                    P.dve(lambda e, t1=t1, sn=sn: e.tensor_tensor(out=rt[3], in0=t1, in1=sn, op=ALU.mult),
                          r=[bb[bk], b_tab2], w=[b_rt[3]])
                    P.dve(lambda e, dst=dst, j=j: e.tensor_tensor(out=dst[:, j, :, 0:8], in0=rt[0], in1=rt[1], op=ALU.subtract),
                          r=[b_rt[0], b_rt[1]], w=[bdst])
                    P.dve(lambda e, dst=dst, j=j: e.tensor_tensor(out=dst[:, j, :, 8:16], in0=rt[2], in1=rt[3], op=ALU.add),
                          r=[b_rt[2], b_rt[3]], w=[bdst])
                    P.act(lambda e, dst=dst, j=j, pv=pv: e.copy(out=dst[:, j, :, 16:64], in_=pv[:, :, 16:64]),
                          r=[bb[bk]], w=[bdst])
                pvv = banks[4][:, :].rearrange("p (h d) -> p h d", d=64)
                P.act(lambda e, t=t, pvv=pvv: e.copy(out=vsb[:, t, :, 0:64], in_=pvv), r=[bb[4]], w=[b_v[t]])
                P.pool(lambda e, t=t: e.memset(vsb[:, t, :, 64:65], 1.0), r=[], w=[b_v[t]])
                pb = bank_bf(7)
                for h in range(NH):
                    P.pe(lambda e, j=j, h=h, pb=pb: e.transpose(out=pb[0:64, h * 128:(h + 1) * 128],
                                                              in_=kb[:, j, h, :], identity=idb),
                         r=[b_kb[j], b_idb], w=[bb[7]])
                P.act(lambda e, j=j, pb=pb: e.copy(out=kTh[:, :, j * 128:(j + 1) * 128],
                                                  in_=pb[0:64, :].rearrange("p (h t) -> p h t", t=128)),
                      r=[bb[7]], w=[b_kTh[j]])
                pb6 = bank_bf(6)
                for pr in range(4):
                    for w2 in range(2):
                        P.pe(lambda e, j=j, pr=pr, w2=w2, pb6=pb6: e.transpose(
                            out=pb6[w2 * 64:(w2 + 1) * 64, pr * 128:(pr + 1) * 128],
                            in_=qb[:, j, 2 * pr + w2, 0:64], identity=idb),
                            r=[b_qb[j], b_idb], w=[bb[6]])
                P.act(lambda e, j=j, pb6=pb6: e.copy(out=qTp[:, :, j * 128:(j + 1) * 128],
                                                    in_=pb6[:, 0:512].rearrange("p (h t) -> p h t", t=128)),
                      r=[bb[6]], w=[b_qTp[j]])

            def B_grp(g):
                for h in range(NH):
                    P.dma('sp', lambda e, h=h, g=g: e.dma_start(out=kT_d[h, :, g * 512:(g + 1) * 512], in_=kTh[:, h, :]),
                          r=b_kTh, w=[b_kT[h][g]])
                P.dve(lambda e: e.tensor_reduce(out=ksum, in_=kTh.rearrange("p h (b t) -> p h b t", t=256),
                                                 axis=AX.X, op=ALU.add), r=b_kTh, w=[b_ksum])
                ks4 = ksum.rearrange("p (pr w) b -> p pr w b", w=2)
                P.dve(lambda e, g=g, ks4=ks4: e.tensor_scalar(
                    out=kmeanBD[0:64, :, 0, 2 * g:2 * g + 2], in0=ks4[:, :, 0, :], scalar1=1.0 / 256.0, scalar2=None,
                    op0=ALU.mult), r=[b_ksum], w=[b_kmean])
                odd_bf = oddk
                P.dve(lambda e, ks4=ks4, odd_bf=odd_bf: e.tensor_scalar(
                    out=odd_bf.rearrange("p (pr b) -> p pr b", b=2), in0=ks4[:, :, 1, :], scalar1=1.0 / 256.0,
                    scalar2=None, op0=ALU.mult), r=[b_ksum], w=[b_oddk])
                P.pe(lambda e, odd_bf=odd_bf: e.matmul(banks[5][:, 0:8], lhsT=shiftb[0:64, :], rhs=odd_bf,
                                                       start=True, stop=True),
                     r=[b_oddk, b_shift], w=[bb[5]])
                P.act(lambda e, g=g: e.copy(out=kmeanBD[64:128, :, 1, 2 * g:2 * g + 2],
                                            in_=banks[5][64:128, 0:8].rearrange("p (pr b) -> p pr b", b=2)),
                      r=[bb[5]], w=[b_kmean])
                for j in range(4):
                    for pr in range(4):
                        P.pe(lambda e, j=j, pr=pr: e.matmul(
                            banks[5][:, (j * 4 + pr) * 32:(j * 4 + pr + 1) * 32],
                            lhsT=qTp[:, pr, j * 128:(j + 1) * 128],
                            rhs=kmeanBD[:, pr, :, :].rearrange("p w n -> p (w n)"), start=True, stop=True),
                            r=[b_qTp[j], b_kmean], w=[bb[5]])
                P.act(lambda e: e.copy(out=gsb, in_=banks[5][:, :]), r=[bb[5]], w=[b_gsb])
                yield
                gps = gsb.rearrange("p (j h n) -> p j h n", h=NH, n=16)
                gc = gcaus_sb[:, 4 * g:4 * g + 4, :].unsqueeze(2).to_broadcast([128, 4, NH, 16])
                go = gown_sb[:, 4 * g:4 * g + 4, :].unsqueeze(2).to_broadcast([128, 4, NH, 16])
                P.dve(lambda e, gps=gps, gc=gc: e.tensor_tensor(out=g0, in0=gps, in1=gc, op=ALU.add),
                      r=[b_gsb, b_tab3], w=[b_g0])
                P.dve(lambda e: e.tensor_copy(out=g1, in_=g0), r=[b_g0], w=[b_g1])
                for rnd in range(3):
                    P.dve(lambda e: e.tensor_reduce(out=gm, in_=g1, axis=AX.X, op=ALU.max), r=[b_g1], w=[b_gm])
                    P.dve(lambda e: e.tensor_tensor(out=ge, in0=g1, in1=gm.unsqueeze(3).to_broadcast([128, 4, NH, 16]),
                                                     op=ALU.is_ge), r=[b_g1, b_gm], w=[b_ge])
                    P.dve(lambda e: e.scalar_tensor_tensor(
                        out=g1.rearrange("p j h n -> p (j h n)"), in0=ge.rearrange("p j h n -> p (j h n)"),
                        scalar=-1e9, in1=g1.rearrange("p j h n -> p (j h n)"), op0=ALU.mult, op1=ALU.add),
                        r=[b_ge, b_g1], w=[b_g1])
                P.dve(lambda e: e.scalar_tensor_tensor(
                    out=ge.rearrange("p j h n -> p (j h n)"), in0=g0.rearrange("p j h n -> p (j h n)"),
                    scalar=-5e8, in1=g1.rearrange("p j h n -> p (j h n)"), op0=ALU.add, op1=ALU.is_lt),
                    r=[b_g0, b_g1], w=[b_ge])
                for j in range(4):
                    P.dve(lambda e, j=j, go=go: e.tensor_tensor(out=qb[:, j, :, 64:80], in0=ge[:, j, :, :],
                                                                 in1=go[:, j, :, :], op=ALU.mult),
                          r=[b_ge, b_tab4], w=[b_qb[j]])
                yield
                for j in range(4):
                    pb = bank_bf(7)
                    for h in range(NH):
                        P.pe(lambda e, j=j, h=h, pb=pb: e.transpose(out=pb[0:80, h * 128:(h + 1) * 128],
                                                                  in_=qb[:, j, h, :], identity=idb),
                             r=[b_qb[j], b_idb], w=[bb[7]])
                    P.act(lambda e, j=j, pb=pb: e.copy(out=qTa[:, :, j * 128:(j + 1) * 128],
                                                      in_=pb[0:80, :].rearrange("p (h t) -> p h t", t=128)),
                          r=[bb[7]], w=[b_qTah[j]])
                for h in range(NH):
                    P.dma('sp', lambda e, h=h, g=g: e.dma_start(out=qTa_d[h, :, g * 512:(g + 1) * 512], in_=qTa[:, h, :]),
                          r=b_qTah, w=[b_qTa[h][g]])


            def R0(q):
                g, c = divmod(q, 4)
                xT = xT2[g % 2]
                b_xT = b_xT2[g % 2]
                bkx = 2 + (q % 2)
                for k in range(8):
                    P.pe(lambda e, c=c, k=k, bkx=bkx, xT=xT: e.matmul(
                        banks[bkx][:, :], lhsT=w_in_sb[:, k, 1536 + c * 128:1536 + (c + 1) * 128],
                        rhs=xT[:, k, :], start=(k == 0), stop=(k == 7)),
                        r=b_xT + b_win, w=[bb[bkx]])
                P.act(lambda e, c=c, bkx=bkx: e.copy(out=xr_sb[:, c, 3:515], in_=banks[bkx][:, :]),
                      r=[bb[bkx]], w=[b_xr[c]])
                bkr = 2 + ((q + 1) % 2)
                for k in range(8):
                    P.pe(lambda e, c=c, k=k, bkr=bkr, xT=xT: e.matmul(
                        banks[bkr][:, :], lhsT=w_in_sb[:, k, 2048 + c * 128:2048 + (c + 1) * 128],
                        rhs=xT[:, k, :], start=(k == 0), stop=(k == 7)),
                        r=b_xT + b_win, w=[bb[bkr]])
                gi = q % 5
                P.act(lambda e, gi=gi, bkr=bkr: e.activation(out=GEb[gi], in_=banks[bkr][:, :], func=AF.Gelu_apprx_tanh),
                      r=[bb[bkr]], w=[b_GE[gi]])

            def R1(q):
                g, c = divmod(q, 4)
                xc = XC[q % 3]
                bxc = b_XC[q % 3]
                P.dve(lambda e, c=c, xc=xc: e.tensor_scalar(
                    out=xc, in0=xr_sb[:, c, 0:512], scalar1=convw_sb[:, c, 0:1], scalar2=convb_sb[:, c:c + 1],
                    op0=ALU.mult, op1=ALU.add), r=[b_xr[c], b_p[0], b_p[1]], w=[bxc])
                for jj in range(1, 4):
                    P.dve(lambda e, c=c, xc=xc, jj=jj: e.scalar_tensor_tensor(
                        out=xc, in0=xr_sb[:, c, jj:jj + 512], scalar=convw_sb[:, c, jj:jj + 1], in1=xc,
                        op0=ALU.mult, op1=ALU.add), r=[b_xr[c], b_p[0], bxc], w=[bxc])
                P.pool(lambda e, c=c: e.tensor_copy(out=xr_sb[:, c, 0:3], in_=xr_sb[:, c, 512:515]),
                       r=[b_xr[c]], w=[b_xr[c]])
                s2 = q % 2
                P.act(lambda e, xc=xc, s2=s2: e.copy(out=xcb[s2], in_=xc), r=[bxc], w=[b_xcb[s2]])

            def R2(q):
                g, c = divmod(q, 4)
                s2 = q % 2
                P.pe(lambda e, c=c, s2=s2: e.matmul(banks[5][:, :], lhsT=wa_sb[:, c, :], rhs=xcb[s2], start=True, stop=True),
                     r=[b_wa, b_xcb[s2]], w=[bb[5]])
                P.act(lambda e, c=c, s2=s2: e.activation(out=RRb[s2], in_=banks[5][:, :], func=AF.Sigmoid,
                                                         bias=ba_sb[:, c:c + 1]), r=[bb[5], b_p[2]], w=[b_RR[s2]])
                P.pe(lambda e, c=c, s2=s2: e.matmul(banks[6][:, :], lhsT=wi_sb[:, c, :], rhs=xcb[s2], start=True, stop=True),
                     r=[b_wi, b_xcb[s2]], w=[bb[6]])
                P.act(lambda e, c=c, s2=s2: e.activation(out=IIb[s2], in_=banks[6][:, :], func=AF.Sigmoid,
                                                         bias=bi_sb[:, c:c + 1]), r=[bb[6], b_p[3]], w=[b_II[s2]])

            def R3a(q):
                g, c = divmod(q, 4)
                s2 = q % 2
                xc = XC[q % 3]
                bxc = b_XC[q % 3]
                P.act(lambda e, c=c, s2=s2: e.activation(out=AAb[s2], in_=RRb[s2], func=AF.Exp, scale=cdec_sb[:, c:c + 1]),
                      r=[b_RR[s2], b_cdec], w=[b_AA[s2]])
                P.act(lambda e, s2=s2: e.activation(out=RRb[s2], in_=AAb[s2], func=AF.Square), r=[b_AA[s2]], w=[b_RR[s2]])
                P.act(lambda e, s2=s2: e.activation(out=RRb[s2], in_=RRb[s2], func=AF.Sqrt, scale=-1.0, bias=1.0),
                      r=[b_RR[s2]], w=[b_RR[s2]])
                P.pool(lambda e, s2=s2, xc=xc: e.tensor_tensor(out=IIb[s2], in0=IIb[s2], in1=xc, op=ALU.mult),
                       r=[b_II[s2], bxc], w=[b_II[s2]])

            def R3d(q):
                s2 = q % 2
                P.dve(lambda e, s2=s2: e.tensor_tensor(out=IIb[s2], in0=IIb[s2], in1=RRb[s2], op=ALU.mult),
                      r=[b_II[s2], b_RR[s2]], w=[b_II[s2]])

            def R4d(q):
                g, c = divmod(q, 4)
                s2 = q % 2
                gi = q % 5
                if g == 0:
                    P.dve(lambda e, s2=s2: e.tensor_tensor_scan(out=HHb, data0=AAb[s2], data1=IIb[s2], initial=0.0,
                                                                op0=ALU.mult, op1=ALU.add),
                          r=[b_AA[s2], b_II[s2]], w=[b_HH])
                else:
                    P.dve(lambda e, s2=s2, c=c: e.tensor_tensor_scan(out=HHb, data0=AAb[s2], data1=IIb[s2],
                                                                     initial=hst[:, c, 0:1], op0=ALU.mult, op1=ALU.add),
                          r=[b_AA[s2], b_II[s2], b_h[c]], w=[b_HH])
                P.pool(lambda e, c=c: e.tensor_copy(out=hst[:, c, 0:1], in_=HHb[:, 511:512]), r=[b_HH], w=[b_h[c]])
                P.dve(lambda e, c=c, gi=gi: e.tensor_tensor(out=recf[:, c, :], in0=HHb, in1=GEb[gi], op=ALU.mult),
                      r=[b_HH, b_GE[gi]], w=[b_recf[c]])

            def R4a(q):
                g, c = divmod(q, 4)
                P.act(lambda e, c=c: e.activation(out=RQb, in_=recf[:, c, :], func=AF.Square), r=[b_recf[c]], w=[b_RQ])

            def R4p(q):
                g, c = divmod(q, 4)
                P.pe(lambda e, c=c: e.matmul(banks[1][:, :], lhsT=ones_f, rhs=RQb, start=(c == 0), stop=(c == 3)),
                     r=[b_ones, b_RQ], w=[bb[1]])
                if c == 3:
                    C_final(g)

            def C_final(g):
                P.act(lambda e: e.activation(out=rstd, in_=banks[1][:, :], func=AF.Sqrt, scale=1.0 / 512.0, bias=RMS_EPS),
                      r=[bb[1]], w=[b_rstd])
                P.dve(lambda e: e.reciprocal(out=rstd, in_=rstd), r=[b_rstd], w=[b_rstd])
                for c in range(4):
                    P.dve(lambda e, c=c: e.scalar_tensor_tensor(out=recn[:, c, :], in0=recf[:, c, :], scalar=grec_sb[:, c:c + 1],
                                                                 in1=rstd, op0=ALU.mult, op1=ALU.mult),
                          r=[b_recf[c], b_rstd, b_p[5]], w=[b_recn])
                P.dma('sp', lambda e, g=g: e.dma_start(out=recT_d[:, :, g * 512:(g + 1) * 512].rearrange("c p t -> p c t"),
                                                      in_=recn), r=[b_recn], w=[b_recT[g]])

            for j in range(4):
                p1_xload(0, j)
            for j in range(4):
                A_tile(0, j)
            NQ = 4 * NG
            bgen = None
            for it in range(NQ + 4):
                if 0 <= it - 3 < NQ:
                    R3a(it - 3)
                if 0 <= it - 4 < NQ:
                    R4d(it - 4)
                if 0 <= it - 3 < NQ:
                    R3d(it - 3)
                if 0 <= it - 4 < NQ:
                    R4a(it - 4)
                if 0 <= it - 2 < NQ:
                    R2(it - 2)
                if 0 <= it - 1 < NQ:
                    R1(it - 1)
                if it < NQ:
                    R0(it)
                if 0 <= it - 4 < NQ:
                    R4p(it - 4)
                gq, m = divmod(it, 4)
                if gq < NG:
                    if m == 0:
                        bgen = B_grp(gq)
                    if bgen is not None:
                        next(bgen, None)
                    if m == 2:
                        for _ in bgen:
                            pass
                        bgen = None
                    if gq + 1 < NG:
                        if m == 2:
                            A_tile(gq + 1, 0)
                            A_tile(gq + 1, 1)
                        if m == 3:
                            A_tile(gq + 1, 2)
                            A_tile(gq + 1, 3)
            P.barrier()

            if l >= 1 and '2' in SKIP1:
                return
            ar.release(mid_mark)
            qh = [ar.alloc([S], BF16, parts=80) for _ in range(2)]
            kh = [ar.alloc([S], BF16, parts=80) for _ in range(2)]
            b_qh = [Buf(), Buf()]
            b_kh = [Buf(), Buf()]
            b_khot = [Buf(), Buf()]
            khot_f = ar.alloc([S], F32, parts=80)
            b_khf = Buf()
            P.dma('sp', lambda e: e.dma_start(out=khot_f[64:80, :], in_=khot_d[:, :]), w=[b_khf])
            for i in range(2):
                P.dve(lambda e, i=i: e.tensor_copy(out=kh[i][64:80, :], in_=khot_f[64:80, :]), r=[b_khf], w=[b_khot[i]])
            zt = ar.alloc([8, D], BF16)
            b_zt = Buf()
            P.dve(lambda e: e.memset(zt, 0.0), w=[b_zt])
            NPT = 3
            pT = [ar.alloc([2, 2, 256], BF16) for _ in range(NPT)]
            b_pT = [Buf() for _ in range(NPT)]
            rcp = [ar.alloc([2], F32) for _ in range(2)]
            b_rcp = [Buf(), Buf()]
            OB = ((4, 5), (6, 7))
            units = []
            for h in range(NH):
                for bi in range(NB):
                    n = 0
                    while n < bi:
                        if n + 1 < bi:
                            units.append((h, bi, (n, n + 1)))
                            n += 2
                        else:
                            units.append((h, bi, (n,)))
                            n += 1
                    units.append((h, bi, (bi,)))
            cnt_state = {}

            def stageA(u, ui):
                h, bi, ns = u
                i = h % 2
                if bi == 0:
                    P.dma('sp', lambda e, h=h, i=i: e.dma_start(out=qh[i], in_=qTa_d[h, :, :]), r=b_qTa[h], w=[b_qh[i]])
                    P.dma('sp', lambda e, h=h, i=i: e.dma_start(out=kh[i][0:64, :], in_=kT_d[h, :, :]), r=b_kT[h], w=[b_kh[i]])
                q0 = bi * 256
                slot = ui % 2
                for ki, n in enumerate(ns):
                    sb_i = slot * 2 + ki
                    sps = banks[sb_i][:, :].rearrange("p (t q) -> p t q", q=256)
                    if n != bi:
                        for tt in range(2):
                            kt = 2 * n + tt
                            P.pe(lambda e, i=i, kt=kt, tt=tt, sps=sps, q0=q0: e.matmul(
                                sps[:, tt, :], lhsT=kh[i][:, kt * 128:(kt + 1) * 128], rhs=qh[i][:, q0:q0 + 256],
                                start=True, stop=True), r=[b_kh[i], b_khot[i], b_qh[i]], w=[bb[sb_i]])
                    else:
                        kt0 = 2 * bi
                        kt1 = 2 * bi + 1
                        P.pe(lambda e, i=i, kt0=kt0, sps=sps, q0=q0: e.matmul(
                            sps[:, 0, :], lhsT=kh[i][:, kt0 * 128:(kt0 + 1) * 128], rhs=qh[i][:, q0:q0 + 256],
                            start=True, stop=False), r=[b_kh[i], b_khot[i], b_qh[i]], w=[bb[sb_i]])
                        P.pe(lambda e, sps=sps: e.matmul(sps[:, 0, 0:128], lhsT=idb, rhs=trib, start=False, stop=True),
                             r=[b_idb, b_trib], w=[bb[sb_i]])
                        P.pe(lambda e, i=i, kt1=kt1, sps=sps, q0=q0: e.matmul(
                            sps[:, 1, 128:256], lhsT=kh[i][:, kt1 * 128:(kt1 + 1) * 128],
                            rhs=qh[i][:, q0 + 128:q0 + 256], start=True, stop=False),
                            r=[b_kh[i], b_khot[i], b_qh[i]], w=[bb[sb_i]])
                        P.pe(lambda e, sps=sps: e.matmul(sps[:, 1, 128:256], lhsT=idb, rhs=trib, start=False, stop=True),
                             r=[b_idb, b_trib], w=[bb[sb_i]])

            def stageB(u, ui):
                h, bi, ns = u
                slot = ui % 2
                pt = pT[ui % NPT]
                bpt = b_pT[ui % NPT]
                if ns[0] != bi:
                    nu = len(ns)
                    src = ps_all[:, slot * 1024:slot * 1024 + nu * 512]
                    dst = pt[:, 0:nu, :, :].rearrange("p u t q -> p (u t q)")
                    P.act(lambda e, dst=dst, src=src: e.activation(out=dst, in_=src, func=AF.Exp, scale=0.125),
                          r=[bb[slot * 2 + k] for k in range(nu)], w=[bpt])
                else:
                    sb_i = slot * 2
                    sps = banks[sb_i][:, :].rearrange("p (t q) -> p t q", q=256)
                    P.act(lambda e, pt=pt, sps=sps: e.activation(out=pt[:, 0, 0, :], in_=sps[:, 0, :], func=AF.Exp, scale=0.125),
                          r=[bb[sb_i]], w=[bpt])
                    P.act(lambda e, pt=pt, sps=sps: e.activation(out=pt[:, 0, 1, 128:256], in_=sps[:, 1, 128:256],
                                                                func=AF.Exp, scale=0.125),
                          r=[bb[sb_i]], w=[bpt])

            def stageC(u, ui):
                h, bi, ns = u
                ob = OB[bi % 2]
                pt = pT[ui % NPT]
                bpt = b_pT[ui % NPT]
                nmm = [2 * bi + 1, 2 * bi + 2]
                if ns[0] == 0:
                    cnt_state[(h, bi)] = [0, 0]
                cntj = cnt_state[(h, bi)]
                for ki, n in enumerate(ns):
                    if n != bi:
                        lst = [(jq, tt, 2 * n + tt) for jq in range(2) for tt in range(2)]
                    else:
                        lst = [(0, 0, 2 * bi), (1, 0, 2 * bi), (1, 1, 2 * bi + 1)]
                    for (jq, tt, kt) in lst:
                        first = (cntj[jq] == 0)
                        cntj[jq] += 1
                        last = (cntj[jq] == nmm[jq])
                        P.pe(lambda e, jq=jq, tt=tt, kt=kt, pt=pt, h=h, first=first, last=last, ob=ob, ki=ki: e.matmul(
                            banks[ob[jq]][:, 0:65], lhsT=pt[:, ki, tt, jq * 128:(jq + 1) * 128],
                            rhs=vsb[:, kt, h, :], start=first, stop=last),
                            r=[bpt, b_v[kt]], w=[bb[ob[jq]]])
                if ns[-1] == bi:
                    assert cntj == nmm
                    rc = rcp[bi % 2]
                    brc = b_rcp[bi % 2]
                    for jq in range(2):
                        t = 2 * bi + jq
                        P.dve(lambda e, jq=jq, rc=rc, ob=ob: e.reciprocal(out=rc[:, jq:jq + 1], in_=banks[ob[jq]][:, 64:65]),
                              r=[bb[ob[jq]]], w=[brc])
                        P.dve(lambda e, jq=jq, rc=rc, ob=ob, t=t, h=h: e.tensor_scalar(
                            out=attn_sb[:, t, h * 64:(h + 1) * 64], in0=banks[ob[jq]][:, 0:64], scalar1=rc[:, jq:jq + 1],
                            scalar2=None, op0=ALU.mult), r=[bb[ob[jq]], brc], w=[b_attn[t][h]])

            LAG = 1
            NU = len(units)
            casts = []
            for ex in range(NEXP):
                casts.append((wgb_d, wg_d, ex, D))
                casts.append((wub_d, wu_d, ex, D))
                casts.append((wdb_d, wd_d, ex, DE))
            cast_every = max(1, (NU - 8) // len(casts))
            ci = 0
            zi_c = [0]
            zero_every = max(6, (NU - 16) // (NSLOT // 1024))
            for ui in range(NU + LAG):
                if ui < NU:
                    stageA(units[ui], ui)
                    stageB(units[ui], ui)
                if ui >= LAG:
                    stageC(units[ui - LAG], ui - LAG)
                if ui % zero_every == 5 and zi_c[0] < NSLOT // 1024:
                    zi = zi_c[0]
                    zi_c[0] += 1
                    P.dma('sp', lambda e, zi=zi: e.dma_start(
                        out=xs_d[zi * 1024:(zi + 1) * 1024, :].rearrange("(a p) d -> p a d", p=128), in_=zt),
                        r=[b_zt], w=[b_xs_z[zi]])
                if ui % cast_every == 0 and ci < len(casts):
                    dst_t, src_t, ex, rows = casts[ci]
                    ci += 1
                    P.dma('pool', lambda e, dst_t=dst_t, src_t=src_t, ex=ex, rows=rows: e.dma_start(
                        out=dst_t[l][ex].rearrange("p (k n) -> p k n", k=rows // 128),
                        in_=src_t[l, ex].rearrange("(k p) n -> p k n", p=128)), r=[b_wc_w])
            while zi_c[0] < NSLOT // 1024:
                zi = zi_c[0]
                zi_c[0] += 1
                P.dma('sp', lambda e, zi=zi: e.dma_start(
                    out=xs_d[zi * 1024:(zi + 1) * 1024, :].rearrange("(a p) d -> p a d", p=128), in_=zt),
                    r=[b_zt], w=[b_xs_z[zi]])
            while ci < len(casts):
                dst_t, src_t, ex, rows = casts[ci]
                ci += 1
                P.dma('pool', lambda e, dst_t=dst_t, src_t=src_t, ex=ex, rows=rows: e.dma_start(
                    out=dst_t[l][ex].rearrange("p (k n) -> p k n", k=rows // 128),
                    in_=src_t[l, ex].rearrange("(k p) n -> p k n", p=128)), r=[b_wc_w])
            P.op('sp', lambda e: e.nop(), w=[b_wc_w, b_wc])
            P.barrier()

            if l >= 1 and '3' in SKIP1:
                return
            ar.release(mid_mark)
            w_out_sb = ar.alloc([8, D], BF16)
            b_wout = [Buf(), Buf()]
            for hh in range(2):
                P.dma('pool', lambda e, hh=hh: e.dma_start(
                    out=w_out_sb[:, hh * 4:(hh + 1) * 4, :],
                    in_=w_out_d[l].rearrange("(k p) n -> p k n", p=128)[:, hh * 4:(hh + 1) * 4, :]), w=[b_wout[hh]])
            gattn_sb = ar.alloc([512], F32)
            ln1g_sb = ar.alloc([D], F32)
            ln1b_sb = ar.alloc([D], F32)
            wr_sb = ar.alloc([8, 20], BF16)
            br_sb = ar.alloc([20], F32)
            b_c3 = [Buf() for _ in range(5)]
            P.dma('sp', lambda e: e.dma_start(out=gattn_sb, in_=gattn_d[l]), w=[b_c3[0]])
            P.dma('sp', lambda e: e.dma_start(out=ln1g_sb, in_=ln1g_d[l]), w=[b_c3[1]])
            P.dma('sp', lambda e: e.dma_start(out=ln1b_sb, in_=ln1b_d[l]), w=[b_c3[2]])
            P.dma('pool', lambda e: e.dma_start(out=wr_sb, in_=wr_d[l].rearrange("(k p) n -> p k n", p=128)), w=[b_c3[3]])
            P.dma('sp', lambda e: e.dma_start(out=br_sb, in_=br_d[l]), w=[b_c3[4]])
            NBUF = 4
            junk = [ar.alloc([512], BF16) for _ in range(NBUF)]
            ssq = [ar.alloc([2], F32) for _ in range(NBUF)]
            an = [ar.alloc([512], BF16) for _ in range(NBUF)]
            mixT = [ar.alloc([8, 128], BF16) for _ in range(NBUF)]
            xt3 = [ar.alloc([D], F32) for _ in range(NBUF)]
            y3 = [ar.alloc([D], F32) for _ in range(NBUF)]
            st3 = [ar.alloc([2, 6], F32) for _ in range(NBUF)]
            mv3 = [ar.alloc([2], F32) for _ in range(NBUF)]
            x1f = [ar.alloc([D], F32) for _ in range(NBUF)]
            x1b = [ar.alloc([D], BF16) for _ in range(NBUF)]
            x1T = [ar.alloc([8, 128], BF16) for _ in range(NBUF)]
            B3 = [{k: Buf() for k in ('junk', 'ssq', 'an', 'mixa', 'mixr', 'xt', 'y', 'st', 'mv', 'x1f', 'x1b', 'x1T')}
                  for _ in range(NBUF)]

            def p3_load(t):
                s = t % NBUF
                B = B3[s]
                g = t // 4
                P.dma('sp', lambda e, t=t, s=s: e.dma_start(
                    out=mixT[s][:, 4:8, :], in_=recT_d[:, :, t * 128:(t + 1) * 128].rearrange("c p t -> p c t")),
                    r=[b_recT[g]], w=[B['mixr']])
                P.dma('sp', lambda e, t=t, s=s: e.dma_start(out=xt3[s], in_=xsrc[t * 128:(t + 1) * 128, :]),
                      r=[b_xres[t]] if l > 0 else [], w=[B['xt']])

            def p3_A1(t):
                s = t % NBUF
                B = B3[s]
                P.act(lambda e, t=t, s=s: e.activation(out=junk[s], in_=attn_sb[:, t, :], func=AF.Square,
                                                       accum_out=ssq[s][:, 0:1]), r=b_attn[t], w=[B['junk'], B['ssq']])
                P.act(lambda e, s=s: e.activation(out=ssq[s][:, 1:2], in_=ssq[s][:, 0:1], func=AF.Sqrt,
                                                  scale=1.0 / 512.0, bias=RMS_EPS), r=[B['ssq']], w=[B['ssq']])
                P.dve(lambda e, s=s: e.reciprocal(out=ssq[s][:, 1:2], in_=ssq[s][:, 1:2]), r=[B['ssq']], w=[B['ssq']])
                P.dve(lambda e, t=t, s=s: e.scalar_tensor_tensor(out=an[s], in0=attn_sb[:, t, :], scalar=ssq[s][:, 1:2],
                                                                  in1=gattn_sb, op0=ALU.mult, op1=ALU.mult),
                      r=b_attn[t] + [B['ssq'], b_c3[0]], w=[B['an']])
                tb = 0 if t % 2 == 0 else 7
                pb = bank_bf(tb)
                for c in range(4):
                    P.pe(lambda e, c=c, s=s, pb=pb: e.transpose(out=pb[:, c * 128:(c + 1) * 128],
                                                              in_=an[s][:, c * 128:(c + 1) * 128], identity=idb),
                         r=[B['an'], b_idb], w=[bb[tb]])
                P.act(lambda e, s=s, pb=pb: e.copy(out=mixT[s][:, 0:4, :], in_=pb[:, 0:512].rearrange("p (c t) -> p c t", t=128)),
                      r=[bb[tb]], w=[B['mixa']])

            def p3_A2(t):
                s = t % NBUF
                B = B3[s]
                ob = 1 if t % 2 == 0 else 5
                for nh in range(2):
                    for k in range(8):
                        P.pe(lambda e, k=k, nh=nh, s=s, ob=ob: e.matmul(banks[ob + nh][:, :], lhsT=mixT[s][:, k, :],
                                                                        rhs=w_out_sb[:, k, nh * 512:(nh + 1) * 512],
                                                                        start=(k == 0), stop=(k == 7)),
                             r=[B['mixa'], B['mixr'], b_wout[0], b_wout[1]], w=[bb[ob + nh]])
                for nh in range(2):
                    P.dve(lambda e, nh=nh, s=s, ob=ob: e.scalar_tensor_tensor(
                        out=y3[s][:, nh * 512:(nh + 1) * 512], in0=xt3[s][:, nh * 512:(nh + 1) * 512], scalar=ALPHA,
                        in1=banks[ob + nh][:, :], op0=ALU.mult, op1=ALU.add), r=[B['xt'], bb[ob + nh]], w=[B['y']])
                ln_stats(P, y3[s], B['y'], st3[s], B['st'], mv3[s], B['mv'])

            def p3_A3(t):
                s = t % NBUF
                B = B3[s]
                ln_apply(P, y3[s], B['y'], mv3[s], B['mv'], ln1g_sb, b_c3[1], ln1b_sb, b_c3[2], x1f[s], B['x1f'])
                P.dma('sp', lambda e, t=t, s=s: e.dma_start(out=xres_d[t * 128:(t + 1) * 128, :], in_=x1f[s]),
                      r=[B['x1f']], w=[b_xres[t]])
                if dbg and l == DBG_LAYER:
                    P.dma('sp', lambda e, t=t, s=s: e.dma_start(out=dbg_d[t * 128:(t + 1) * 128, :], in_=x1f[s]),
                          r=[B['x1f'], b_out_done])
                P.act(lambda e, s=s: e.copy(out=x1b[s], in_=x1f[s]), r=[B['x1f']], w=[B['x1b']])

            def p3_B(t):
                s = t % NBUF
                B = B3[s]
                pb = bank_bf(3)
                for k in range(8):
                    P.pe(lambda e, k=k, s=s, pb=pb: e.transpose(out=pb[:, k * 128:(k + 1) * 128],
                                                              in_=x1b[s][:, k * 128:(k + 1) * 128], identity=idb),
                         r=[B['x1b'], b_idb], w=[bb[3]])
                P.act(lambda e, s=s, pb=pb: e.copy(out=x1T[s], in_=pb.rearrange("p (k t) -> p k t", t=128)),
                      r=[bb[3]], w=[B['x1T']])
                for k in range(8):
                    P.pe(lambda e, k=k, s=s: e.matmul(banks[4][:, 0:20], lhsT=x1T[s][:, k, :], rhs=wr_sb[:, k, :],
                                                      start=(k == 0), stop=(k == 7)),
                         r=[B['x1T'], b_c3[3]], w=[bb[4]])
                P.dve(lambda e, t=t: e.tensor_tensor(out=lg_sb[:, t, :], in0=banks[4][:, 0:20], in1=br_sb, op=ALU.add),
                      r=[bb[4], b_c3[4]], w=[b_lg[t]])

            p3_load(0)
            for i in range(NT + 3):
                if i + 1 < NT:
                    p3_load(i + 1)
                if 0 <= i - 3 < NT:
                    p3_B(i - 3)
                if 0 <= i - 2 < NT:
                    p3_A3(i - 2)
                if 0 <= i - 1 < NT:
                    p3_A2(i - 1)
                if i < NT:
                    p3_A1(i)
            P.barrier()
            ar.release(base_mark)
            NQ = NT * 4
            LE = ar.alloc([NQ, 4], F32)
            EQ = ar.alloc([NQ, 4], F32)
            gsm = ar.alloc([6, NT, 4], F32)
            exg = gsm[:, 0, :, :]
            gm1 = gsm[:, 1, :, :]
            gmask = gsm[:, 2, :, :]
            m1 = gsm[:, 3, :, :].rearrange("p t g -> p (t g)")
            m2 = gsm[:, 4, :, :].rearrange("p t g -> p (t g)")
            den = gsm[:, 5, :, :].rearrange("p t g -> p (t g)")
            gs2 = ar.alloc([2, NT], F32)
            mg = gs2[:, 0, :]
            sgm = gs2[:, 1, :]
            RR = [Buf()]
            Lg = lg_sb[:, :, 0:4]
            P.dve(lambda e: e.tensor_reduce(out=mg, in_=Lg, axis=AX.X, op=ALU.max), r=b_lg, w=RR)
            mgb = mg.unsqueeze(2).to_broadcast([128, NT, 4])
            P.dve(lambda e: e.tensor_tensor(out=gm1, in0=Lg, in1=mgb, op=ALU.is_ge), r=b_lg + RR, w=RR)
            P.dve(lambda e: e.tensor_tensor(out=exg, in0=Lg, in1=mgb, op=ALU.subtract), r=b_lg + RR, w=RR)
            P.act(lambda e: e.activation(out=exg, in_=exg, func=AF.Exp), r=RR, w=RR)
            P.dve(lambda e: e.tensor_reduce(out=sgm, in_=exg, axis=AX.X, op=ALU.add), r=RR, w=RR)
            P.dve(lambda e: e.reciprocal(out=sgm, in_=sgm), r=RR, w=RR)
            P.dve(lambda e: e.tensor_tensor(out=gmask, in0=gm1, in1=sgm.unsqueeze(2).to_broadcast([128, NT, 4]), op=ALU.mult),
                  r=RR, w=RR)
            LEt = LE.rearrange("p (t g) e -> p t (g e)", g=4)
            LEf = LE.rearrange("p q e -> p (q e)")
            EQf = EQ.rearrange("p q e -> p (q e)")
            P.dve(lambda e: e.tensor_copy(out=LEt, in_=lg_sb[:, :, 4:20]), r=b_lg, w=RR)
            m1b = m1.unsqueeze(2).to_broadcast([128, NQ, 4])
            m2b = m2.unsqueeze(2).to_broadcast([128, NQ, 4])
            P.dve(lambda e: e.tensor_reduce(out=m1, in_=LE, axis=AX.X, op=ALU.max), r=RR, w=RR)
            P.dve(lambda e: e.tensor_tensor(out=EQ, in0=LE, in1=m1b, op=ALU.is_ge), r=RR, w=RR)
            P.dve(lambda e: e.scalar_tensor_tensor(out=EQf, in0=EQf, scalar=-1e9, in1=LEf, op0=ALU.mult, op1=ALU.add), r=RR, w=RR)
            P.dve(lambda e: e.tensor_reduce(out=m2, in_=EQ, axis=AX.X, op=ALU.max), r=RR, w=RR)
            P.dve(lambda e: e.tensor_tensor(out=EQ, in0=LE, in1=m2b, op=ALU.is_ge), r=RR, w=RR)
            gm1b = gm1.rearrange("p t g -> p (t g)").unsqueeze(2).to_broadcast([128, NQ, 4])
            P.dve(lambda e: e.tensor_tensor(out=sel_sb.rearrange("p t (g e) -> p (t g) e", e=4), in0=EQ, in1=gm1b, op=ALU.mult),
                  r=RR, w=b_sel)
            P.dve(lambda e: e.tensor_tensor(out=LE, in0=LE, in1=m1b, op=ALU.subtract), r=RR, w=RR)
            P.act(lambda e: e.activation(out=LEf, in_=LEf, func=AF.Exp), r=RR, w=RR)
            P.dve(lambda e: e.tensor_tensor(out=LEf, in0=LEf, in1=EQf, op=ALU.mult), r=RR, w=RR)
            P.dve(lambda e: e.tensor_reduce(out=den, in_=LE, axis=AX.X, op=ALU.add), r=RR, w=RR)
            P.dve(lambda e: e.reciprocal(out=den, in_=den), r=RR, w=RR)
            P.dve(lambda e: e.tensor_tensor(out=den, in0=den, in1=gmask.rearrange("p t g -> p (t g)"), op=ALU.mult), r=RR, w=RR)
            P.dve(lambda e: e.tensor_tensor(out=comb_sb.rearrange("p t (g e) -> p (t g) e", e=4), in0=LE,
                                             in1=den.unsqueeze(2).to_broadcast([128, NQ, 4]), op=ALU.mult), r=RR, w=b_comb)
            selflat = sel_sb.rearrange("p t e -> p (t e)")
            P.pe(lambda e: e.matmul(banks[0][:, 0:NT * 16], lhsT=ltrib, rhs=selflat, start=True, stop=True),
                 r=[b_ltri] + b_sel, w=[bb[0]])
            P.pe(lambda e: e.matmul(banks[1][:, 0:NT * 16], lhsT=onesb, rhs=selflat, start=True, stop=True),
                 r=[b_onesb] + b_sel, w=[bb[1]])
            cinc = ar.alloc([16, NT], F32)
            onesT = ar.alloc([NT], F32)
            b_cinc = Buf()
            P.dve(lambda e: e.memset(onesT, 1.0), w=[b_cinc])
            tot_et = banks[1][:, 0:NT * 16].rearrange("p (t e) -> p e t", e=16)
            for ee in range(16):
                P.dve(lambda e, ee=ee: e.tensor_tensor_scan(out=cinc[:, ee, :], data0=onesT, data1=tot_et[:, ee, :], initial=0.0,
                                                            op0=ALU.mult, op1=ALU.add), r=[bb[1], b_cinc], w=[b_cinc])
            P.dve(lambda e: e.tensor_copy(out=carry_sb, in_=cinc[:, :, NT - 1]), r=[b_cinc], w=[b_carry])
            P.dve(lambda e: e.tensor_tensor(out=rank_sb, in0=banks[0][:, 0:NT * 16].rearrange("p (t e) -> p t e", e=16),
                                             in1=cinc.rearrange("p e t -> p t e"), op=ALU.add), r=[bb[0], b_cinc], w=b_rank)
            P.dve(lambda e: e.tensor_tensor(out=rank_sb, in0=rank_sb, in1=banks[1][:, 0:NT * 16].rearrange("p (t e) -> p t e", e=16),
                                             op=ALU.subtract), r=[bb[1]] + b_rank, w=b_rank)
            jstart_sb = ar.alloc([32, 16], F32)
            b_jstart = Buf()
            P.dma('sp', lambda e: e.dma_start(out=jstart_sb, in_=jstart_d[:, :, :]), w=[b_jstart])
            seg = ar.alloc([8, 16], F32)
            segi = ar.alloc([16], I32)
            cmp3 = ar.alloc([32, 16], F32)
            ejf = ar.alloc([32], F32)
            big = [ar.alloc([NT, 16], F32) for _ in range(4)]
            red = ar.alloc([4, NT], F32)
            b_seg = Buf()
            RS = [b_seg]
            m_e = seg[:, 0, :]
            cend = seg[:, 1, :]
            s_e = seg[:, 2, :]
            ones16 = seg[:, 3, :]
            P.dve(lambda e: e.tensor_scalar(out=segi, in0=carry_sb, scalar1=511.0, scalar2=None, op0=ALU.add), r=[b_carry], w=RS)
            P.dve(lambda e: e.tensor_scalar(out=segi, in0=segi, scalar1=9, scalar2=9, op0=ALU.arith_shift_right,
                                             op1=ALU.logical_shift_left), r=RS, w=RS)
            P.dve(lambda e: e.tensor_copy(out=m_e, in_=segi), r=RS, w=RS)
            P.dve(lambda e: e.memset(ones16, 1.0), r=RS, w=RS)
            P.dve(lambda e: e.tensor_tensor_scan(out=cend, data0=ones16, data1=m_e, initial=0.0, op0=ALU.mult, op1=ALU.add),
                  r=RS, w=RS)
            P.dve(lambda e: e.tensor_tensor(out=s_e, in0=cend, in1=m_e, op=ALU.subtract), r=RS, w=RS)
            P.dve(lambda e: e.tensor_tensor(out=cmp3, in0=cend.unsqueeze(1).to_broadcast([128, 32, 16]), in1=jstart_sb,
                                             op=ALU.is_le), r=RS + [b_jstart], w=RS)
            P.dve(lambda e: e.tensor_reduce(out=ejf, in_=cmp3, axis=AX.X, op=ALU.add), r=RS, w=RS)
            P.dve(lambda e: e.tensor_scalar(out=ejc, in0=ejf, scalar1=15.0, scalar2=None, op0=ALU.min), r=RS, w=[b_ej])
            P.dve(lambda e: e.tensor_scalar(out=ejs, in0=ejc, scalar1=128.0, scalar2=0.0, op0=ALU.mult, op1=ALU.add),
                  r=[b_ej], w=[b_ej])
            P.dve(lambda e: e.tensor_scalar(out=idxw, in0=ejs, scalar1=wbase_sb[:, 0:1], scalar2=None, op0=ALU.add),
                  r=[b_ej, b_wbase], w=[b_ej])
            rk = rank_sb.rearrange("p t e -> p (t e)")
            sl2 = sel_sb.rearrange("p t e -> p (t e)")
            cb2 = comb_sb.rearrange("p t e -> p (t e)")
            v_, mk, tm, v2 = [b.rearrange("p t e -> p (t e)") for b in big]
            v3, mk3, tm3, v23 = big
            i0, i1, w0, w1 = red[:, 0, :], red[:, 1, :], red[:, 2, :], red[:, 3, :]
            RB = [Buf()]
            allr = b_rank + b_sel + b_comb
            P.dve(lambda e: e.tensor_tensor(out=v3, in0=rank_sb, in1=s_e.unsqueeze(1).to_broadcast([128, NT, 16]), op=ALU.add),
                  r=allr + RS, w=RB)
            P.dve(lambda e: e.scalar_tensor_tensor(out=v_, in0=v_, scalar=1.0, in1=sl2, op0=ALU.add, op1=ALU.mult), r=RB + allr, w=RB)
            P.dve(lambda e: e.tensor_reduce(out=i0, in_=v3, axis=AX.X, op=ALU.max), r=RB, w=RB)
            P.dve(lambda e: e.tensor_tensor(out=mk3, in0=v3, in1=i0.unsqueeze(2).to_broadcast([128, NT, 16]), op=ALU.is_equal),
                  r=RB, w=RB)
            P.dve(lambda e: e.tensor_tensor(out=tm, in0=mk, in1=cb2, op=ALU.mult), r=RB + allr, w=RB)
            P.dve(lambda e: e.tensor_reduce(out=w0, in_=tm3, axis=AX.X, op=ALU.add), r=RB, w=RB)
            P.dve(lambda e: e.tensor_tensor(out=tm, in0=mk, in1=v_, op=ALU.mult), r=RB, w=RB)
            P.dve(lambda e: e.tensor_tensor(out=v2, in0=v_, in1=tm, op=ALU.subtract), r=RB, w=RB)
            P.dve(lambda e: e.tensor_reduce(out=i1, in_=v23, axis=AX.X, op=ALU.max), r=RB, w=RB)
            P.dve(lambda e: e.tensor_tensor(out=mk3, in0=v23, in1=i1.unsqueeze(2).to_broadcast([128, NT, 16]), op=ALU.is_equal),
                  r=RB, w=RB)
            P.dve(lambda e: e.tensor_tensor(out=tm, in0=mk, in1=cb2, op=ALU.mult), r=RB + allr, w=RB)
            P.dve(lambda e: e.tensor_reduce(out=w1, in_=tm3, axis=AX.X, op=ALU.add), r=RB, w=RB)
            P.dve(lambda e: e.tensor_scalar(out=idx_i[:, :, 0], in0=i0, scalar1=-1.0, scalar2=None, op0=ALU.add), r=RB, w=[b_idx])
            P.dve(lambda e: e.tensor_scalar(out=idx_i[:, :, 1], in0=i1, scalar1=-1.0, scalar2=None, op0=ALU.add), r=RB, w=[b_idx])
            P.dve(lambda e: e.tensor_copy(out=wts_sb[:, :, 0], in_=w0), r=RB, w=[b_idx])
            P.dve(lambda e: e.tensor_copy(out=wts_sb[:, :, 1], in_=w1), r=RB, w=[b_idx])
            xsf = [ar.alloc([D], F32) for _ in range(2)]
            xsb = [ar.alloc([D], BF16) for _ in range(2)]
            b_xsf = [Buf(), Buf()]
            b_xsb = [Buf(), Buf()]
            for t in range(NT):
                s = t % 2
                P.dma('sp', lambda e, t=t, s=s: e.dma_start(out=xsf[s], in_=xres_d[t * 128:(t + 1) * 128, :]),
                      r=[b_xres[t]], w=[b_xsf[s]])
                P.act(lambda e, s=s: e.copy(out=xsb[s], in_=xsf[s]), r=[b_xsf[s]], w=[b_xsb[s]])
                for kk in range(2):
                    P.dma('pool', lambda e, t=t, s=s, kk=kk: e.indirect_dma_start(
                        out=xs_d[:, :], out_offset=bass.IndirectOffsetOnAxis(ap=idx_i[:, t, kk:kk + 1], axis=0),
                        in_=xsb[s], in_offset=None), r=[b_xsb[s], b_idx, b_xs_w] + b_xs_z)
            P.op('sp', lambda e: e.nop(), w=[b_xs_w, b_xs])
            P.barrier()

            if l >= 1 and '4' in SKIP1:
                return
            ar.release(base_mark)
            wgs = [ar.alloc([8, DE], BF16) for _ in range(2)]
            wus = [ar.alloc([8, DE], BF16) for _ in range(2)]
            wds = [ar.alloc([4, D], BF16) for _ in range(2)]
            b_wg = [Buf(), Buf()]
            b_wu = [Buf(), Buf()]
            b_wd = [Buf(), Buf()]
            ln2g_sb = ar.alloc([D], F32)
            ln2b_sb = ar.alloc([D], F32)
            b_c4 = [Buf(), Buf()]
            P.dma('sp', lambda e: e.dma_start(out=ln2g_sb, in_=ln2g_d[l]), w=[b_c4[0]])
            P.dma('sp', lambda e: e.dma_start(out=ln2b_sb, in_=ln2b_d[l]), w=[b_c4[1]])
            xst = [ar.alloc([4, D], BF16) for _ in range(2)]
            b_xst = [Buf(), Buf()]
            xTs = [ar.alloc([8, 512], BF16) for _ in range(2)]
            b_xTs = [[Buf() for _ in range(4)] for _ in range(2)]
            sg = [ar.alloc([512], F32) for _ in range(2)]
            b_sg = [Buf(), Buf()]
            hT = [ar.alloc([4, 512], BF16) for _ in range(2)]
            b_hT = [[Buf() for _ in range(4)] for _ in range(2)]
            NYS = 3
            yst = [ar.alloc([D], F32) for _ in range(NYS)]
            b_yst = [Buf() for _ in range(NYS)]
            gu_i = 0
            yb_i = 0
            ys_i = 0
            tp_i = 0
            wg_rows = wgb_d[l].rearrange("e p m -> (e p) m")
            wu_rows = wub_d[l].rearrange("e p m -> (e p) m")
            wd_rows = wdb_d[l].rearrange("e p m -> (e p) m")

            def stageW(j):
                wb = j % 2
                io = bass.IndirectOffsetOnAxis(ap=idxw[:, j:j + 1], axis=0)
                P.dma('pool', lambda e, wb=wb, io=io: e.indirect_dma_start(
                    out=wgs[wb].rearrange("p k n -> p (k n)"), out_offset=None, in_=wg_rows[:, :], in_offset=io),
                    r=[b_ej, b_wc], w=[b_wg[wb]])
                P.dma('pool', lambda e, wb=wb, io=io: e.indirect_dma_start(
                    out=wus[wb].rearrange("p k n -> p (k n)"), out_offset=None, in_=wu_rows[:, :], in_offset=io),
                    r=[b_ej, b_wc], w=[b_wu[wb]])
                P.dma('pool', lambda e, wb=wb, io=io: e.indirect_dma_start(
                    out=wds[wb].rearrange("p k n -> p (k n)"), out_offset=None, in_=wd_rows[:, :], in_offset=io),
                    r=[b_ej, b_wc], w=[b_wd[wb]])

            def stageXload(j):
                wb = j % 2
                P.dma('sp', lambda e, j=j, wb=wb: e.dma_start(
                    out=xst[wb], in_=xs_d[j * 512:(j + 1) * 512, :].rearrange("(a p) d -> p a d", p=128)),
                    r=[b_xs], w=[b_xst[wb]])

            tpc = [0]

            def stageT(j):
                wb = j % 2
                for a in range(4):
                    bk = 4 + (tpc[0] % 2)
                    tpc[0] += 1
                    pb = bank_bf(bk)
                    for k in range(8):
                        P.pe(lambda e, a=a, k=k, wb=wb, pb=pb: e.transpose(out=pb[:, k * 128:(k + 1) * 128],
                                                                         in_=xst[wb][:, a, k * 128:(k + 1) * 128], identity=idb),
                             r=[b_xst[wb], b_idb], w=[bb[bk]])
                    if a % 2 == 0:
                        P.act(lambda e, a=a, wb=wb, pb=pb: e.copy(out=xTs[wb][:, :, a * 128:(a + 1) * 128],
                                                                  in_=pb.rearrange("p (k t) -> p k t", t=128)),
                              r=[bb[bk]], w=[b_xTs[wb][a]])
                    else:
                        P.dve(lambda e, a=a, wb=wb, pb=pb: e.tensor_copy(out=xTs[wb][:, :, a * 128:(a + 1) * 128],
                                                                         in_=pb.rearrange("p (k t) -> p k t", t=128)),
                              r=[bb[bk]], w=[b_xTs[wb][a]])

            guc = [0]

            def stageGU(j):
                wb = j % 2
                hb = j % 2
                for c in range(4):
                    bg = (guc[0] % 2) * 2
                    guc[0] += 1
                    for k in range(8):
                        P.pe(lambda e, k=k, c=c, wb=wb, bg=bg: e.matmul(
                            banks[bg][:, :], lhsT=wgs[wb][:, k, c * 128:(c + 1) * 128],
                            rhs=xTs[wb][:, k, :], start=(k == 0), stop=(k == 7)),
                            r=[b_wg[wb]] + b_xTs[wb], w=[bb[bg]])
                    for k in range(8):
                        P.pe(lambda e, k=k, c=c, wb=wb, bg=bg: e.matmul(
                            banks[bg + 1][:, :], lhsT=wus[wb][:, k, c * 128:(c + 1) * 128],
                            rhs=xTs[wb][:, k, :], start=(k == 0), stop=(k == 7)),
                            r=[b_wu[wb]] + b_xTs[wb], w=[bb[bg + 1]])
                    sgi = guc[0] % 2
                    P.act(lambda e, bg=bg, sgi=sgi: e.activation(out=sg[sgi], in_=banks[bg][:, :], func=AF.Silu),
                          r=[bb[bg]], w=[b_sg[sgi]])
                    P.dve(lambda e, bg=bg, sgi=sgi, hb=hb, c=c: e.tensor_tensor(
                        out=hT[hb][:, c, :], in0=sg[sgi], in1=banks[bg + 1][:, :], op=ALU.mult),
                        r=[b_sg[sgi], bb[bg + 1]], w=[b_hT[hb][c]])

            ybc = [0, 0]

            def stageY(j):
                wb = j % 2
                hb = j % 2
                for a in range(4):
                    ysb = ybc[1] % NYS
                    ybc[1] += 1
                    for nh in range(2):
                        yb = 6 + (ybc[0] % 2)
                        ybc[0] += 1
                        for c in range(4):
                            P.pe(lambda e, c=c, a=a, nh=nh, hb=hb, wb=wb, yb=yb: e.matmul(
                                banks[yb][:, :], lhsT=hT[hb][:, c, a * 128:(a + 1) * 128],
                                rhs=wds[wb][:, c, nh * 512:(nh + 1) * 512], start=(c == 0), stop=(c == 3)),
                                r=[b_hT[hb][c], b_wd[wb]], w=[bb[yb]])
                        if nh == 0:
                            P.act(lambda e, ysb=ysb, yb=yb: e.copy(out=yst[ysb][:, 0:512], in_=banks[yb][:, :]),
                                  r=[bb[yb]], w=[b_yst[ysb]])
                        else:
                            P.dve(lambda e, ysb=ysb, yb=yb: e.tensor_copy(out=yst[ysb][:, 512:1024], in_=banks[yb][:, :]),
                                  r=[bb[yb]], w=[b_yst[ysb]])
                    row0 = (j * 4 + a) * 128
                    P.dma('sp', lambda e, ysb=ysb, row0=row0: e.dma_start(out=ys_d[row0:row0 + 128, :], in_=yst[ysb]),
                          r=[b_yst[ysb], b_ys_w])

            stageW(0)
            stageXload(0)
            stageT(0)
            for j in range(NTILE):
                if j + 1 < NTILE:
                    stageW(j + 1)
                    stageXload(j + 1)
                stageGU(j)
                if j + 1 < NTILE:
                    stageT(j + 1)
                stageY(j)
            P.op('sp', lambda e: e.nop(), w=[b_ys_w, b_ys])
            NB5 = 4
            g0t = [ar.alloc([D], F32) for _ in range(NB5)]
            g1t = [ar.alloc([D], F32) for _ in range(NB5)]
            x4 = [ar.alloc([D], F32) for _ in range(NB5)]
            st4 = [ar.alloc([2, 6], F32) for _ in range(NB5)]
            mv4 = [ar.alloc([2], F32) for _ in range(NB5)]
            o4 = [ar.alloc([D], F32) for _ in range(NB5)]
            B4 = [{k: Buf() for k in ('g0', 'g1', 'x', 'st', 'mv', 'o')} for _ in range(NB5)]
            def p5_load(t):
                s = t % NB5
                B = B4[s]
                P.dma('pool', lambda e, t=t, s=s: e.indirect_dma_start(
                    out=g0t[s], out_offset=None, in_=ys_d[:, :],
                    in_offset=bass.IndirectOffsetOnAxis(ap=idx_i[:, t, 0:1], axis=0)), r=[b_ys, b_idx], w=[B['g0']])
                P.dma('pool', lambda e, t=t, s=s: e.indirect_dma_start(
                    out=g1t[s], out_offset=None, in_=ys_d[:, :],
                    in_offset=bass.IndirectOffsetOnAxis(ap=idx_i[:, t, 1:2], axis=0)), r=[b_ys, b_idx], w=[B['g1']])
                P.dma('sp', lambda e, t=t, s=s: e.dma_start(out=x4[s], in_=xres_d[t * 128:(t + 1) * 128, :]),
                      r=[b_xres[t]], w=[B['x']])

            def p5_compute(t):
                s = t % NB5
                B = B4[s]
                P.act(lambda e, t=t, s=s: e.activation(out=g0t[s], in_=g0t[s], func=AF.Copy, scale=wts_sb[:, t, 0:1]),
                      r=[B['g0'], b_idx], w=[B['g0']])
                P.dve(lambda e, t=t, s=s: e.scalar_tensor_tensor(out=g0t[s], in0=g1t[s], scalar=wts_sb[:, t, 1:2], in1=g0t[s],
                                                                  op0=ALU.mult, op1=ALU.add),
                      r=[B['g0'], B['g1'], b_idx], w=[B['g0']])
                P.dve(lambda e, s=s: e.scalar_tensor_tensor(out=x4[s], in0=x4[s], scalar=ALPHA, in1=g0t[s],
                                                            op0=ALU.mult, op1=ALU.add),
                      r=[B['x'], B['g0']], w=[B['x']])
                ln_tile(P, x4[s], B['x'], st4[s], B['st'], mv4[s], B['mv'], ln2g_sb, b_c4[0], ln2b_sb, b_c4[1],
                        o4[s], B['o'], mul_eng='act_dve')
                if l == depth - 1:
                    P.dma('sp', lambda e, t=t, s=s: e.dma_start(out=out_d[t * 128:(t + 1) * 128, :], in_=o4[s]),
                          r=[B['o'], b_out_done])
                else:
                    P.dma('sp', lambda e, t=t, s=s: e.dma_start(out=xres_d[t * 128:(t + 1) * 128, :], in_=o4[s]),
                          r=[B['o'], B['x']], w=[b_xres[t]])

            LA5 = 2
            for t in range(min(LA5, NT)):
                p5_load(t)
            for t in range(NT):
                if t + LA5 < NT:
                    p5_load(t + LA5)
                p5_compute(t)
            P.barrier()
            if dbg and l == 0 and depth > 1:
                P.dma('sp', lambda e: e.dma_start(out=dbg2_d[:, :], in_=xres_d[:, :]), r=b_xres + [b_out_done])
                P.barrier()
        for l_ in range(depth if NLAYERS_RUN is None else NLAYERS_RUN):
            do_layer(l_)
        P.op('sp', lambda e: e.nop(), w=[b_out_done])
        P.emit(st)
    return nc, P, ar


def ln_stats(P, y, b_y, st, b_st, mv, b_mv):
    for hh in range(2):
        P.dve(lambda e, hh=hh: e.bn_stats(out=st[:, hh, :], in_=y[:, hh * 512:(hh + 1) * 512]), r=[b_y], w=[b_st])
    P.dve(lambda e: e.bn_aggr(out=mv, in_=st.rearrange("p a b -> p (a b)")), r=[b_st], w=[b_mv])


def ln_apply(P, y, b_y, mv, b_mv, g_sb, b_g, b_sb, b_b, out, b_out):
    P.act(lambda e: e.activation(out=mv[:, 1:2], in_=mv[:, 1:2], func=AF.Sqrt, bias=LN_EPS, scale=1.0), r=[b_mv], w=[b_mv])
    P.dve(lambda e: e.reciprocal(out=mv[:, 1:2], in_=mv[:, 1:2]), r=[b_mv], w=[b_mv])
    P.dve(lambda e: e.scalar_tensor_tensor(out=out, in0=y, scalar=mv[:, 0:1], in1=g_sb, op0=ALU.subtract, op1=ALU.mult),
          r=[b_y, b_mv, b_g], w=[b_out])
    P.dve(lambda e: e.scalar_tensor_tensor(out=out, in0=out, scalar=mv[:, 1:2], in1=b_sb, op0=ALU.mult, op1=ALU.add),
          r=[b_out, b_mv, b_b], w=[b_out])


def ln_tile(P, y, b_y, st, b_st, mv, b_mv, g_sb, b_g, b_sb, b_b, out, b_out, mul_eng='pool'):
    ln_stats(P, y, b_y, st, b_st, mv, b_mv)
    ln_apply(P, y, b_y, mv, b_mv, g_sb, b_g, b_sb, b_b, out, b_out)


def router_tile(P, lg_ps, b_ps, br_sb, b_br, rl, b_rl, comb, b_comb, sel, b_sel):
    L = rl[:, 0:20]
    mg = rl[:, 20:21]
    sgm = rl[:, 21:22]
    gmask = rl[:, 22:26]
    m1 = rl[:, 26:30]
    m2 = rl[:, 30:34]
    eq = rl[:, 34:50]
    ex = rl[:, 50:54]
    den = rl[:, 54:58]
    gm1 = rl[:, 58:62]
    le = L[:, 4:20]
    le3 = le.rearrange("p (g e) -> p g e", e=4)
    eq3 = eq.rearrange("p (g e) -> p g e", e=4)
    R = [b_rl]
    P.dve(lambda e: e.tensor_tensor(out=L, in0=lg_ps, in1=br_sb, op=ALU.add), r=[b_ps, b_br], w=R)
    P.dve(lambda e: e.tensor_reduce(out=mg, in_=L[:, 0:4], axis=AX.X, op=ALU.max), r=R, w=R)
    P.dve(lambda e: e.tensor_scalar(out=gm1, in0=L[:, 0:4], scalar1=mg, scalar2=None, op0=ALU.is_ge), r=R, w=R)
    P.dve(lambda e: e.tensor_scalar(out=ex, in0=L[:, 0:4], scalar1=mg, scalar2=None, op0=ALU.subtract), r=R, w=R)
    P.act(lambda e: e.activation(out=ex, in_=ex, func=AF.Exp, accum_out=sgm), r=R, w=R)
    P.dve(lambda e: e.reciprocal(out=sgm, in_=sgm), r=R, w=R)
    P.dve(lambda e: e.tensor_scalar(out=gmask, in0=gm1, scalar1=sgm, scalar2=None, op0=ALU.mult), r=R, w=R)
    P.dve(lambda e: e.tensor_reduce(out=m1, in_=le3, axis=AX.X, op=ALU.max), r=R, w=R)
    m1b = m1.unsqueeze(2).to_broadcast([128, 4, 4])
    P.dve(lambda e: e.tensor_tensor(out=eq3, in0=le3, in1=m1b, op=ALU.is_ge), r=R, w=R)
    P.dve(lambda e: e.scalar_tensor_tensor(out=eq, in0=eq, scalar=-1e9, in1=le, op0=ALU.mult, op1=ALU.add), r=R, w=R)
    P.dve(lambda e: e.tensor_reduce(out=m2, in_=eq3, axis=AX.X, op=ALU.max), r=R, w=R)
    m2b = m2.unsqueeze(2).to_broadcast([128, 4, 4])
    P.dve(lambda e: e.tensor_tensor(out=eq3, in0=le3, in1=m2b, op=ALU.is_ge), r=R, w=R)
    gm1b = gm1.unsqueeze(2).to_broadcast([128, 4, 4])
    P.dve(lambda e: e.tensor_tensor(out=sel.rearrange("p (g e) -> p g e", e=4), in0=eq3, in1=gm1b, op=ALU.mult),
          r=R, w=[b_sel])
    P.dve(lambda e: e.tensor_tensor(out=le3, in0=le3, in1=m1b, op=ALU.subtract), r=R, w=R)
    P.act(lambda e: e.activation(out=le, in_=le, func=AF.Exp), r=R, w=R)
    P.dve(lambda e: e.tensor_tensor(out=le, in0=le, in1=eq, op=ALU.mult), r=R, w=R)
    P.dve(lambda e: e.tensor_reduce(out=den, in_=le3, axis=AX.X, op=ALU.add), r=R, w=R)
    P.dve(lambda e: e.reciprocal(out=den, in_=den), r=R, w=R)
    P.dve(lambda e: e.tensor_tensor(out=den, in0=den, in1=gmask, op=ALU.mult), r=R, w=R)
    denb = den.unsqueeze(2).to_broadcast([128, 4, 4])
    P.dve(lambda e: e.tensor_tensor(out=comb.rearrange("p (g e) -> p g e", e=4), in0=le3, in1=denb, op=ALU.mult),
          r=R, w=[b_comb])


def host_consts(S):
    NT = S // 128
    pos = np.arange(S, dtype=np.float32)
    half = 8
    inv_freq = (np.float32(500000.0) ** (-np.arange(half, dtype=np.float32) * np.float32(2.0) / np.float32(16))).astype(np.float32)
    ang = pos[:, None] * inv_freq[None, :]
    cos = np.cos(ang).astype(np.float32)
    sin = np.sin(ang).astype(np.float32)

    def tab(a):
        return np.ascontiguousarray(a.reshape(NT, 128, 8).transpose(1, 0, 2))
    n = np.arange(16)[None, :]
    bi_t = (np.arange(NT) // 2)[:, None]
    gcaus = np.where(n < bi_t, 0.0, -1e30).astype(np.float32)
    gown = np.where(n == bi_t, 0.0, NEG).astype(np.float32)
    gcaus = np.ascontiguousarray(np.broadcast_to(gcaus[None], (128, NT, 16)))
    gown = np.ascontiguousarray(np.broadcast_to(gown[None], (128, NT, 16)))
    kk = np.arange(128)[:, None]
    qq = np.arange(128)[None, :]
    tri = np.where(kk <= qq, 0.0, NEG).astype(np.float32)
    khot = (np.arange(S)[None, :] // 256 == np.arange(16)[:, None]).astype(np.float32)
    ltri = (np.arange(128)[:, None] < np.arange(128)[None, :]).astype(np.float32)
    jstart = np.ascontiguousarray(np.broadcast_to((512.0 * np.arange(32, dtype=np.float32))[None, :, None], (128, 32, 16)))
    wbase = (np.arange(128, dtype=np.float32)[:, None] + 128.0 * np.arange(8, dtype=np.float32)[None, :]).astype(np.float32)
    shift = np.zeros((128, 128), np.float32)
    shift[np.arange(64), np.arange(64) + 64] = 1.0
    return {"c_ident": np.eye(128, dtype=np.float32), "c_cos": tab(cos), "c_sin": tab(sin), "c_gcaus": gcaus,
            "c_gown": gown, "c_tri": tri, "c_khot": khot, "c_shift": shift, "c_ltri": ltri, "c_jstart": jstart, "c_wbase": wbase}


def host_params(inp, depth):
    def f(a):
        return np.ascontiguousarray(np.asarray(a, dtype=np.float32))

    def chunkvec(v):
        return f(np.asarray(v)[:depth].reshape(depth, 4, 128).transpose(0, 2, 1))

    def bcast(v):
        v = np.asarray(v)[:depth]
        return f(np.broadcast_to(v[:, None, :], (depth, 128, v.shape[-1])))

    def blockdiag(w):
        w = np.asarray(w)[:depth]
        o = np.zeros((depth, 4, 128, 128), np.float32)
        for c in range(4):
            o[:, c, 0:64, 0:64] = w[:, 2 * c]
            o[:, c, 64:128, 64:128] = w[:, 2 * c + 1]
        return o
    p = {}
    p["w_in"] = f(inp["w_in"][:depth])
    p["conv_w"] = f(np.asarray(inp["conv_w"])[:depth].reshape(depth, 4, 4, 128).transpose(0, 3, 2, 1))
    p["conv_b"] = chunkvec(inp["conv_b"])
    p["w_rg_a"] = blockdiag(inp["w_rg_a"])
    p["b_rg_a"] = chunkvec(inp["b_rg_a"])
    p["w_rg_i"] = blockdiag(inp["w_rg_i"])
    p["b_rg_i"] = chunkvec(inp["b_rg_i"])
    p["lru_lambda"] = chunkvec(inp["lru_lambda"])
    p["g_attn_norm"] = bcast(inp["g_attn_norm"])
    p["g_rec_norm"] = chunkvec(inp["g_rec_norm"])
    p["w_out"] = f(inp["w_out"][:depth])
    p["ln1_g"] = bcast(inp["ln1_g"])
    p["ln1_b"] = bcast(inp["ln1_b"])
    p["w_router"] = f(np.concatenate([np.asarray(inp["w_router_group"])[:depth], np.asarray(inp["w_router_expert"])[:depth]], axis=-1))
    p["b_router"] = bcast(np.concatenate([np.asarray(inp["b_router_group"])[:depth], np.asarray(inp["b_router_expert"])[:depth]], axis=-1))
    p["w_gate"] = f(inp["w_gate"][:depth])
    p["w_up"] = f(inp["w_up"][:depth])
    p["w_down"] = f(inp["w_down"][:depth])
    p["ln2_g"] = bcast(inp["ln2_g"])
    p["ln2_b"] = bcast(inp["ln2_b"])
    return p


_CACHE = {}


def kernel(**inputs):
    x = np.asarray(inputs["x"], dtype=np.float32)
    B, S, _ = x.shape
    depth = int(np.asarray(inputs["w_in"]).shape[0])
    key = (S, depth)
    if key not in _CACHE:
        _CACHE[key] = build(S, depth)[0]
    nc = _CACHE[key]
    shared = host_params(inputs, depth)
    shared.update(host_consts(S))
    in_maps = []
    for b in range(B):
        m = dict(shared)
        m["x"] = np.ascontiguousarray(x[b])
        in_maps.append(m)
    res = run_bass_kernel_spmd(nc, in_maps, core_ids=list(range(B)))
    return np.stack([np.asarray(r["out"], dtype=np.float32) for r in res.results], axis=0)
```

```python
import numpy as np
from contextlib import ExitStack
import concourse.bass as bass
import concourse.mybir as mybir
from concourse.bass_utils import run_bass_kernel_spmd

F32 = mybir.dt.float32
BF16 = mybir.dt.bfloat16
I32 = mybir.dt.int32
AF = mybir.ActivationFunctionType
ALU = mybir.AluOpType
AX = mybir.AxisListType

D = 1024
NH = 8
HD = 64
NEXP = 16
DE = 512
ALPHA = float((2 * 2) ** 0.25)
LN_EPS = 1e-5
RMS_EPS = 1e-6
NEG = -60000.0
COMPUTE = ('pe', 'act', 'dve', 'pool', 'sp')
NDMASEM = 24


class Buf:
    __slots__ = ('name', 'lw', 'rd_eng', 'rd_dma')

    def __init__(self, name=''):
        self.name = name
        self.lw = None
        self.rd_eng = {}
        self.rd_dma = []


class Op:
    __slots__ = ('idx', 'eng', 'fn', 'deps', 'is_dma', 'dq', 'dsem', 'dtarget', 'sig', 'sigval')

    def __init__(self):
        self.sig = False
        self.sigval = 0
        self.is_dma = False
        self.dq = 0


class Prog:
    def __init__(self, nc):
        self.nc = nc
        self.ops = []
        self.dma_count = {'sp': 0, 'act': 0, 'pool': 0}
        self.last = {}
        self.pending_dma = []

    def op(self, eng, fn, r=(), w=(), dma=False):
        ops = self.ops
        o = Op()
        o.idx = len(ops)
        o.eng = eng
        o.fn = fn
        o.is_dma = dma
        deps = set()
        raw_src = set()
        for b in r:
            if b.lw is not None:
                deps.add(b.lw)
                raw_src.add(b.lw)
        for b in w:
            if b.lw is not None:
                deps.add(b.lw)
            deps.update(b.rd_eng.values())
            deps.update(b.rd_dma)
        best = {}
        final = []
        for d in deps:
            p = ops[d]
            if p.is_dma:
                final.append(d)
                continue
            if p.eng == eng and not dma:
                if eng == 'pe':
                    continue
            if p.eng not in best or best[p.eng] < d:
                best[p.eng] = d
        final.extend(best.values())
        o.deps = final
        for d in final:
            ops[d].sig = True
        if dma:
            o.dq = self.dma_count[eng]
            self.dma_count[eng] += 1
            o.sig = True
            self.pending_dma.append(o.idx)
        else:
            self.last[eng] = o.idx
        for b in r:
            if dma:
                b.rd_dma.append(o.idx)
            else:
                b.rd_eng[eng] = o.idx
        for b in w:
            b.lw = o.idx
            b.rd_eng = {}
            b.rd_dma = []
        ops.append(o)
        return o

    def pe(self, fn, r=(), w=()):
        return self.op('pe', fn, r, w)

    def act(self, fn, r=(), w=()):
        return self.op('act', fn, r, w)

    def dve(self, fn, r=(), w=()):
        return self.op('dve', fn, r, w)

    def pool(self, fn, r=(), w=()):
        return self.op('pool', fn, r, w)

    def dma(self, q, fn, r=(), w=()):
        return self.op(q, fn, r, w, dma=True)

    def barrier(self):
        ops = self.ops
        last = dict(self.last)
        pend = list(self.pending_dma)
        for e in ('pe', 'act', 'dve', 'pool', 'sp'):
            o = Op()
            o.idx = len(ops)
            o.eng = e
            o.fn = lambda eng: eng.nop()
            o.deps = [v for k, v in last.items() if k != e] + pend
            for d in o.deps:
                ops[d].sig = True
            ops.append(o)
            if e != 'sp':
                self.last[e] = o.idx
        self.pending_dma = []

    def emit(self, stack):
        nc = self.nc
        ops = self.ops
        sems = {}
        for e in COMPUTE:
            sems[e] = stack.enter_context(nc.semaphore('s_' + e))
        dsems = {}
        for q in ('sp', 'act', 'pool'):
            if self.dma_count[q]:
                dsems[q] = [stack.enter_context(nc.semaphore('d_%s%d' % (q, i)))
                            for i in range(min(NDMASEM, self.dma_count[q]))]
        cnt = {e: 0 for e in COMPUTE}
        for o in ops:
            if o.is_dma:
                n = len(dsems[o.eng])
                o.dsem = dsems[o.eng][o.dq % n]
                o.dtarget = 16 * (o.dq // n + 1)
            elif o.sig:
                cnt[o.eng] += 1
                o.sigval = cnt[o.eng]
        self.sig_counts = dict(cnt)
        streams = {e: [] for e in ('pe', 'act', 'dve', 'pool', 'sp')}
        for o in ops:
            streams[o.eng].append(o)
        block = stack.enter_context(nc.Block())

        def run(engname, eng):
            waited = {}

            def wait(sem, val):
                k = sem.num
                if waited.get(k, 0) >= val:
                    return
                waited[k] = val
                eng.wait_ge(sem, val)

            for o in streams[engname]:
                for d in o.deps:
                    p = ops[d]
                    if p.is_dma:
                        wait(p.dsem, p.dtarget)
                    else:
                        wait(sems[p.eng], p.sigval)
                if o.is_dma:
                    if o.dtarget > 16:
                        wait(o.dsem, o.dtarget - 16)
                    o.fn(eng).then_inc(o.dsem, 16)
                else:
                    ins = o.fn(eng)
                    if o.sig:
                        ins.then_inc(sems[o.eng], 1)

        @block.sync
        def _(e):
            run('sp', e)

        @block.scalar
        def _(e):
            run('act', e)

        @block.vector
        def _(e):
            run('dve', e)

        @block.gpsimd
        def _(e):
            run('pool', e)

        @block.tensor
        def _(e):
            run('pe', e)


class Arena:
    def __init__(self, ap, total):
        self.ap = ap
        self.total = total
        self.top = 0
        self.peak = 0

    def alloc(self, free_shape, dtype, parts=128):
        n = int(np.prod(free_shape))
        four = dtype in (F32, I32)
        nf = n if four else (n + 1) // 2
        assert self.top + nf <= self.total, ('SBUF arena overflow', self.top, nf, self.total)
        v = self.ap[0:parts, self.top:self.top + nf]
        self.top += nf
        self.peak = max(self.peak, self.top)
        if dtype != F32:
            v = v.bitcast(dtype)
            if (not four) and n % 2:
                v = v[:, 0:n]
        if len(free_shape) == 2:
            v = v.rearrange("p (a b) -> p a b", b=free_shape[1])
        elif len(free_shape) == 3:
            v = v.rearrange("p (a b c) -> p a b c", b=free_shape[1], c=free_shape[2])
        elif len(free_shape) == 4:
            v = v.rearrange("p (a b c d) -> p a b c d", b=free_shape[1], c=free_shape[2], d=free_shape[3])
        return v

    def mark(self):
        return self.top

    def release(self, m):
        self.top = m


ARENA_F32 = 53000
DBG_LAYER = 0
SKIP1 = ''
NLAYERS_RUN = None


def build(S, depth, dbg=False):
    NT = S // 128
    NG = S // 512
    NB = S // 256
    nc = bass.Bass("TRN2", target_bir_lowering=False)

    def din(name, shape, dt=F32):
        return nc.dram_tensor(name, list(shape), dt, kind="ExternalInput").ap()

    def dscr(name, shape, dt):
        return nc.dram_tensor(name, list(shape), dt).ap()

    x_d = din("x", [S, D])
    w_in_d = din("w_in", [depth, D, 2560])
    convw_d = din("conv_w", [depth, 128, 4, 4])
    convb_d = din("conv_b", [depth, 128, 4])
    wa_d = din("w_rg_a", [depth, 4, 128, 128])
    ba_d = din("b_rg_a", [depth, 128, 4])
    wi_d = din("w_rg_i", [depth, 4, 128, 128])
    bi_d = din("b_rg_i", [depth, 128, 4])
    lam_d = din("lru_lambda", [depth, 128, 4])
    gattn_d = din("g_attn_norm", [depth, 128, 512])
    grec_d = din("g_rec_norm", [depth, 128, 4])
    w_out_d = din("w_out", [depth, D, D])
    ln1g_d = din("ln1_g", [depth, 128, D])
    ln1b_d = din("ln1_b", [depth, 128, D])
    wr_d = din("w_router", [depth, D, 20])
    br_d = din("b_router", [depth, 128, 20])
    wg_d = din("w_gate", [depth, NEXP, D, DE])
    wu_d = din("w_up", [depth, NEXP, D, DE])
    wd_d = din("w_down", [depth, NEXP, DE, D])
    ln2g_d = din("ln2_g", [depth, 128, D])
    ln2b_d = din("ln2_b", [depth, 128, D])
    ident_d = din("c_ident", [128, 128])
    cos_d = din("c_cos", [128, NT, 8])
    sin_d = din("c_sin", [128, NT, 8])
    gcaus_d = din("c_gcaus", [128, NT, 16])
    gown_d = din("c_gown", [128, NT, 16])
    tri_d = din("c_tri", [128, 128])
    khot_d = din("c_khot", [16, S])
    shift_d = din("c_shift", [128, 128])
    ltri_d = din("c_ltri", [128, 128])
    wbase_d = din("c_wbase", [128, 8])
    jstart_d = din("c_jstart", [128, 32, 16])
    out_d = nc.dram_tensor("out", [S, D], F32, kind="ExternalOutput").ap()
    dbg_d = None
    if dbg:
        dbg_d = nc.dram_tensor("dbg_x1", [S, D], F32, kind="ExternalOutput").ap()
        dbg2_d = nc.dram_tensor("dbg_x2", [S, D], F32, kind="ExternalOutput").ap()

    xres_d = dscr("xres", [S, D], F32)
    qTa_d = dscr("qTa", [NH, 80, S], BF16)
    kT_d = dscr("kT", [NH, 64, S], BF16)
    recT_d = dscr("recT", [4, 128, S], BF16)
    NSLOT = 2 * S + 16 * 512
    NTILE = NSLOT // 512
    assert NTILE <= 32
    xs_d = dscr("xs", [NSLOT, D], BF16)
    x1b_d = dscr("x1b", [S, D], BF16)
    wgb_d = [dscr("wgb%d" % i, [NEXP, 128, 8 * DE], BF16) for i in range(depth)]
    wub_d = [dscr("wub%d" % i, [NEXP, 128, 8 * DE], BF16) for i in range(depth)]
    wdb_d = [dscr("wdb%d" % i, [NEXP, 128, 4 * D], BF16) for i in range(depth)]
    ys_d = dscr("ys", [NSLOT, D], F32)

    P = Prog(nc)
    with ExitStack() as st:
        arena_t = st.enter_context(nc.sbuf_tensor("arena", [128, ARENA_F32], F32))
        ar = Arena(arena_t, ARENA_F32)
        banks = [st.enter_context(nc.psum_tensor("bank%d" % i, [128, 512], F32)) for i in range(8)]
        bb = [Buf('bank%d' % i) for i in range(8)]

        def bank_bf(i):
            return banks[i][:, :].bitcast(BF16)

        identf = ar.alloc([128], F32)
        idb = ar.alloc([128], BF16)
        trif = ar.alloc([128], F32)
        trib = ar.alloc([128], BF16)
        ones_f = ar.alloc([128], F32)
        b_identf, b_idb, b_trif, b_trib, b_ones = Buf(), Buf(), Buf(), Buf(), Buf()
        P.dma('sp', lambda e: e.dma_start(out=identf, in_=ident_d[:, :]), w=[b_identf])
        P.dma('sp', lambda e: e.dma_start(out=trif, in_=tri_d[:, :]), w=[b_trif])
        P.dve(lambda e: e.tensor_copy(out=idb, in_=identf), r=[b_identf], w=[b_idb])
        P.dve(lambda e: e.tensor_copy(out=trib, in_=trif), r=[b_trif], w=[b_trib])
        P.dve(lambda e: e.memset(ones_f, 1.0), w=[b_ones])
        shiftf = ar.alloc([128], F32)
        shiftb = ar.alloc([128], BF16)
        b_shiftf, b_shift = Buf(), Buf()
        P.dma('sp', lambda e: e.dma_start(out=shiftf, in_=shift_d[:, :]), w=[b_shiftf])
        P.dve(lambda e: e.tensor_copy(out=shiftb, in_=shiftf), r=[b_shiftf], w=[b_shift])
        comb_sb = ar.alloc([NT, 16], F32)
        b_comb = [Buf() for _ in range(NT)]
        ltrif = ar.alloc([128], F32)
        ltrib = ar.alloc([128], BF16)
        onesb = ar.alloc([128], BF16)
        b_ltrif, b_ltri, b_onesb = Buf(), Buf(), Buf()
        P.dma('sp', lambda e: e.dma_start(out=ltrif, in_=ltri_d[:, :]), w=[b_ltrif])
        P.dve(lambda e: e.tensor_copy(out=ltrib, in_=ltrif), r=[b_ltrif], w=[b_ltri])
        P.dve(lambda e: e.memset(onesb, 1.0), w=[b_onesb])
        lg_sb = ar.alloc([NT, 20], F32)
        b_lg = [Buf() for _ in range(NT)]
        sel_sb = ar.alloc([NT, 16], BF16)
        b_sel = [Buf() for _ in range(NT)]
        rank_sb = ar.alloc([NT, 16], F32)
        b_rank = [Buf() for _ in range(NT)]
        carry_sb = ar.alloc([16], F32)
        b_carry = Buf()
        idx_i = ar.alloc([NT, 2], I32)
        wts_sb = ar.alloc([NT, 2], F32)
        b_idx = Buf()
        ej_i = ar.alloc([32], I32)
        b_ej = Buf()
        wbase_sb = ar.alloc([8], F32)
        b_wbase = Buf()
        P.dma('sp', lambda e: e.dma_start(out=wbase_sb, in_=wbase_d[:, :]), w=[b_wbase])
        idxw = ar.alloc([32], I32)
        ejc = ar.alloc([32], F32)
        ejs = ar.alloc([32], F32)
        base_mark = ar.mark()
        vsb = ar.alloc([NT, NH, 65], BF16)
        b_v = [Buf('v%d' % t) for t in range(NT)]
        v_mark = ar.mark()
        attn_sb = ar.alloc([NT, 512], BF16)
        b_attn = [[Buf() for _ in range(NH)] for _ in range(NT)]
        mid_mark = ar.mark()
        b_out_done = Buf('out_done')
        b_xres = [Buf() for _ in range(NT)]
        b_qTa = [[Buf() for _ in range(NG)] for _ in range(NH)]
        b_kT = [[Buf() for _ in range(NG)] for _ in range(NH)]
        b_recT = [Buf() for _ in range(NG)]
        b_xs_w, b_xs, b_ys_w, b_ys = Buf(), Buf(), Buf(), Buf()
        b_x1b_d = [Buf() for _ in range(NT)]
        b_wc_w, b_wc = Buf(), Buf()
        b_xs_z = [Buf() for _ in range(NSLOT // 1024)]

        def do_layer(l):
            xsrc = x_d if l == 0 else xres_d
            xdst2 = out_d if l == depth - 1 else xres_d

            if l >= 1 and '1' in SKIP1:
                return
            ar.release(v_mark)
            w_in_sb = ar.alloc([8, 2560], BF16)
            b_wink = [[Buf(), Buf()] for _ in range(8)]
            for k in range(8):
                for hh in range(2):
                    P.dma('pool', lambda e, hh=hh, k=k: e.dma_start(
                        out=w_in_sb[:, k, hh * 1280:(hh + 1) * 1280],
                        in_=w_in_d[l, k * 128:(k + 1) * 128, hh * 1280:(hh + 1) * 1280]),
                        w=[b_wink[k][hh]])
            cos_sb = ar.alloc([NT, 8], F32)
            sin_sb = ar.alloc([NT, 8], F32)
            gcaus_sb = ar.alloc([NT, 16], F32)
            gown_sb = ar.alloc([NT, 16], F32)
            b_tab = Buf()
            P.dma('sp', lambda e: e.dma_start(out=cos_sb, in_=cos_d[:, :, :]), w=[b_tab])
            b_tab2, b_tab3, b_tab4 = Buf(), Buf(), Buf()
            P.dma('sp', lambda e: e.dma_start(out=sin_sb, in_=sin_d[:, :, :]), w=[b_tab2])
            P.dma('sp', lambda e: e.dma_start(out=gcaus_sb, in_=gcaus_d[:, :, :]), w=[b_tab3])
            P.dma('sp', lambda e: e.dma_start(out=gown_sb, in_=gown_d[:, :, :]), w=[b_tab4])
            convw_sb = ar.alloc([4, 4], F32)
            convb_sb = ar.alloc([4], F32)
            ba_sb = ar.alloc([4], F32)
            bi_sb = ar.alloc([4], F32)
            lam_sb = ar.alloc([4], F32)
            cdec_sb = ar.alloc([4], F32)
            grec_sb = ar.alloc([4], F32)
            wa_sb = ar.alloc([4, 128], BF16)
            wi_sb = ar.alloc([4, 128], BF16)
            b_par = Buf()
            b_wa, b_wi, b_waf, b_wif, b_cdec, b_lam = Buf(), Buf(), Buf(), Buf(), Buf(), Buf()
            b_p = [Buf() for _ in range(6)]
            P.dma('sp', lambda e: e.dma_start(out=convw_sb, in_=convw_d[l]), w=[b_p[0]])
            P.dma('sp', lambda e: e.dma_start(out=convb_sb, in_=convb_d[l]), w=[b_p[1]])
            P.dma('sp', lambda e: e.dma_start(out=ba_sb, in_=ba_d[l]), w=[b_p[2]])
            P.dma('sp', lambda e: e.dma_start(out=bi_sb, in_=bi_d[l]), w=[b_p[3]])
            P.dma('sp', lambda e: e.dma_start(out=lam_sb, in_=lam_d[l]), w=[b_lam])
            P.dma('sp', lambda e: e.dma_start(out=grec_sb, in_=grec_d[l]), w=[b_p[5]])
            P.dma('pool', lambda e: e.dma_start(out=wa_sb, in_=wa_d[l].rearrange("c p n -> p c n")), w=[b_wa])
            P.dma('pool', lambda e: e.dma_start(out=wi_sb, in_=wi_d[l].rearrange("c p n -> p c n")), w=[b_wi])
            P.act(lambda e: e.activation(out=cdec_sb, in_=lam_sb, func=AF.Exp, scale=-1.0), r=[b_lam], w=[b_cdec])
            P.act(lambda e: e.activation(out=cdec_sb, in_=cdec_sb, func=AF.Ln, bias=1.0), r=[b_cdec], w=[b_cdec])
            P.dve(lambda e: e.tensor_scalar(out=cdec_sb, in0=cdec_sb, scalar1=-8.0, scalar2=None, op0=ALU.mult),
                  r=[b_cdec], w=[b_cdec])
            b_par_all = b_p + [b_cdec]

            kmeanBD = ar.alloc([4, 2, 16], BF16)
            b_kmean = Buf()
            P.dve(lambda e: e.memset(kmeanBD, 0.0), w=[b_kmean])

            xbf = ar.alloc([4, D], BF16)
            b_xbf = [Buf() for _ in range(4)]
            oddk = ar.alloc([8], BF16, parts=64)
            b_oddk = Buf()
            xT2 = [ar.alloc([8, 512], BF16) for _ in range(2)]
            b_xT2 = [[Buf() for _ in range(4)] for _ in range(2)]
            qb = ar.alloc([4, NH, 80], BF16)
            b_qb = [Buf() for _ in range(4)]
            kb = ar.alloc([4, NH, 64], BF16)
            b_kb = [Buf() for _ in range(4)]
            qTp = ar.alloc([4, 512], BF16)
            b_qTp = [Buf() for _ in range(4)]
            kTh = ar.alloc([NH, 512], BF16, parts=64)
            b_kTh = [Buf() for _ in range(4)]
            qTa = ar.alloc([NH, 512], BF16, parts=80)
            b_qTah = [Buf() for _ in range(4)]
            ksum = ar.alloc([NH, 2], F32, parts=64)
            b_ksum = Buf()
            rt = [ar.alloc([NH, 8], F32) for _ in range(4)]
            b_rt = [Buf() for _ in range(4)]
            gm = ar.alloc([4, NH], F32)
            b_gm = Buf()
            xr_sb = ar.alloc([4, 515], F32)
            b_xr = [Buf() for _ in range(4)]
            P.dve(lambda e: e.memset(xr_sb, 0.0), w=b_xr)
            hst = ar.alloc([4, 1], F32)
            b_h = [Buf() for _ in range(4)]
            XC = [ar.alloc([512], F32) for _ in range(3)]
            b_XC = [Buf() for _ in range(3)]
            RRb = [ar.alloc([512], F32) for _ in range(2)]
            b_RR = [Buf() for _ in range(2)]
            IIb = [ar.alloc([512], F32) for _ in range(2)]
            b_II = [Buf() for _ in range(2)]
            AAb = [ar.alloc([512], F32) for _ in range(2)]
            b_AA = [Buf() for _ in range(2)]
            HHb = ar.alloc([512], F32)
            b_HH = Buf()
            RQb = ar.alloc([512], F32)
            b_RQ = Buf()
            GEb = [ar.alloc([512], BF16) for _ in range(5)]
            b_GE = [Buf() for _ in range(5)]
            g0 = ar.alloc([4, NH, 16], F32)
            g1 = ar.alloc([4, NH, 16], F32)
            ge = ar.alloc([4, NH, 16], F32)
            b_g0, b_g1, b_ge = Buf(), Buf(), Buf()
            xcb = [ar.alloc([512], BF16) for _ in range(2)]
            b_xcb = [Buf(), Buf()]
            recf = ar.alloc([4, 512], F32)
            b_recf = [Buf() for _ in range(4)]
            recn = ar.alloc([4, 512], BF16)
            b_recn = Buf()
            rstd = ar.alloc([512], F32)
            b_rstd = Buf()
            gsb = ar.alloc([512], F32)
            b_gsb = Buf()

            def p1_xload(g, j):
                t_ = 4 * g + j
                P.dma('pool', lambda e, t_=t_, j=j: e.dma_start(out=xbf[:, j, :], in_=xsrc[t_ * 128:(t_ + 1) * 128, :]),
                      r=[b_xres[t_]] if l > 0 else [], w=[b_xbf[j]])

            def A_tile(g, j):
                xi = xbf
                bxi = b_xbf[j]
                t = 4 * g + j
                xT = xT2[g % 2]
                b_xT = b_xT2[g % 2]
                pb = bank_bf(0)
                for k in range(8):
                    P.pe(lambda e, j=j, k=k, pb=pb, xi=xi: e.transpose(out=pb[:, k * 128:(k + 1) * 128],
                                                                     in_=xi[:, j, k * 128:(k + 1) * 128], identity=idb),
                         r=[bxi, b_idb], w=[bb[0]])
                P.act(lambda e, j=j, pb=pb, xT=xT: e.copy(out=xT[:, :, j * 128:(j + 1) * 128],
                                                         in_=pb.rearrange("p (k t) -> p k t", t=128)),
                      r=[bb[0]], w=[b_xT[j]])
                if g + 1 < NG:
                    p1_xload(g + 1, j)
                for which in range(3):
                    bk = 2 + which
                    for k in range(8):
                        P.pe(lambda e, j=j, k=k, which=which, bk=bk: e.matmul(
                            banks[bk][:, :], lhsT=xT[:, k, j * 128:(j + 1) * 128],
                            rhs=w_in_sb[:, k, which * 512:(which + 1) * 512], start=(k == 0), stop=(k == 7)),
                            r=[b_xT[j]] + (b_wink[k] if which == 2 else [b_wink[k][0]]), w=[bb[bk]])
                for which, dst in ((0, qb), (1, kb)):
                    bk = 2 + which
                    pv = banks[bk][:, :].rearrange("p (h d) -> p h d", d=64)
                    t1 = pv[:, :, 0:8]
                    t2 = pv[:, :, 8:16]
                    cs = cos_sb[:, t, :].unsqueeze(1).to_broadcast([128, NH, 8])
                    sn = sin_sb[:, t, :].unsqueeze(1).to_broadcast([128, NH, 8])
                    bdst = b_qb[j] if which == 0 else b_kb[j]
                    P.dve(lambda e, t1=t1, cs=cs: e.tensor_tensor(out=rt[0], in0=t1, in1=cs, op=ALU.mult),
                          r=[bb[bk], b_tab], w=[b_rt[0]])
                    P.dve(lambda e, t2=t2, sn=sn: e.tensor_tensor(out=rt[1], in0=t2, in1=sn, op=ALU.mult),
                          r=[bb[bk], b_tab2], w=[b_rt[1]])
                    P.dve(lambda e, t2=t2, cs=cs: e.tensor_tensor(out=rt[2], in0=t2, in1=cs, op=ALU.mult),
                          r=[bb[bk], b_tab], w=[b_rt[2]])
                    P.dve(lambda e, t1=t1, sn=sn: e.tensor_tensor(out=rt[3], in0=t1, in1=sn, op=ALU.mult),
                          r=[bb[bk], b_tab2], w=[b_rt[3]])
                    P.dve(lambda e, dst=dst, j=j: e.tensor_tensor(out=dst[:, j, :, 0:8], in0=rt[0], in1=rt[1], op=ALU.subtract),
                          r=[b_rt[0], b_rt[1]], w=[bdst])
                    P.dve(lambda e, dst=dst, j=j: e.tensor_tensor(out=dst[:, j, :, 8:16], in0=rt[2], in1=rt[3], op=ALU.add),
                          r=[b_rt[2], b_rt[3]], w=[bdst])
                    P.act(lambda e, dst=dst, j=j, pv=pv: e.copy(out=dst[:, j, :, 16:64], in_=pv[:, :, 16:64]),
                          r=[bb[bk]], w=[bdst])
                pvv = banks[4][:, :].rearrange("p (h d) -> p h d", d=64)
                P.act(lambda e, t=t, pvv=pvv: e.copy(out=vsb[:, t, :, 0:64], in_=pvv), r=[bb[4]], w=[b_v[t]])
                P.pool(lambda e, t=t: e.memset(vsb[:, t, :, 64:65], 1.0), r=[], w=[b_v[t]])
                pb = bank_bf(7)
                for h in range(NH):
                    P.pe(lambda e, j=j, h=h, pb=pb: e.transpose(out=pb[0:64, h * 128:(h + 1) * 128],
                                                              in_=kb[:, j, h, :], identity=idb),
                         r=[b_kb[j], b_idb], w=[bb[7]])
                P.act(lambda e, j=j, pb=pb: e.copy(out=kTh[:, :, j * 128:(j + 1) * 128],
                                                  in_=pb[0:64, :].rearrange("p (h t) -> p h t", t=128)),
                      r=[bb[7]], w=[b_kTh[j]])
                pb6 = bank_bf(6)
                for pr in range(4):
                    for w2 in range(2):
                        P.pe(lambda e, j=j, pr=pr, w2=w2, pb6=pb6: e.transpose(
                            out=pb6[w2 * 64:(w2 + 1) * 64, pr * 128:(pr + 1) * 128],
                            in_=qb[:, j, 2 * pr + w2, 0:64], identity=idb),
                            r=[b_qb[j], b_idb], w=[bb[6]])
                P.act(lambda e, j=j, pb6=pb6: e.copy(out=qTp[:, :, j * 128:(j + 1) * 128],
                                                    in_=pb6[:, 0:512].rearrange("p (h t) -> p h t", t=128)),
                      r=[bb[6]], w=[b_qTp[j]])

            def B_grp(g):
                for h in range(NH):
                    P.dma('sp', lambda e, h=h, g=g: e.dma_start(out=kT_d[h, :, g * 512:(g + 1) * 512], in_=kTh[:, h, :]),
                          r=b_kTh, w=[b_kT[h][g]])
                P.dve(lambda e: e.tensor_reduce(out=ksum, in_=kTh.rearrange("p h (b t) -> p h b t", t=256),
                                                 axis=AX.X, op=ALU.add), r=b_kTh, w=[b_ksum])
                ks4 = ksum.rearrange("p (pr w) b -> p pr w b", w=2)
                P.dve(lambda e, g=g, ks4=ks4: e.tensor_scalar(
                    out=kmeanBD[0:64, :, 0, 2 * g:2 * g + 2], in0=ks4[:, :, 0, :], scalar1=1.0 / 256.0, scalar2=None,
                    op0=ALU.mult), r=[b_ksum], w=[b_kmean])
                odd_bf = oddk
                P.dve(lambda e, ks4=ks4, odd_bf=odd_bf: e.tensor_scalar(
                    out=odd_bf.rearrange("p (pr b) -> p pr b", b=2), in0=ks4[:, :, 1, :], scalar1=1.0 / 256.0,
                    scalar2=None, op0=ALU.mult), r=[b_ksum], w=[b_oddk])
                P.pe(lambda e, odd_bf=odd_bf: e.matmul(banks[5][:, 0:8], lhsT=shiftb[0:64, :], rhs=odd_bf,
                                                       start=True, stop=True),
                     r=[b_oddk, b_shift], w=[bb[5]])
                P.act(lambda e, g=g: e.copy(out=kmeanBD[64:128, :, 1, 2 * g:2 * g + 2],
                                            in_=banks[5][64:128, 0:8].rearrange("p (pr b) -> p pr b", b=2)),
                      r=[bb[5]], w=[b_kmean])
                for j in range(4):
                    for pr in range(4):
                        P.pe(lambda e, j=j, pr=pr: e.matmul(
                            banks[5][:, (j * 4 + pr) * 32:(j * 4 + pr + 1) * 32],
                            lhsT=qTp[:, pr, j * 128:(j + 1) * 128],
                            rhs=kmeanBD[:, pr, :, :].rearrange("p w n -> p (w n)"), start=True, stop=True),
                            r=[b_qTp[j], b_kmean], w=[bb[5]])
                P.act(lambda e: e.copy(out=gsb, in_=banks[5][:, :]), r=[bb[5]], w=[b_gsb])
                yield
                gps = gsb.rearrange("p (j h n) -> p j h n", h=NH, n=16)
                gc = gcaus_sb[:, 4 * g:4 * g + 4, :].unsqueeze(2).to_broadcast([128, 4, NH, 16])
                go = gown_sb[:, 4 * g:4 * g + 4, :].unsqueeze(2).to_broadcast([128, 4, NH, 16])
                P.dve(lambda e, gps=gps, gc=gc: e.tensor_tensor(out=g0, in0=gps, in1=gc, op=ALU.add),
                      r=[b_gsb, b_tab3], w=[b_g0])
                P.dve(lambda e: e.tensor_copy(out=g1, in_=g0), r=[b_g0], w=[b_g1])
                for rnd in range(3):
                    P.dve(lambda e: e.tensor_reduce(out=gm, in_=g1, axis=AX.X, op=ALU.max), r=[b_g1], w=[b_gm])
                    P.dve(lambda e: e.tensor_tensor(out=ge, in0=g1, in1=gm.unsqueeze(3).to_broadcast([128, 4, NH, 16]),
                                                     op=ALU.is_ge), r=[b_g1, b_gm], w=[b_ge])
                    P.dve(lambda e: e.scalar_tensor_tensor(
                        out=g1.rearrange("p j h n -> p (j h n)"), in0=ge.rearrange("p j h n -> p (j h n)"),
                        scalar=-1e9, in1=g1.rearrange("p j h n -> p (j h n)"), op0=ALU.mult, op1=ALU.add),
                        r=[b_ge, b_g1], w=[b_g1])
                P.dve(lambda e: e.scalar_tensor_tensor(
                    out=ge.rearrange("p j h n -> p (j h n)"), in0=g0.rearrange("p j h n -> p (j h n)"),
                    scalar=-5e8, in1=g1.rearrange("p j h n -> p (j h n)"), op0=ALU.add, op1=ALU.is_lt),
                    r=[b_g0, b_g1], w=[b_ge])
                for j in range(4):
                    P.dve(lambda e, j=j, go=go: e.tensor_tensor(out=qb[:, j, :, 64:80], in0=ge[:, j, :, :],
                                                                 in1=go[:, j, :, :], op=ALU.mult),
                          r=[b_ge, b_tab4], w=[b_qb[j]])
                yield
                for j in range(4):
                    pb = bank_bf(7)
                    for h in range(NH):
                        P.pe(lambda e, j=j, h=h, pb=pb: e.transpose(out=pb[0:80, h * 128:(h + 1) * 128],
                                                                  in_=qb[:, j, h, :], identity=idb),
                             r=[b_qb[j], b_idb], w=[bb[7]])
                    P.act(lambda e, j=j, pb=pb: e.copy(out=qTa[:, :, j * 128:(j + 1) * 128],
                                                      in_=pb[0:80, :].rearrange("p (h t) -> p h t", t=128)),
                          r=[bb[7]], w=[b_qTah[j]])
                for h in range(NH):
                    P.dma('sp', lambda e, h=h, g=g: e.dma_start(out=qTa_d[h, :, g * 512:(g + 1) * 512], in_=qTa[:, h, :]),
                          r=b_qTah, w=[b_qTa[h][g]])


            def R0(q):
                g, c = divmod(q, 4)
                xT = xT2[g % 2]
                b_xT = b_xT2[g % 2]
                bkx = 2 + (q % 2)
                for k in range(8):
                    P.pe(lambda e, c=c, k=k, bkx=bkx, xT=xT: e.matmul(
                        banks[bkx][:, :], lhsT=w_in_sb[:, k, 1536 + c * 128:1536 + (c + 1) * 128],
                        rhs=xT[:, k, :], start=(k == 0), stop=(k == 7)),
                        r=b_xT + [b_wink[k][1]], w=[bb[bkx]])
                P.act(lambda e, c=c, bkx=bkx: e.copy(out=xr_sb[:, c, 3:515], in_=banks[bkx][:, :]),
                      r=[bb[bkx]], w=[b_xr[c]])
                bkr = 2 + ((q + 1) % 2)
                for k in range(8):
                    P.pe(lambda e, c=c, k=k, bkr=bkr, xT=xT: e.matmul(
                        banks[bkr][:, :], lhsT=w_in_sb[:, k, 2048 + c * 128:2048 + (c + 1) * 128],
                        rhs=xT[:, k, :], start=(k == 0), stop=(k == 7)),
                        r=b_xT + [b_wink[k][1]], w=[bb[bkr]])
                gi = q % 5
                P.act(lambda e, gi=gi, bkr=bkr: e.activation(out=GEb[gi], in_=banks[bkr][:, :], func=AF.Gelu_apprx_tanh),
                      r=[bb[bkr]], w=[b_GE[gi]])

            def R1(q):
                g, c = divmod(q, 4)
                xc = XC[q % 3]
                bxc = b_XC[q % 3]
                P.dve(lambda e, c=c, xc=xc: e.tensor_scalar(
                    out=xc, in0=xr_sb[:, c, 0:512], scalar1=convw_sb[:, c, 0:1], scalar2=convb_sb[:, c:c + 1],
                    op0=ALU.mult, op1=ALU.add), r=[b_xr[c], b_p[0], b_p[1]], w=[bxc])
                for jj in range(1, 4):
                    P.dve(lambda e, c=c, xc=xc, jj=jj: e.scalar_tensor_tensor(
                        out=xc, in0=xr_sb[:, c, jj:jj + 512], scalar=convw_sb[:, c, jj:jj + 1], in1=xc,
                        op0=ALU.mult, op1=ALU.add), r=[b_xr[c], b_p[0], bxc], w=[bxc])
                P.pool(lambda e, c=c: e.tensor_copy(out=xr_sb[:, c, 0:3], in_=xr_sb[:, c, 512:515]),
                       r=[b_xr[c]], w=[b_xr[c]])
                s2 = q % 2
                P.act(lambda e, xc=xc, s2=s2: e.copy(out=xcb[s2], in_=xc), r=[bxc], w=[b_xcb[s2]])

            def R2(q):
                g, c = divmod(q, 4)
                s2 = q % 2
                P.pe(lambda e, c=c, s2=s2: e.matmul(banks[5][:, :], lhsT=wa_sb[:, c, :], rhs=xcb[s2], start=True, stop=True),
                     r=[b_wa, b_xcb[s2]], w=[bb[5]])
                P.act(lambda e, c=c, s2=s2: e.activation(out=RRb[s2], in_=banks[5][:, :], func=AF.Sigmoid,
                                                         bias=ba_sb[:, c:c + 1]), r=[bb[5], b_p[2]], w=[b_RR[s2]])
                P.pe(lambda e, c=c, s2=s2: e.matmul(banks[6][:, :], lhsT=wi_sb[:, c, :], rhs=xcb[s2], start=True, stop=True),
                     r=[b_wi, b_xcb[s2]], w=[bb[6]])
                P.act(lambda e, c=c, s2=s2: e.activation(out=IIb[s2], in_=banks[6][:, :], func=AF.Sigmoid,
                                                         bias=bi_sb[:, c:c + 1]), r=[bb[6], b_p[3]], w=[b_II[s2]])

            def R3a(q):
                g, c = divmod(q, 4)
                s2 = q % 2
                xc = XC[q % 3]
                bxc = b_XC[q % 3]
                P.act(lambda e, c=c, s2=s2: e.activation(out=AAb[s2], in_=RRb[s2], func=AF.Exp, scale=cdec_sb[:, c:c + 1]),
                      r=[b_RR[s2], b_cdec], w=[b_AA[s2]])
                P.act(lambda e, s2=s2: e.activation(out=RRb[s2], in_=AAb[s2], func=AF.Square), r=[b_AA[s2]], w=[b_RR[s2]])
                P.act(lambda e, s2=s2: e.activation(out=RRb[s2], in_=RRb[s2], func=AF.Sqrt, scale=-1.0, bias=1.0),
                      r=[b_RR[s2]], w=[b_RR[s2]])
                P.pool(lambda e, s2=s2, xc=xc: e.tensor_tensor(out=IIb[s2], in0=IIb[s2], in1=xc, op=ALU.mult),
                       r=[b_II[s2], bxc], w=[b_II[s2]])

            def R3d(q):
                s2 = q % 2
                P.dve(lambda e, s2=s2: e.tensor_tensor(out=IIb[s2], in0=IIb[s2], in1=RRb[s2], op=ALU.mult),
                      r=[b_II[s2], b_RR[s2]], w=[b_II[s2]])

            def R4d(q):
                g, c = divmod(q, 4)
                s2 = q % 2
                gi = q % 5
                if g == 0:
                    P.dve(lambda e, s2=s2: e.tensor_tensor_scan(out=HHb, data0=AAb[s2], data1=IIb[s2], initial=0.0,
                                                                op0=ALU.mult, op1=ALU.add),
                          r=[b_AA[s2], b_II[s2]], w=[b_HH])
                else:
                    P.dve(lambda e, s2=s2, c=c: e.tensor_tensor_scan(out=HHb, data0=AAb[s2], data1=IIb[s2],
                                                                     initial=hst[:, c, 0:1], op0=ALU.mult, op1=ALU.add),
                          r=[b_AA[s2], b_II[s2], b_h[c]], w=[b_HH])
                P.pool(lambda e, c=c: e.tensor_copy(out=hst[:, c, 0:1], in_=HHb[:, 511:512]), r=[b_HH], w=[b_h[c]])
                P.dve(lambda e, c=c, gi=gi: e.tensor_tensor(out=recf[:, c, :], in0=HHb, in1=GEb[gi], op=ALU.mult),
                      r=[b_HH, b_GE[gi]], w=[b_recf[c]])

            def R4a(q):
                g, c = divmod(q, 4)
                P.act(lambda e, c=c: e.activation(out=RQb, in_=recf[:, c, :], func=AF.Square), r=[b_recf[c]], w=[b_RQ])

            def R4p(q):
                g, c = divmod(q, 4)
                P.pe(lambda e, c=c: e.matmul(banks[1][:, :], lhsT=ones_f, rhs=RQb, start=(c == 0), stop=(c == 3)),
                     r=[b_ones, b_RQ], w=[bb[1]])
                if c == 3:
                    C_final(g)

            def C_final(g):
                P.act(lambda e: e.activation(out=rstd, in_=banks[1][:, :], func=AF.Sqrt, scale=1.0 / 512.0, bias=RMS_EPS),
                      r=[bb[1]], w=[b_rstd])
                P.dve(lambda e: e.reciprocal(out=rstd, in_=rstd), r=[b_rstd], w=[b_rstd])
                for c in range(4):
                    P.dve(lambda e, c=c: e.scalar_tensor_tensor(out=recn[:, c, :], in0=recf[:, c, :], scalar=grec_sb[:, c:c + 1],
                                                                 in1=rstd, op0=ALU.mult, op1=ALU.mult),
                          r=[b_recf[c], b_rstd, b_p[5]], w=[b_recn])
                P.dma('sp', lambda e, g=g: e.dma_start(out=recT_d[:, :, g * 512:(g + 1) * 512].rearrange("c p t -> p c t"),
                                                      in_=recn), r=[b_recn], w=[b_recT[g]])

            for j in range(4):
                p1_xload(0, j)
            for j in range(4):
                A_tile(0, j)
            NQ = 4 * NG
            bgen = None
            for it in range(NQ + 4):
                if 0 <= it - 3 < NQ:
                    R3a(it - 3)
                if 0 <= it - 4 < NQ:
                    R4d(it - 4)
                if 0 <= it - 3 < NQ:
                    R3d(it - 3)
                if 0 <= it - 4 < NQ:
                    R4a(it - 4)
                if 0 <= it - 2 < NQ:
                    R2(it - 2)
                if 0 <= it - 1 < NQ:
                    R1(it - 1)
                if it < NQ:
                    R0(it)
                if 0 <= it - 4 < NQ:
                    R4p(it - 4)
                gq, m = divmod(it, 4)
                if gq < NG:
                    if m == 0:
                        bgen = B_grp(gq)
                    if bgen is not None:
                        next(bgen, None)
                    if m == 2:
                        for _ in bgen:
                            pass
                        bgen = None
                    if gq + 1 < NG:
                        if m == 2:
                            A_tile(gq + 1, 0)
                            A_tile(gq + 1, 1)
                        if m == 3:
                            A_tile(gq + 1, 2)
                            A_tile(gq + 1, 3)
            P.barrier()

            if l >= 1 and '2' in SKIP1:
                return
            ar.release(mid_mark)
            qh = [ar.alloc([S], BF16, parts=80) for _ in range(2)]
            kh = [ar.alloc([S], BF16, parts=80) for _ in range(2)]
            b_qh = [Buf(), Buf()]
            b_kh = [Buf(), Buf()]
            b_khot = [Buf(), Buf()]
            khot_f = ar.alloc([S], F32, parts=80)
            b_khf = Buf()
            P.dma('sp', lambda e: e.dma_start(out=khot_f[64:80, :], in_=khot_d[:, :]), w=[b_khf])
            for i in range(2):
                P.dve(lambda e, i=i: e.tensor_copy(out=kh[i][64:80, :], in_=khot_f[64:80, :]), r=[b_khf], w=[b_khot[i]])
            zt = ar.alloc([8, D], BF16)
            b_zt = Buf()
            P.dve(lambda e: e.memset(zt, 0.0), w=[b_zt])
            NPT = 4
            pT = [ar.alloc([2, 256], BF16) for _ in range(NPT)]
            b_pT = [Buf() for _ in range(NPT)]
            rcp = [ar.alloc([2], F32) for _ in range(2)]
            b_rcp = [Buf(), Buf()]
            SB = (0, 1, 2, 7)
            OB = ((3, 4), (5, 6))
            units = []
            for h in range(NH):
                for bi in range(NB):
                    for n in range(bi + 1):
                        units.append((h, bi, n))
            cnt_state = {}

            def stageA(u, ui):
                h, bi, n = u
                i = h % 2
                if bi == 0 and n == 0:
                    P.dma('sp', lambda e, h=h, i=i: e.dma_start(out=qh[i], in_=qTa_d[h, :, :]), r=b_qTa[h], w=[b_qh[i]])
                    P.dma('sp', lambda e, h=h, i=i: e.dma_start(out=kh[i][0:64, :], in_=kT_d[h, :, :]), r=b_kT[h], w=[b_kh[i]])
                q0 = bi * 256
                sb_i = SB[ui % 4]
                sps = banks[sb_i][:, :].rearrange("p (t q) -> p t q", q=256)
                if n != bi:
                    for tt in range(2):
                        kt = 2 * n + tt
                        P.pe(lambda e, i=i, kt=kt, tt=tt, sps=sps, q0=q0: e.matmul(
                            sps[:, tt, :], lhsT=kh[i][:, kt * 128:(kt + 1) * 128], rhs=qh[i][:, q0:q0 + 256],
                            start=True, stop=True), r=[b_kh[i], b_khot[i], b_qh[i]], w=[bb[sb_i]])
                else:
                    kt0 = 2 * bi
                    kt1 = 2 * bi + 1
                    P.pe(lambda e, i=i, kt0=kt0, sps=sps, q0=q0: e.matmul(
                        sps[:, 0, :], lhsT=kh[i][:, kt0 * 128:(kt0 + 1) * 128], rhs=qh[i][:, q0:q0 + 256],
                        start=True, stop=False), r=[b_kh[i], b_khot[i], b_qh[i]], w=[bb[sb_i]])
                    P.pe(lambda e, sps=sps: e.matmul(sps[:, 0, 0:128], lhsT=idb, rhs=trib, start=False, stop=True),
                         r=[b_idb, b_trib], w=[bb[sb_i]])
                    P.pe(lambda e, i=i, kt1=kt1, sps=sps, q0=q0: e.matmul(
                        sps[:, 1, 128:256], lhsT=kh[i][:, kt1 * 128:(kt1 + 1) * 128],
                        rhs=qh[i][:, q0 + 128:q0 + 256], start=True, stop=False),
                        r=[b_kh[i], b_khot[i], b_qh[i]], w=[bb[sb_i]])
                    P.pe(lambda e, sps=sps: e.matmul(sps[:, 1, 128:256], lhsT=idb, rhs=trib, start=False, stop=True),
                         r=[b_idb, b_trib], w=[bb[sb_i]])

            def stageB(u, ui):
                h, bi, n = u
                sb_i = SB[ui % 4]
                sps = banks[sb_i][:, :].rearrange("p (t q) -> p t q", q=256)
                pt = pT[ui % NPT]
                bpt = b_pT[ui % NPT]
                if n != bi:
                    P.act(lambda e, pt=pt, sps=sps: e.activation(out=pt, in_=sps, func=AF.Exp, scale=0.125),
                          r=[bb[sb_i]], w=[bpt])
                else:
                    P.act(lambda e, pt=pt, sps=sps: e.activation(out=pt[:, 0, :], in_=sps[:, 0, :], func=AF.Exp, scale=0.125),
                          r=[bb[sb_i]], w=[bpt])
                    P.act(lambda e, pt=pt, sps=sps: e.activation(out=pt[:, 1, 128:256], in_=sps[:, 1, 128:256],
                                                                func=AF.Exp, scale=0.125),
                          r=[bb[sb_i]], w=[bpt])

            def stageC(u, ui):
                h, bi, n = u
                ob = OB[bi % 2]
                pt = pT[ui % NPT]
                bpt = b_pT[ui % NPT]
                nmm = [2 * bi + 1, 2 * bi + 2]
                if n == 0:
                    cnt_state[(h, bi)] = [0, 0]
                cntj = cnt_state[(h, bi)]
                if n != bi:
                    lst = [(jq, tt, 2 * n + tt) for jq in range(2) for tt in range(2)]
                else:
                    lst = [(0, 0, 2 * bi), (1, 0, 2 * bi), (1, 1, 2 * bi + 1)]
                for (jq, tt, kt) in lst:
                    first = (cntj[jq] == 0)
                    cntj[jq] += 1
                    last = (cntj[jq] == nmm[jq])
                    P.pe(lambda e, jq=jq, tt=tt, kt=kt, pt=pt, h=h, first=first, last=last, ob=ob: e.matmul(
                        banks[ob[jq]][:, 0:65], lhsT=pt[:, tt, jq * 128:(jq + 1) * 128],
                        rhs=vsb[:, kt, h, :], start=first, stop=last),
                        r=[bpt, b_v[kt]], w=[bb[ob[jq]]])
                if n == bi:
                    assert cntj == nmm
                    rc = rcp[bi % 2]
                    brc = b_rcp[bi % 2]
                    for jq in range(2):
                        t = 2 * bi + jq
                        P.dve(lambda e, jq=jq, rc=rc, ob=ob: e.reciprocal(out=rc[:, jq:jq + 1], in_=banks[ob[jq]][:, 64:65]),
                              r=[bb[ob[jq]]], w=[brc])
                        P.dve(lambda e, jq=jq, rc=rc, ob=ob, t=t, h=h: e.tensor_scalar(
                            out=attn_sb[:, t, h * 64:(h + 1) * 64], in0=banks[ob[jq]][:, 0:64], scalar1=rc[:, jq:jq + 1],
                            scalar2=None, op0=ALU.mult), r=[bb[ob[jq]], brc], w=[b_attn[t][h]])

            LAG = 2
            NU = len(units)
            casts = []
            for ex in range(NEXP):
                casts.append((wgb_d, wg_d, ex, D))
                casts.append((wub_d, wu_d, ex, D))
                casts.append((wdb_d, wd_d, ex, DE))
            cast_every = max(1, (NU - 8) // len(casts))
            ci = 0
            zi_c = [0]
            zero_every = max(6, (NU - 16) // (NSLOT // 1024))
            for ui in range(NU + LAG):
                if ui < NU:
                    stageA(units[ui], ui)
                    stageB(units[ui], ui)
                if ui >= LAG:
                    stageC(units[ui - LAG], ui - LAG)
                if ui % zero_every == 5 and zi_c[0] < NSLOT // 1024:
                    zi = zi_c[0]
                    zi_c[0] += 1
                    P.dma('sp', lambda e, zi=zi: e.dma_start(
                        out=xs_d[zi * 1024:(zi + 1) * 1024, :].rearrange("(a p) d -> p a d", p=128), in_=zt),
                        r=[b_zt], w=[b_xs_z[zi]])
                if ui % cast_every == 0 and ci < len(casts):
                    dst_t, src_t, ex, rows = casts[ci]
                    ci += 1
                    P.dma('pool', lambda e, dst_t=dst_t, src_t=src_t, ex=ex, rows=rows: e.dma_start(
                        out=dst_t[l][ex].rearrange("p (k n) -> p k n", k=rows // 128),
                        in_=src_t[l, ex].rearrange("(k p) n -> p k n", p=128)), r=[b_wc_w])
            while zi_c[0] < NSLOT // 1024:
                zi = zi_c[0]
                zi_c[0] += 1
                P.dma('sp', lambda e, zi=zi: e.dma_start(
                    out=xs_d[zi * 1024:(zi + 1) * 1024, :].rearrange("(a p) d -> p a d", p=128), in_=zt),
                    r=[b_zt], w=[b_xs_z[zi]])
            while ci < len(casts):
                dst_t, src_t, ex, rows = casts[ci]
                ci += 1
                P.dma('pool', lambda e, dst_t=dst_t, src_t=src_t, ex=ex, rows=rows: e.dma_start(
                    out=dst_t[l][ex].rearrange("p (k n) -> p k n", k=rows // 128),
                    in_=src_t[l, ex].rearrange("(k p) n -> p k n", p=128)), r=[b_wc_w])
            P.op('sp', lambda e: e.nop(), w=[b_wc_w, b_wc])
            P.barrier()

            if l >= 1 and '3' in SKIP1:
                return
            ar.release(mid_mark)
            w_out_sb = ar.alloc([8, D], BF16)
            b_wout = [Buf() for _ in range(8)]
            for k in range(8):
                P.dma('pool', lambda e, k=k: e.dma_start(out=w_out_sb[:, k, :], in_=w_out_d[l, k * 128:(k + 1) * 128, :]),
                      w=[b_wout[k]])
            gattn_sb = ar.alloc([512], F32)
            ln1g_sb = ar.alloc([D], F32)
            ln1b_sb = ar.alloc([D], F32)
            wr_sb = ar.alloc([8, 20], BF16)
            br_sb = ar.alloc([20], F32)
            b_c3 = [Buf() for _ in range(5)]
            P.dma('sp', lambda e: e.dma_start(out=gattn_sb, in_=gattn_d[l]), w=[b_c3[0]])
            P.dma('sp', lambda e: e.dma_start(out=ln1g_sb, in_=ln1g_d[l]), w=[b_c3[1]])
            P.dma('sp', lambda e: e.dma_start(out=ln1b_sb, in_=ln1b_d[l]), w=[b_c3[2]])
            P.dma('pool', lambda e: e.dma_start(out=wr_sb, in_=wr_d[l].rearrange("(k p) n -> p k n", p=128)), w=[b_c3[3]])
            P.dma('sp', lambda e: e.dma_start(out=br_sb, in_=br_d[l]), w=[b_c3[4]])
            NBUF = 4
            junk = [ar.alloc([512], BF16) for _ in range(NBUF)]
            ssq = [ar.alloc([2], F32) for _ in range(NBUF)]
            an = [ar.alloc([512], BF16) for _ in range(NBUF)]
            mixT = [ar.alloc([8, 128], BF16) for _ in range(NBUF)]
            xt3 = [ar.alloc([D], F32) for _ in range(NBUF)]
            y3 = [ar.alloc([D], F32) for _ in range(NBUF)]
            st3 = [ar.alloc([2, 6], F32) for _ in range(NBUF)]
            mv3 = [ar.alloc([2], F32) for _ in range(NBUF)]
            x1f = [ar.alloc([D], F32) for _ in range(NBUF)]
            x1b = [ar.alloc([D], BF16) for _ in range(NBUF)]
            x1T = [ar.alloc([8, 128], BF16) for _ in range(NBUF)]
            B3 = [{k: Buf() for k in ('junk', 'ssq', 'an', 'mixa', 'mixr', 'xt', 'y', 'st', 'mv', 'x1f', 'x1b', 'x1T')}
                  for _ in range(NBUF)]

            def p3_load(t):
                s = t % NBUF
                B = B3[s]
                g = t // 4
                P.dma('sp', lambda e, t=t, s=s: e.dma_start(
                    out=mixT[s][:, 4:8, :], in_=recT_d[:, :, t * 128:(t + 1) * 128].rearrange("c p t -> p c t")),
                    r=[b_recT[g]], w=[B['mixr']])
                P.dma('sp', lambda e, t=t, s=s: e.dma_start(out=xt3[s], in_=xsrc[t * 128:(t + 1) * 128, :]),
                      r=[b_xres[t]] if l > 0 else [], w=[B['xt']])

            def p3_A1(t):
                s = t % NBUF
                B = B3[s]
                P.act(lambda e, t=t, s=s: e.activation(out=junk[s], in_=attn_sb[:, t, :], func=AF.Square,
                                                       accum_out=ssq[s][:, 0:1]), r=b_attn[t], w=[B['junk'], B['ssq']])
                P.act(lambda e, s=s: e.activation(out=ssq[s][:, 1:2], in_=ssq[s][:, 0:1], func=AF.Sqrt,
                                                  scale=1.0 / 512.0, bias=RMS_EPS), r=[B['ssq']], w=[B['ssq']])
                P.dve(lambda e, s=s: e.reciprocal(out=ssq[s][:, 1:2], in_=ssq[s][:, 1:2]), r=[B['ssq']], w=[B['ssq']])
                P.dve(lambda e, t=t, s=s: e.scalar_tensor_tensor(out=an[s], in0=attn_sb[:, t, :], scalar=ssq[s][:, 1:2],
                                                                  in1=gattn_sb, op0=ALU.mult, op1=ALU.mult),
                      r=b_attn[t] + [B['ssq'], b_c3[0]], w=[B['an']])
                tb = 0 if t % 2 == 0 else 7
                pb = bank_bf(tb)
                for c in range(4):
                    P.pe(lambda e, c=c, s=s, pb=pb: e.transpose(out=pb[:, c * 128:(c + 1) * 128],
                                                              in_=an[s][:, c * 128:(c + 1) * 128], identity=idb),
                         r=[B['an'], b_idb], w=[bb[tb]])
                P.act(lambda e, s=s, pb=pb: e.copy(out=mixT[s][:, 0:4, :], in_=pb[:, 0:512].rearrange("p (c t) -> p c t", t=128)),
                      r=[bb[tb]], w=[B['mixa']])

            def p3_A2(t):
                s = t % NBUF
                B = B3[s]
                ob = 1 if t % 2 == 0 else 5
                for nh in range(2):
                    for k in range(8):
                        P.pe(lambda e, k=k, nh=nh, s=s, ob=ob: e.matmul(banks[ob + nh][:, :], lhsT=mixT[s][:, k, :],
                                                                        rhs=w_out_sb[:, k, nh * 512:(nh + 1) * 512],
                                                                        start=(k == 0), stop=(k == 7)),
                             r=[B['mixa'], B['mixr'], b_wout[k]], w=[bb[ob + nh]])
                for nh in range(2):
                    P.dve(lambda e, nh=nh, s=s, ob=ob: e.scalar_tensor_tensor(
                        out=y3[s][:, nh * 512:(nh + 1) * 512], in0=xt3[s][:, nh * 512:(nh + 1) * 512], scalar=ALPHA,
                        in1=banks[ob + nh][:, :], op0=ALU.mult, op1=ALU.add), r=[B['xt'], bb[ob + nh]], w=[B['y']])
                ln_stats(P, y3[s], B['y'], st3[s], B['st'], mv3[s], B['mv'])

            def p3_A3(t):
                s = t % NBUF
                B = B3[s]
                ln_apply(P, y3[s], B['y'], mv3[s], B['mv'], ln1g_sb, b_c3[1], ln1b_sb, b_c3[2], x1f[s], B['x1f'])
                P.dma('sp', lambda e, t=t, s=s: e.dma_start(out=xres_d[t * 128:(t + 1) * 128, :], in_=x1f[s]),
                      r=[B['x1f']], w=[b_xres[t]])
                if dbg and l == DBG_LAYER:
                    P.dma('sp', lambda e, t=t, s=s: e.dma_start(out=dbg_d[t * 128:(t + 1) * 128, :], in_=x1f[s]),
                          r=[B['x1f'], b_out_done])
                P.act(lambda e, s=s: e.copy(out=x1b[s], in_=x1f[s]), r=[B['x1f']], w=[B['x1b']])
                P.dma('sp', lambda e, t=t, s=s: e.dma_start(out=x1b_d[t * 128:(t + 1) * 128, :], in_=x1b[s]),
                      r=[B['x1b']], w=[b_x1b_d[t]])

            def p3_B(t):
                s = t % NBUF
                B = B3[s]
                pb = bank_bf(3)
                for k in range(8):
                    P.pe(lambda e, k=k, s=s, pb=pb: e.transpose(out=pb[:, k * 128:(k + 1) * 128],
                                                              in_=x1b[s][:, k * 128:(k + 1) * 128], identity=idb),
                         r=[B['x1b'], b_idb], w=[bb[3]])
                P.act(lambda e, s=s, pb=pb: e.copy(out=x1T[s], in_=pb.rearrange("p (k t) -> p k t", t=128)),
                      r=[bb[3]], w=[B['x1T']])
                for k in range(8):
                    P.pe(lambda e, k=k, s=s: e.matmul(banks[4][:, 0:20], lhsT=x1T[s][:, k, :], rhs=wr_sb[:, k, :],
                                                      start=(k == 0), stop=(k == 7)),
                         r=[B['x1T'], b_c3[3]], w=[bb[4]])
                P.dve(lambda e, t=t: e.tensor_tensor(out=lg_sb[:, t, :], in0=banks[4][:, 0:20], in1=br_sb, op=ALU.add),
                      r=[bb[4], b_c3[4]], w=[b_lg[t]])

            p3_load(0)
            for i in range(NT + 3):
                if i + 1 < NT:
                    p3_load(i + 1)
                if 0 <= i - 3 < NT:
                    p3_B(i - 3)
                if 0 <= i - 2 < NT:
                    p3_A3(i - 2)
                if 0 <= i - 1 < NT:
                    p3_A2(i - 1)
                if i < NT:
                    p3_A1(i)
            P.barrier()
            ar.release(base_mark)
            NQ = NT * 4
            LE = ar.alloc([NQ, 4], F32)
            EQ = ar.alloc([NQ, 4], F32)
            gsm = ar.alloc([6, NT, 4], F32)
            exg = gsm[:, 0, :, :]
            gm1 = gsm[:, 1, :, :]
            gmask = gsm[:, 2, :, :]
            m1 = gsm[:, 3, :, :].rearrange("p t g -> p (t g)")
            m2 = gsm[:, 4, :, :].rearrange("p t g -> p (t g)")
            den = gsm[:, 5, :, :].rearrange("p t g -> p (t g)")
            gs2 = ar.alloc([2, NT], F32)
            mg = gs2[:, 0, :]
            sgm = gs2[:, 1, :]
            RR = [Buf()]
            Lg = lg_sb[:, :, 0:4]
            P.dve(lambda e: e.tensor_reduce(out=mg, in_=Lg, axis=AX.X, op=ALU.max), r=b_lg, w=RR)
            mgb = mg.unsqueeze(2).to_broadcast([128, NT, 4])
            P.dve(lambda e: e.tensor_tensor(out=gm1, in0=Lg, in1=mgb, op=ALU.is_ge), r=b_lg + RR, w=RR)
            P.dve(lambda e: e.tensor_tensor(out=exg, in0=Lg, in1=mgb, op=ALU.subtract), r=b_lg + RR, w=RR)
            P.act(lambda e: e.activation(out=exg, in_=exg, func=AF.Exp), r=RR, w=RR)
            P.dve(lambda e: e.tensor_reduce(out=sgm, in_=exg, axis=AX.X, op=ALU.add), r=RR, w=RR)
            P.dve(lambda e: e.reciprocal(out=sgm, in_=sgm), r=RR, w=RR)
            P.dve(lambda e: e.tensor_tensor(out=gmask, in0=gm1, in1=sgm.unsqueeze(2).to_broadcast([128, NT, 4]), op=ALU.mult),
                  r=RR, w=RR)
            LEt = LE.rearrange("p (t g) e -> p t (g e)", g=4)
            LEf = LE.rearrange("p q e -> p (q e)")
            EQf = EQ.rearrange("p q e -> p (q e)")
            P.dve(lambda e: e.tensor_copy(out=LEt, in_=lg_sb[:, :, 4:20]), r=b_lg, w=RR)
            m1b = m1.unsqueeze(2).to_broadcast([128, NQ, 4])
            m2b = m2.unsqueeze(2).to_broadcast([128, NQ, 4])
            P.dve(lambda e: e.tensor_reduce(out=m1, in_=LE, axis=AX.X, op=ALU.max), r=RR, w=RR)
            P.dve(lambda e: e.tensor_tensor(out=EQ, in0=LE, in1=m1b, op=ALU.is_ge), r=RR, w=RR)
            P.dve(lambda e: e.scalar_tensor_tensor(out=EQf, in0=EQf, scalar=-1e9, in1=LEf, op0=ALU.mult, op1=ALU.add), r=RR, w=RR)
            P.dve(lambda e: e.tensor_reduce(out=m2, in_=EQ, axis=AX.X, op=ALU.max), r=RR, w=RR)
            P.dve(lambda e: e.tensor_tensor(out=EQ, in0=LE, in1=m2b, op=ALU.is_ge), r=RR, w=RR)
            gm1b = gm1.rearrange("p t g -> p (t g)").unsqueeze(2).to_broadcast([128, NQ, 4])
            P.dve(lambda e: e.tensor_tensor(out=sel_sb.rearrange("p t (g e) -> p (t g) e", e=4), in0=EQ, in1=gm1b, op=ALU.mult),
                  r=RR, w=b_sel)
            P.dve(lambda e: e.tensor_tensor(out=LE, in0=LE, in1=m1b, op=ALU.subtract), r=RR, w=RR)
            P.act(lambda e: e.activation(out=LEf, in_=LEf, func=AF.Exp), r=RR, w=RR)
            P.dve(lambda e: e.tensor_tensor(out=LEf, in0=LEf, in1=EQf, op=ALU.mult), r=RR, w=RR)
            P.dve(lambda e: e.tensor_reduce(out=den, in_=LE, axis=AX.X, op=ALU.add), r=RR, w=RR)
            P.dve(lambda e: e.reciprocal(out=den, in_=den), r=RR, w=RR)
            P.dve(lambda e: e.tensor_tensor(out=den, in0=den, in1=gmask.rearrange("p t g -> p (t g)"), op=ALU.mult), r=RR, w=RR)
            P.dve(lambda e: e.tensor_tensor(out=comb_sb.rearrange("p t (g e) -> p (t g) e", e=4), in0=LE,
                                             in1=den.unsqueeze(2).to_broadcast([128, NQ, 4]), op=ALU.mult), r=RR, w=b_comb)
            selflat = sel_sb.rearrange("p t e -> p (t e)")
            P.pe(lambda e: e.matmul(banks[0][:, 0:NT * 16], lhsT=ltrib, rhs=selflat, start=True, stop=True),
                 r=[b_ltri] + b_sel, w=[bb[0]])
            P.pe(lambda e: e.matmul(banks[1][:, 0:NT * 16], lhsT=onesb, rhs=selflat, start=True, stop=True),
                 r=[b_onesb] + b_sel, w=[bb[1]])
            cinc = ar.alloc([16, NT], F32)
            onesT = ar.alloc([NT], F32)
            b_cinc = Buf()
            P.dve(lambda e: e.memset(onesT, 1.0), w=[b_cinc])
            tot_et = banks[1][:, 0:NT * 16].rearrange("p (t e) -> p e t", e=16)
            for ee in range(16):
                P.dve(lambda e, ee=ee: e.tensor_tensor_scan(out=cinc[:, ee, :], data0=onesT, data1=tot_et[:, ee, :], initial=0.0,
                                                            op0=ALU.mult, op1=ALU.add), r=[bb[1], b_cinc], w=[b_cinc])
            P.dve(lambda e: e.tensor_copy(out=carry_sb, in_=cinc[:, :, NT - 1]), r=[b_cinc], w=[b_carry])
            P.dve(lambda e: e.tensor_tensor(out=rank_sb, in0=banks[0][:, 0:NT * 16].rearrange("p (t e) -> p t e", e=16),
                                             in1=cinc.rearrange("p e t -> p t e"), op=ALU.add), r=[bb[0], b_cinc], w=b_rank)
            P.dve(lambda e: e.tensor_tensor(out=rank_sb, in0=rank_sb, in1=banks[1][:, 0:NT * 16].rearrange("p (t e) -> p t e", e=16),
                                             op=ALU.subtract), r=[bb[1]] + b_rank, w=b_rank)
            jstart_sb = ar.alloc([32, 16], F32)
            b_jstart = Buf()
            P.dma('sp', lambda e: e.dma_start(out=jstart_sb, in_=jstart_d[:, :, :]), w=[b_jstart])
            seg = ar.alloc([8, 16], F32)
            segi = ar.alloc([16], I32)
            cmp3 = ar.alloc([32, 16], F32)
            ejf = ar.alloc([32], F32)
            big = [ar.alloc([NT, 16], F32) for _ in range(4)]
            red = ar.alloc([4, NT], F32)
            b_seg = Buf()
            RS = [b_seg]
            m_e = seg[:, 0, :]
            cend = seg[:, 1, :]
            s_e = seg[:, 2, :]
            ones16 = seg[:, 3, :]
            P.dve(lambda e: e.tensor_scalar(out=segi, in0=carry_sb, scalar1=511.0, scalar2=None, op0=ALU.add), r=[b_carry], w=RS)
            P.dve(lambda e: e.tensor_scalar(out=segi, in0=segi, scalar1=9, scalar2=9, op0=ALU.arith_shift_right,
                                             op1=ALU.logical_shift_left), r=RS, w=RS)
            P.dve(lambda e: e.tensor_copy(out=m_e, in_=segi), r=RS, w=RS)
            P.dve(lambda e: e.memset(ones16, 1.0), r=RS, w=RS)
            P.dve(lambda e: e.tensor_tensor_scan(out=cend, data0=ones16, data1=m_e, initial=0.0, op0=ALU.mult, op1=ALU.add),
                  r=RS, w=RS)
            P.dve(lambda e: e.tensor_tensor(out=s_e, in0=cend, in1=m_e, op=ALU.subtract), r=RS, w=RS)
            P.dve(lambda e: e.tensor_tensor(out=cmp3, in0=cend.unsqueeze(1).to_broadcast([128, 32, 16]), in1=jstart_sb,
                                             op=ALU.is_le), r=RS + [b_jstart], w=RS)
            P.dve(lambda e: e.tensor_reduce(out=ejf, in_=cmp3, axis=AX.X, op=ALU.add), r=RS, w=RS)
            P.dve(lambda e: e.tensor_scalar(out=ejc, in0=ejf, scalar1=15.0, scalar2=None, op0=ALU.min), r=RS, w=[b_ej])
            P.dve(lambda e: e.tensor_scalar(out=ejs, in0=ejc, scalar1=128.0, scalar2=0.0, op0=ALU.mult, op1=ALU.add),
                  r=[b_ej], w=[b_ej])
            P.dve(lambda e: e.tensor_scalar(out=idxw, in0=ejs, scalar1=wbase_sb[:, 0:1], scalar2=None, op0=ALU.add),
                  r=[b_ej, b_wbase], w=[b_ej])
            rk = rank_sb.rearrange("p t e -> p (t e)")
            sl2 = sel_sb.rearrange("p t e -> p (t e)")
            cb2 = comb_sb.rearrange("p t e -> p (t e)")
            v_, mk, tm, v2 = [b.rearrange("p t e -> p (t e)") for b in big]
            v3, mk3, tm3, v23 = big
            i0, i1, w0, w1 = red[:, 0, :], red[:, 1, :], red[:, 2, :], red[:, 3, :]
            RB = [Buf()]
            allr = b_rank + b_sel + b_comb
            P.dve(lambda e: e.tensor_tensor(out=v3, in0=rank_sb, in1=s_e.unsqueeze(1).to_broadcast([128, NT, 16]), op=ALU.add),
                  r=allr + RS, w=RB)
            P.dve(lambda e: e.scalar_tensor_tensor(out=v_, in0=v_, scalar=1.0, in1=sl2, op0=ALU.add, op1=ALU.mult), r=RB + allr, w=RB)
            P.dve(lambda e: e.tensor_reduce(out=i0, in_=v3, axis=AX.X, op=ALU.max), r=RB, w=RB)
            P.dve(lambda e: e.tensor_tensor(out=mk3, in0=v3, in1=i0.unsqueeze(2).to_broadcast([128, NT, 16]), op=ALU.is_equal),
                  r=RB, w=RB)
            P.dve(lambda e: e.tensor_tensor(out=tm, in0=mk, in1=cb2, op=ALU.mult), r=RB + allr, w=RB)
            P.dve(lambda e: e.tensor_reduce(out=w0, in_=tm3, axis=AX.X, op=ALU.add), r=RB, w=RB)
            P.dve(lambda e: e.tensor_tensor(out=tm, in0=mk, in1=v_, op=ALU.mult), r=RB, w=RB)
            P.dve(lambda e: e.tensor_tensor(out=v2, in0=v_, in1=tm, op=ALU.subtract), r=RB, w=RB)
            P.dve(lambda e: e.tensor_reduce(out=i1, in_=v23, axis=AX.X, op=ALU.max), r=RB, w=RB)
            P.dve(lambda e: e.tensor_tensor(out=mk3, in0=v23, in1=i1.unsqueeze(2).to_broadcast([128, NT, 16]), op=ALU.is_equal),
                  r=RB, w=RB)
            P.dve(lambda e: e.tensor_tensor(out=tm, in0=mk, in1=cb2, op=ALU.mult), r=RB + allr, w=RB)
            P.dve(lambda e: e.tensor_reduce(out=w1, in_=tm3, axis=AX.X, op=ALU.add), r=RB, w=RB)
            P.dve(lambda e: e.tensor_scalar(out=idx_i[:, :, 0], in0=i0, scalar1=-1.0, scalar2=None, op0=ALU.add), r=RB, w=[b_idx])
            P.dve(lambda e: e.tensor_scalar(out=idx_i[:, :, 1], in0=i1, scalar1=-1.0, scalar2=None, op0=ALU.add), r=RB, w=[b_idx])
            P.dve(lambda e: e.tensor_copy(out=wts_sb[:, :, 0], in_=w0), r=RB, w=[b_idx])
            P.dve(lambda e: e.tensor_copy(out=wts_sb[:, :, 1], in_=w1), r=RB, w=[b_idx])
            NXS = 4
            xsb = [ar.alloc([D], BF16) for _ in range(NXS)]
            b_xsb = [Buf() for _ in range(NXS)]
            for t in range(NT):
                s = t % NXS
                P.dma('sp', lambda e, t=t, s=s: e.dma_start(out=xsb[s], in_=x1b_d[t * 128:(t + 1) * 128, :]),
                      r=[b_x1b_d[t]], w=[b_xsb[s]])
                for kk in range(2):
                    P.dma('pool', lambda e, t=t, s=s, kk=kk: e.indirect_dma_start(
                        out=xs_d[:, :], out_offset=bass.IndirectOffsetOnAxis(ap=idx_i[:, t, kk:kk + 1], axis=0),
                        in_=xsb[s], in_offset=None), r=[b_xsb[s], b_idx, b_xs_w] + b_xs_z)
            P.op('sp', lambda e: e.nop(), w=[b_xs_w, b_xs])
            P.barrier()

            if l >= 1 and '4' in SKIP1:
                return
            ar.release(base_mark)
            wgs = [ar.alloc([8, DE], BF16) for _ in range(2)]
            wus = [ar.alloc([8, DE], BF16) for _ in range(2)]
            wds = [ar.alloc([4, D], BF16) for _ in range(2)]
            b_wg = [Buf(), Buf()]
            b_wu = [Buf(), Buf()]
            b_wd = [Buf(), Buf()]
            ln2g_sb = ar.alloc([D], F32)
            ln2b_sb = ar.alloc([D], F32)
            b_c4 = [Buf(), Buf()]
            P.dma('sp', lambda e: e.dma_start(out=ln2g_sb, in_=ln2g_d[l]), w=[b_c4[0]])
            P.dma('sp', lambda e: e.dma_start(out=ln2b_sb, in_=ln2b_d[l]), w=[b_c4[1]])
            xst = [ar.alloc([4, D], BF16) for _ in range(2)]
            b_xst = [Buf(), Buf()]
            xTs = [ar.alloc([8, 512], BF16) for _ in range(2)]
            b_xTs = [[Buf() for _ in range(4)] for _ in range(2)]
            sg = [ar.alloc([512], F32) for _ in range(2)]
            b_sg = [Buf(), Buf()]
            hT = [ar.alloc([4, 512], BF16) for _ in range(2)]
            b_hT = [[Buf() for _ in range(4)] for _ in range(2)]
            NYS = 3
            yst = [ar.alloc([D], F32) for _ in range(NYS)]
            b_yst = [Buf() for _ in range(NYS)]
            gu_i = 0
            yb_i = 0
            ys_i = 0
            tp_i = 0
            wg_rows = wgb_d[l].rearrange("e p m -> (e p) m")
            wu_rows = wub_d[l].rearrange("e p m -> (e p) m")
            wd_rows = wdb_d[l].rearrange("e p m -> (e p) m")

            def stageW(j):
                wb = j % 2
                io = bass.IndirectOffsetOnAxis(ap=idxw[:, j:j + 1], axis=0)
                P.dma('pool', lambda e, wb=wb, io=io: e.indirect_dma_start(
                    out=wgs[wb].rearrange("p k n -> p (k n)"), out_offset=None, in_=wg_rows[:, :], in_offset=io),
                    r=[b_ej, b_wc], w=[b_wg[wb]])
                P.dma('pool', lambda e, wb=wb, io=io: e.indirect_dma_start(
                    out=wus[wb].rearrange("p k n -> p (k n)"), out_offset=None, in_=wu_rows[:, :], in_offset=io),
                    r=[b_ej, b_wc], w=[b_wu[wb]])
                P.dma('pool', lambda e, wb=wb, io=io: e.indirect_dma_start(
                    out=wds[wb].rearrange("p k n -> p (k n)"), out_offset=None, in_=wd_rows[:, :], in_offset=io),
                    r=[b_ej, b_wc], w=[b_wd[wb]])

            def stageXload(j):
                wb = j % 2
                P.dma('sp', lambda e, j=j, wb=wb: e.dma_start(
                    out=xst[wb], in_=xs_d[j * 512:(j + 1) * 512, :].rearrange("(a p) d -> p a d", p=128)),
                    r=[b_xs], w=[b_xst[wb]])

            tpc = [0]

            def stageT(j):
                wb = j % 2
                for a in range(4):
                    bk = 4 + (tpc[0] % 2)
                    tpc[0] += 1
                    pb = bank_bf(bk)
                    for k in range(8):
                        P.pe(lambda e, a=a, k=k, wb=wb, pb=pb: e.transpose(out=pb[:, k * 128:(k + 1) * 128],
                                                                         in_=xst[wb][:, a, k * 128:(k + 1) * 128], identity=idb),
                             r=[b_xst[wb], b_idb], w=[bb[bk]])
                    if a % 2 == 0:
                        P.act(lambda e, a=a, wb=wb, pb=pb: e.copy(out=xTs[wb][:, :, a * 128:(a + 1) * 128],
                                                                  in_=pb.rearrange("p (k t) -> p k t", t=128)),
                              r=[bb[bk]], w=[b_xTs[wb][a]])
                    else:
                        P.dve(lambda e, a=a, wb=wb, pb=pb: e.tensor_copy(out=xTs[wb][:, :, a * 128:(a + 1) * 128],
                                                                         in_=pb.rearrange("p (k t) -> p k t", t=128)),
                              r=[bb[bk]], w=[b_xTs[wb][a]])

            guc = [0]

            def stageGU(j):
                wb = j % 2
                hb = j % 2
                for c in range(4):
                    bg = (guc[0] % 2) * 2
                    guc[0] += 1
                    for k in range(8):
                        P.pe(lambda e, k=k, c=c, wb=wb, bg=bg: e.matmul(
                            banks[bg][:, :], lhsT=wgs[wb][:, k, c * 128:(c + 1) * 128],
                            rhs=xTs[wb][:, k, :], start=(k == 0), stop=(k == 7)),
                            r=[b_wg[wb]] + b_xTs[wb], w=[bb[bg]])
                    for k in range(8):
                        P.pe(lambda e, k=k, c=c, wb=wb, bg=bg: e.matmul(
                            banks[bg + 1][:, :], lhsT=wus[wb][:, k, c * 128:(c + 1) * 128],
                            rhs=xTs[wb][:, k, :], start=(k == 0), stop=(k == 7)),
                            r=[b_wu[wb]] + b_xTs[wb], w=[bb[bg + 1]])
                    sgi = guc[0] % 2
                    P.act(lambda e, bg=bg, sgi=sgi: e.activation(out=sg[sgi], in_=banks[bg][:, :], func=AF.Silu),
                          r=[bb[bg]], w=[b_sg[sgi]])
                    P.dve(lambda e, bg=bg, sgi=sgi, hb=hb, c=c: e.tensor_tensor(
                        out=hT[hb][:, c, :], in0=sg[sgi], in1=banks[bg + 1][:, :], op=ALU.mult),
                        r=[b_sg[sgi], bb[bg + 1]], w=[b_hT[hb][c]])

            ybc = [0, 0]

            def stageY(j):
                wb = j % 2
                hb = j % 2
                for a in range(4):
                    ysb = ybc[1] % NYS
                    ybc[1] += 1
                    for nh in range(2):
                        yb = 6 + (ybc[0] % 2)
                        ybc[0] += 1
                        for c in range(4):
                            P.pe(lambda e, c=c, a=a, nh=nh, hb=hb, wb=wb, yb=yb: e.matmul(
                                banks[yb][:, :], lhsT=hT[hb][:, c, a * 128:(a + 1) * 128],
                                rhs=wds[wb][:, c, nh * 512:(nh + 1) * 512], start=(c == 0), stop=(c == 3)),
                                r=[b_hT[hb][c], b_wd[wb]], w=[bb[yb]])
                        if nh == 0:
                            P.act(lambda e, ysb=ysb, yb=yb: e.copy(out=yst[ysb][:, 0:512], in_=banks[yb][:, :]),
                                  r=[bb[yb]], w=[b_yst[ysb]])
                        else:
                            P.dve(lambda e, ysb=ysb, yb=yb: e.tensor_copy(out=yst[ysb][:, 512:1024], in_=banks[yb][:, :]),
                                  r=[bb[yb]], w=[b_yst[ysb]])
                    row0 = (j * 4 + a) * 128
                    P.dma('sp', lambda e, ysb=ysb, row0=row0: e.dma_start(out=ys_d[row0:row0 + 128, :], in_=yst[ysb]),
                          r=[b_yst[ysb], b_ys_w])

            stageW(0)
            stageXload(0)
            stageT(0)
            for j in range(NTILE):
                if j + 1 < NTILE:
                    stageW(j + 1)
                    stageXload(j + 1)
                stageGU(j)
                if j + 1 < NTILE:
                    stageT(j + 1)
                stageY(j)
            P.op('sp', lambda e: e.nop(), w=[b_ys_w, b_ys])
            NB5 = 4
            g0t = [ar.alloc([D], F32) for _ in range(NB5)]
            g1t = [ar.alloc([D], F32) for _ in range(NB5)]
            x4 = [ar.alloc([D], F32) for _ in range(NB5)]
            st4 = [ar.alloc([2, 6], F32) for _ in range(NB5)]
            mv4 = [ar.alloc([2], F32) for _ in range(NB5)]
            o4 = [ar.alloc([D], F32) for _ in range(NB5)]
            B4 = [{k: Buf() for k in ('g0', 'g1', 'x', 'st', 'mv', 'o')} for _ in range(NB5)]
            def p5_load(t):
                s = t % NB5
                B = B4[s]
                P.dma('pool', lambda e, t=t, s=s: e.indirect_dma_start(
                    out=g0t[s], out_offset=None, in_=ys_d[:, :],
                    in_offset=bass.IndirectOffsetOnAxis(ap=idx_i[:, t, 0:1], axis=0)), r=[b_ys, b_idx], w=[B['g0']])
                P.dma('pool', lambda e, t=t, s=s: e.indirect_dma_start(
                    out=g1t[s], out_offset=None, in_=ys_d[:, :],
                    in_offset=bass.IndirectOffsetOnAxis(ap=idx_i[:, t, 1:2], axis=0)), r=[b_ys, b_idx], w=[B['g1']])
                P.dma('sp', lambda e, t=t, s=s: e.dma_start(out=x4[s], in_=xres_d[t * 128:(t + 1) * 128, :]),
                      r=[b_xres[t]], w=[B['x']])

            def p5_compute(t):
                s = t % NB5
                B = B4[s]
                P.act(lambda e, t=t, s=s: e.activation(out=g0t[s], in_=g0t[s], func=AF.Copy, scale=wts_sb[:, t, 0:1]),
                      r=[B['g0'], b_idx], w=[B['g0']])
                P.dve(lambda e, t=t, s=s: e.scalar_tensor_tensor(out=g0t[s], in0=g1t[s], scalar=wts_sb[:, t, 1:2], in1=g0t[s],
                                                                  op0=ALU.mult, op1=ALU.add),
                      r=[B['g0'], B['g1'], b_idx], w=[B['g0']])
                P.dve(lambda e, s=s: e.scalar_tensor_tensor(out=x4[s], in0=x4[s], scalar=ALPHA, in1=g0t[s],
                                                            op0=ALU.mult, op1=ALU.add),
                      r=[B['x'], B['g0']], w=[B['x']])
                ln_tile(P, x4[s], B['x'], st4[s], B['st'], mv4[s], B['mv'], ln2g_sb, b_c4[0], ln2b_sb, b_c4[1],
                        o4[s], B['o'], mul_eng='act_dve')
                if l == depth - 1:
                    P.dma('sp', lambda e, t=t, s=s: e.dma_start(out=out_d[t * 128:(t + 1) * 128, :], in_=o4[s]),
                          r=[B['o'], b_out_done])
                else:
                    P.dma('sp', lambda e, t=t, s=s: e.dma_start(out=xres_d[t * 128:(t + 1) * 128, :], in_=o4[s]),
                          r=[B['o'], B['x']], w=[b_xres[t]])

            LA5 = 2
            for t in range(min(LA5, NT)):
                p5_load(t)
            for t in range(NT):
                if t + LA5 < NT:
                    p5_load(t + LA5)
                p5_compute(t)
            P.barrier()
            if dbg and l == 0 and depth > 1:
                P.dma('sp', lambda e: e.dma_start(out=dbg2_d[:, :], in_=xres_d[:, :]), r=b_xres + [b_out_done])
                P.barrier()
        for l_ in range(depth if NLAYERS_RUN is None else NLAYERS_RUN):
            do_layer(l_)
        P.op('sp', lambda e: e.nop(), w=[b_out_done])
        P.emit(st)
    return nc, P, ar


def ln_stats(P, y, b_y, st, b_st, mv, b_mv):
    for hh in range(2):
        P.dve(lambda e, hh=hh: e.bn_stats(out=st[:, hh, :], in_=y[:, hh * 512:(hh + 1) * 512]), r=[b_y], w=[b_st])
    P.dve(lambda e: e.bn_aggr(out=mv, in_=st.rearrange("p a b -> p (a b)")), r=[b_st], w=[b_mv])


def ln_apply(P, y, b_y, mv, b_mv, g_sb, b_g, b_sb, b_b, out, b_out):
    P.act(lambda e: e.activation(out=mv[:, 1:2], in_=mv[:, 1:2], func=AF.Sqrt, bias=LN_EPS, scale=1.0), r=[b_mv], w=[b_mv])
    P.dve(lambda e: e.reciprocal(out=mv[:, 1:2], in_=mv[:, 1:2]), r=[b_mv], w=[b_mv])
    P.dve(lambda e: e.scalar_tensor_tensor(out=out, in0=y, scalar=mv[:, 0:1], in1=g_sb, op0=ALU.subtract, op1=ALU.mult),
          r=[b_y, b_mv, b_g], w=[b_out])
    P.dve(lambda e: e.scalar_tensor_tensor(out=out, in0=out, scalar=mv[:, 1:2], in1=b_sb, op0=ALU.mult, op1=ALU.add),
          r=[b_out, b_mv, b_b], w=[b_out])


def ln_tile(P, y, b_y, st, b_st, mv, b_mv, g_sb, b_g, b_sb, b_b, out, b_out, mul_eng='pool'):
    ln_stats(P, y, b_y, st, b_st, mv, b_mv)
    ln_apply(P, y, b_y, mv, b_mv, g_sb, b_g, b_sb, b_b, out, b_out)


def router_tile(P, lg_ps, b_ps, br_sb, b_br, rl, b_rl, comb, b_comb, sel, b_sel):
    L = rl[:, 0:20]
    mg = rl[:, 20:21]
    sgm = rl[:, 21:22]
    gmask = rl[:, 22:26]
    m1 = rl[:, 26:30]
    m2 = rl[:, 30:34]
    eq = rl[:, 34:50]
    ex = rl[:, 50:54]
    den = rl[:, 54:58]
    gm1 = rl[:, 58:62]
    le = L[:, 4:20]
    le3 = le.rearrange("p (g e) -> p g e", e=4)
    eq3 = eq.rearrange("p (g e) -> p g e", e=4)
    R = [b_rl]
    P.dve(lambda e: e.tensor_tensor(out=L, in0=lg_ps, in1=br_sb, op=ALU.add), r=[b_ps, b_br], w=R)
    P.dve(lambda e: e.tensor_reduce(out=mg, in_=L[:, 0:4], axis=AX.X, op=ALU.max), r=R, w=R)
    P.dve(lambda e: e.tensor_scalar(out=gm1, in0=L[:, 0:4], scalar1=mg, scalar2=None, op0=ALU.is_ge), r=R, w=R)
    P.dve(lambda e: e.tensor_scalar(out=ex, in0=L[:, 0:4], scalar1=mg, scalar2=None, op0=ALU.subtract), r=R, w=R)
    P.act(lambda e: e.activation(out=ex, in_=ex, func=AF.Exp, accum_out=sgm), r=R, w=R)
    P.dve(lambda e: e.reciprocal(out=sgm, in_=sgm), r=R, w=R)
    P.dve(lambda e: e.tensor_scalar(out=gmask, in0=gm1, scalar1=sgm, scalar2=None, op0=ALU.mult), r=R, w=R)
    P.dve(lambda e: e.tensor_reduce(out=m1, in_=le3, axis=AX.X, op=ALU.max), r=R, w=R)
    m1b = m1.unsqueeze(2).to_broadcast([128, 4, 4])
    P.dve(lambda e: e.tensor_tensor(out=eq3, in0=le3, in1=m1b, op=ALU.is_ge), r=R, w=R)
    P.dve(lambda e: e.scalar_tensor_tensor(out=eq, in0=eq, scalar=-1e9, in1=le, op0=ALU.mult, op1=ALU.add), r=R, w=R)
    P.dve(lambda e: e.tensor_reduce(out=m2, in_=eq3, axis=AX.X, op=ALU.max), r=R, w=R)
    m2b = m2.unsqueeze(2).to_broadcast([128, 4, 4])
    P.dve(lambda e: e.tensor_tensor(out=eq3, in0=le3, in1=m2b, op=ALU.is_ge), r=R, w=R)
    gm1b = gm1.unsqueeze(2).to_broadcast([128, 4, 4])
    P.dve(lambda e: e.tensor_tensor(out=sel.rearrange("p (g e) -> p g e", e=4), in0=eq3, in1=gm1b, op=ALU.mult),
          r=R, w=[b_sel])
    P.dve(lambda e: e.tensor_tensor(out=le3, in0=le3, in1=m1b, op=ALU.subtract), r=R, w=R)
    P.act(lambda e: e.activation(out=le, in_=le, func=AF.Exp), r=R, w=R)
    P.dve(lambda e: e.tensor_tensor(out=le, in0=le, in1=eq, op=ALU.mult), r=R, w=R)
    P.dve(lambda e: e.tensor_reduce(out=den, in_=le3, axis=AX.X, op=ALU.add), r=R, w=R)
    P.dve(lambda e: e.reciprocal(out=den, in_=den), r=R, w=R)
    P.dve(lambda e: e.tensor_tensor(out=den, in0=den, in1=gmask, op=ALU.mult), r=R, w=R)
    denb = den.unsqueeze(2).to_broadcast([128, 4, 4])
    P.dve(lambda e: e.tensor_tensor(out=comb.rearrange("p (g e) -> p g e", e=4), in0=le3, in1=denb, op=ALU.mult),
          r=R, w=[b_comb])


def host_consts(S):
    NT = S // 128
    pos = np.arange(S, dtype=np.float32)
    half = 8
    inv_freq = (np.float32(500000.0) ** (-np.arange(half, dtype=np.float32) * np.float32(2.0) / np.float32(16))).astype(np.float32)
    ang = pos[:, None] * inv_freq[None, :]
    cos = np.cos(ang).astype(np.float32)
    sin = np.sin(ang).astype(np.float32)

    def tab(a):
        return np.ascontiguousarray(a.reshape(NT, 128, 8).transpose(1, 0, 2))
    n = np.arange(16)[None, :]
    bi_t = (np.arange(NT) // 2)[:, None]
    gcaus = np.where(n < bi_t, 0.0, -1e30).astype(np.float32)
    gown = np.where(n == bi_t, 0.0, NEG).astype(np.float32)
    gcaus = np.ascontiguousarray(np.broadcast_to(gcaus[None], (128, NT, 16)))
    gown = np.ascontiguousarray(np.broadcast_to(gown[None], (128, NT, 16)))
    kk = np.arange(128)[:, None]
    qq = np.arange(128)[None, :]
    tri = np.where(kk <= qq, 0.0, NEG).astype(np.float32)
    khot = (np.arange(S)[None, :] // 256 == np.arange(16)[:, None]).astype(np.float32)
    ltri = (np.arange(128)[:, None] < np.arange(128)[None, :]).astype(np.float32)
    jstart = np.ascontiguousarray(np.broadcast_to((512.0 * np.arange(32, dtype=np.float32))[None, :, None], (128, 32, 16)))
    wbase = (np.arange(128, dtype=np.float32)[:, None] + 128.0 * np.arange(8, dtype=np.float32)[None, :]).astype(np.float32)
    shift = np.zeros((128, 128), np.float32)
    shift[np.arange(64), np.arange(64) + 64] = 1.0
    return {"c_ident": np.eye(128, dtype=np.float32), "c_cos": tab(cos), "c_sin": tab(sin), "c_gcaus": gcaus,
            "c_gown": gown, "c_tri": tri, "c_khot": khot, "c_shift": shift, "c_ltri": ltri, "c_jstart": jstart, "c_wbase": wbase}


def host_params(inp, depth):
    def f(a):
        return np.ascontiguousarray(np.asarray(a, dtype=np.float32))

    def chunkvec(v):
        return f(np.asarray(v)[:depth].reshape(depth, 4, 128).transpose(0, 2, 1))

    def bcast(v):
        v = np.asarray(v)[:depth]
        return f(np.broadcast_to(v[:, None, :], (depth, 128, v.shape[-1])))

    def blockdiag(w):
        w = np.asarray(w)[:depth]
        o = np.zeros((depth, 4, 128, 128), np.float32)
        for c in range(4):
            o[:, c, 0:64, 0:64] = w[:, 2 * c]
            o[:, c, 64:128, 64:128] = w[:, 2 * c + 1]
        return o
    p = {}
    p["w_in"] = f(inp["w_in"][:depth])
    p["conv_w"] = f(np.asarray(inp["conv_w"])[:depth].reshape(depth, 4, 4, 128).transpose(0, 3, 2, 1))
    p["conv_b"] = chunkvec(inp["conv_b"])
    p["w_rg_a"] = blockdiag(inp["w_rg_a"])
    p["b_rg_a"] = chunkvec(inp["b_rg_a"])
    p["w_rg_i"] = blockdiag(inp["w_rg_i"])
    p["b_rg_i"] = chunkvec(inp["b_rg_i"])
    p["lru_lambda"] = chunkvec(inp["lru_lambda"])
    p["g_attn_norm"] = bcast(inp["g_attn_norm"])
    p["g_rec_norm"] = chunkvec(inp["g_rec_norm"])
    p["w_out"] = f(inp["w_out"][:depth])
    p["ln1_g"] = bcast(inp["ln1_g"])
    p["ln1_b"] = bcast(inp["ln1_b"])
    p["w_router"] = f(np.concatenate([np.asarray(inp["w_router_group"])[:depth], np.asarray(inp["w_router_expert"])[:depth]], axis=-1))
    p["b_router"] = bcast(np.concatenate([np.asarray(inp["b_router_group"])[:depth], np.asarray(inp["b_router_expert"])[:depth]], axis=-1))
    p["w_gate"] = f(inp["w_gate"][:depth])
    p["w_up"] = f(inp["w_up"][:depth])
    p["w_down"] = f(inp["w_down"][:depth])
    p["ln2_g"] = bcast(inp["ln2_g"])
    p["ln2_b"] = bcast(inp["ln2_b"])
    return p


_CACHE = {}


def kernel(**inputs):
    x = np.asarray(inputs["x"], dtype=np.float32)
    B, S, _ = x.shape
    depth = int(np.asarray(inputs["w_in"]).shape[0])
    key = (S, depth)
    if key not in _CACHE:
        _CACHE[key] = build(S, depth)[0]
    nc = _CACHE[key]
    shared = host_params(inputs, depth)
    shared.update(host_consts(S))
    in_maps = []
    for b in range(B):
        m = dict(shared)
        m["x"] = np.ascontiguousarray(x[b])
        in_maps.append(m)
    res = run_bass_kernel_spmd(nc, in_maps, core_ids=list(range(B)))
    return np.stack([np.asarray(r["out"], dtype=np.float32) for r in res.results], axis=0)
```

```python
import numpy as np
from contextlib import ExitStack
import concourse.bass as bass
import concourse.mybir as mybir
from concourse.bass_utils import run_bass_kernel_spmd

F32 = mybir.dt.float32
BF16 = mybir.dt.bfloat16
I32 = mybir.dt.int32
AF = mybir.ActivationFunctionType
ALU = mybir.AluOpType
AX = mybir.AxisListType

D = 1024
NH = 8
HD = 64
NEXP = 16
DE = 512
ALPHA = float((2 * 2) ** 0.25)
LN_EPS = 1e-5
RMS_EPS = 1e-6
NEG = -60000.0
COMPUTE = ('pe', 'act', 'dve', 'pool', 'sp')
NDMASEM = 24


class Buf:
    __slots__ = ('name', 'lw', 'rd_eng', 'rd_dma')

    def __init__(self, name=''):
        self.name = name
        self.lw = None
        self.rd_eng = {}
        self.rd_dma = []


class Op:
    __slots__ = ('idx', 'eng', 'fn', 'deps', 'is_dma', 'dq', 'dsem', 'dtarget', 'sig', 'sigval')

    def __init__(self):
        self.sig = False
        self.sigval = 0
        self.is_dma = False
        self.dq = 0


class Prog:
    def __init__(self, nc):
        self.nc = nc
        self.ops = []
        self.dma_count = {'sp': 0, 'act': 0, 'pool': 0}
        self.last = {}
        self.pending_dma = []

    def op(self, eng, fn, r=(), w=(), dma=False):
        ops = self.ops
        o = Op()
        o.idx = len(ops)
        o.eng = eng
        o.fn = fn
        o.is_dma = dma
        deps = set()
        raw_src = set()
        for b in r:
            if b.lw is not None:
                deps.add(b.lw)
                raw_src.add(b.lw)
        for b in w:
            if b.lw is not None:
                deps.add(b.lw)
            deps.update(b.rd_eng.values())
            deps.update(b.rd_dma)
        best = {}
        final = []
        for d in deps:
            p = ops[d]
            if p.is_dma:
                final.append(d)
                continue
            if p.eng == eng and not dma:
                if eng == 'pe':
                    continue
            if p.eng not in best or best[p.eng] < d:
                best[p.eng] = d
        final.extend(best.values())
        o.deps = final
        for d in final:
            ops[d].sig = True
        if dma:
            o.dq = self.dma_count[eng]
            self.dma_count[eng] += 1
            o.sig = True
            self.pending_dma.append(o.idx)
        else:
            self.last[eng] = o.idx
        for b in r:
            if dma:
                b.rd_dma.append(o.idx)
            else:
                b.rd_eng[eng] = o.idx
        for b in w:
            b.lw = o.idx
            b.rd_eng = {}
            b.rd_dma = []
        ops.append(o)
        return o

    def pe(self, fn, r=(), w=()):
        return self.op('pe', fn, r, w)

    def act(self, fn, r=(), w=()):
        return self.op('act', fn, r, w)

    def dve(self, fn, r=(), w=()):
        return self.op('dve', fn, r, w)

    def pool(self, fn, r=(), w=()):
        return self.op('pool', fn, r, w)

    def dma(self, q, fn, r=(), w=()):
        return self.op(q, fn, r, w, dma=True)

    def barrier(self):
        ops = self.ops
        last = dict(self.last)
        pend = list(self.pending_dma)
        for e in ('pe', 'act', 'dve', 'pool', 'sp'):
            o = Op()
            o.idx = len(ops)
            o.eng = e
            o.fn = lambda eng: eng.nop()
            o.deps = [v for k, v in last.items() if k != e] + pend
            for d in o.deps:
                ops[d].sig = True
            ops.append(o)
            if e != 'sp':
                self.last[e] = o.idx
        self.pending_dma = []

    def emit(self, stack):
        nc = self.nc
        ops = self.ops
        sems = {}
        for e in COMPUTE:
            sems[e] = stack.enter_context(nc.semaphore('s_' + e))
        dsems = {}
        for q in ('sp', 'act', 'pool'):
            if self.dma_count[q]:
                dsems[q] = [stack.enter_context(nc.semaphore('d_%s%d' % (q, i)))
                            for i in range(min(NDMASEM, self.dma_count[q]))]
        cnt = {e: 0 for e in COMPUTE}
        for o in ops:
            if o.is_dma:
                n = len(dsems[o.eng])
                o.dsem = dsems[o.eng][o.dq % n]
                o.dtarget = 16 * (o.dq // n + 1)
            elif o.sig:
                cnt[o.eng] += 1
                o.sigval = cnt[o.eng]
        self.sig_counts = dict(cnt)
        streams = {e: [] for e in ('pe', 'act', 'dve', 'pool', 'sp')}
        for o in ops:
            streams[o.eng].append(o)
        block = stack.enter_context(nc.Block())

        def run(engname, eng):
            waited = {}

            def wait(sem, val):
                k = sem.num
                if waited.get(k, 0) >= val:
                    return
                waited[k] = val
                eng.wait_ge(sem, val)

            for o in streams[engname]:
                for d in o.deps:
                    p = ops[d]
                    if p.is_dma:
                        wait(p.dsem, p.dtarget)
                    else:
                        wait(sems[p.eng], p.sigval)
                if o.is_dma:
                    if o.dtarget > 16:
                        wait(o.dsem, o.dtarget - 16)
                    o.fn(eng).then_inc(o.dsem, 16)
                else:
                    if engname == 'pool' and o.sig and o.sigval > 1:
                        wait(sems['pool'], o.sigval - 1)
                    ins = o.fn(eng)
                    if o.sig:
                        ins.then_inc(sems[o.eng], 1)

        @block.sync
        def _(e):
            run('sp', e)

        @block.scalar
        def _(e):
            run('act', e)

        @block.vector
        def _(e):
            run('dve', e)

        @block.gpsimd
        def _(e):
            run('pool', e)

        @block.tensor
        def _(e):
            run('pe', e)


class Arena:
    def __init__(self, ap, total):
        self.ap = ap
        self.total = total
        self.top = 0
        self.peak = 0

    def alloc(self, free_shape, dtype, parts=128):
        n = int(np.prod(free_shape))
        four = dtype in (F32, I32)
        nf = n if four else (n + 1) // 2
        assert self.top + nf <= self.total, ('SBUF arena overflow', self.top, nf, self.total)
        v = self.ap[0:parts, self.top:self.top + nf]
        self.top += nf
        self.peak = max(self.peak, self.top)
        if dtype != F32:
            v = v.bitcast(dtype)
            if (not four) and n % 2:
                v = v[:, 0:n]
        if len(free_shape) == 2:
            v = v.rearrange("p (a b) -> p a b", b=free_shape[1])
        elif len(free_shape) == 3:
            v = v.rearrange("p (a b c) -> p a b c", b=free_shape[1], c=free_shape[2])
        elif len(free_shape) == 4:
            v = v.rearrange("p (a b c d) -> p a b c d", b=free_shape[1], c=free_shape[2], d=free_shape[3])
        return v

    def mark(self):
        return self.top

    def release(self, m):
        self.top = m


ARENA_F32 = 53000
DBG_LAYER = 0
SKIP1 = ''
NLAYERS_RUN = None


def build(S, depth, dbg=False):
    NT = S // 128
    NG = S // 512
    NB = S // 256
    nc = bass.Bass("TRN2", target_bir_lowering=False)

    def din(name, shape, dt=F32):
        return nc.dram_tensor(name, list(shape), dt, kind="ExternalInput").ap()

    def dscr(name, shape, dt):
        return nc.dram_tensor(name, list(shape), dt).ap()

    x_d = din("x", [S, D])
    w_in_d = din("w_in", [depth, D, 2560])
    convw_d = din("conv_w", [depth, 128, 4, 4])
    convb_d = din("conv_b", [depth, 128, 4])
    wa_d = din("w_rg_a", [depth, 4, 128, 128])
    ba_d = din("b_rg_a", [depth, 128, 4])
    wi_d = din("w_rg_i", [depth, 4, 128, 128])
    bi_d = din("b_rg_i", [depth, 128, 4])
    lam_d = din("lru_lambda", [depth, 128, 4])
    gattn_d = din("g_attn_norm", [depth, 128, 512])
    grec_d = din("g_rec_norm", [depth, 128, 4])
    w_out_d = din("w_out", [depth, D, D])
    ln1g_d = din("ln1_g", [depth, 128, D])
    ln1b_d = din("ln1_b", [depth, 128, D])
    wr_d = din("w_router", [depth, D, 20])
    br_d = din("b_router", [depth, 128, 20])
    wg_d = din("w_gate", [depth, NEXP, D, DE])
    wu_d = din("w_up", [depth, NEXP, D, DE])
    wd_d = din("w_down", [depth, NEXP, DE, D])
    ln2g_d = din("ln2_g", [depth, 128, D])
    ln2b_d = din("ln2_b", [depth, 128, D])
    ident_d = din("c_ident", [128, 128])
    cos_d = din("c_cos", [128, NT, 8])
    sin_d = din("c_sin", [128, NT, 8])
    gcaus_d = din("c_gcaus", [128, NT, 16])
    gown_d = din("c_gown", [128, NT, 16])
    tri_d = din("c_tri", [128, 128])
    khot_d = din("c_khot", [16, S])
    shift_d = din("c_shift", [128, 128])
    ltri_d = din("c_ltri", [128, 128])
    wbase_d = din("c_wbase", [128, 8])
    jstart_d = din("c_jstart", [128, 32, 16])
    out_d = nc.dram_tensor("out", [S, D], F32, kind="ExternalOutput").ap()
    dbg_d = None
    if dbg:
        dbg_d = nc.dram_tensor("dbg_x1", [S, D], F32, kind="ExternalOutput").ap()
        dbg2_d = nc.dram_tensor("dbg_x2", [S, D], F32, kind="ExternalOutput").ap()

    xres_d = dscr("xres", [S, D], F32)
    qTa_d = dscr("qTa", [NH, 80, S], BF16)
    kT_d = dscr("kT", [NH, 64, S], BF16)
    recT_d = dscr("recT", [4, 128, S], BF16)
    NSLOT = 2 * S + 16 * 512
    NTILE = NSLOT // 512
    assert NTILE <= 32
    xs_d = dscr("xs", [NSLOT, D], BF16)
    wgb_d = [dscr("wgb%d" % i, [NEXP, 128, 8 * DE], BF16) for i in range(depth)]
    wub_d = [dscr("wub%d" % i, [NEXP, 128, 8 * DE], BF16) for i in range(depth)]
    wdb_d = [dscr("wdb%d" % i, [NEXP, 128, 4 * D], BF16) for i in range(depth)]
    ys_d = dscr("ys", [NSLOT, D], F32)

    P = Prog(nc)
    with ExitStack() as st:
        arena_t = st.enter_context(nc.sbuf_tensor("arena", [128, ARENA_F32], F32))
        ar = Arena(arena_t, ARENA_F32)
        banks = [st.enter_context(nc.psum_tensor("bank%d" % i, [128, 512], F32)) for i in range(8)]
        bb = [Buf('bank%d' % i) for i in range(8)]

        def bank_bf(i):
            return banks[i][:, :].bitcast(BF16)

        identf = ar.alloc([128], F32)
        idb = ar.alloc([128], BF16)
        trif = ar.alloc([128], F32)
        trib = ar.alloc([128], BF16)
        ones_f = ar.alloc([128], F32)
        b_identf, b_idb, b_trif, b_trib, b_ones = Buf(), Buf(), Buf(), Buf(), Buf()
        P.dma('sp', lambda e: e.dma_start(out=identf, in_=ident_d[:, :]), w=[b_identf])
        P.dma('sp', lambda e: e.dma_start(out=trif, in_=tri_d[:, :]), w=[b_trif])
        P.dve(lambda e: e.tensor_copy(out=idb, in_=identf), r=[b_identf], w=[b_idb])
        P.dve(lambda e: e.tensor_copy(out=trib, in_=trif), r=[b_trif], w=[b_trib])
        P.dve(lambda e: e.memset(ones_f, 1.0), w=[b_ones])
        shiftf = ar.alloc([128], F32)
        shiftb = ar.alloc([128], BF16)
        b_shiftf, b_shift = Buf(), Buf()
        P.dma('sp', lambda e: e.dma_start(out=shiftf, in_=shift_d[:, :]), w=[b_shiftf])
        P.dve(lambda e: e.tensor_copy(out=shiftb, in_=shiftf), r=[b_shiftf], w=[b_shift])
        comb_sb = ar.alloc([NT, 16], F32)
        b_comb = [Buf() for _ in range(NT)]
        ltrif = ar.alloc([128], F32)
        ltrib = ar.alloc([128], BF16)
        onesb = ar.alloc([128], BF16)
        b_ltrif, b_ltri, b_onesb = Buf(), Buf(), Buf()
        P.dma('sp', lambda e: e.dma_start(out=ltrif, in_=ltri_d[:, :]), w=[b_ltrif])
        P.dve(lambda e: e.tensor_copy(out=ltrib, in_=ltrif), r=[b_ltrif], w=[b_ltri])
        P.dve(lambda e: e.memset(onesb, 1.0), w=[b_onesb])
        lg_sb = ar.alloc([NT, 20], F32)
        b_lg = [Buf() for _ in range(NT)]
        sel_sb = ar.alloc([NT, 16], BF16)
        b_sel = [Buf() for _ in range(NT)]
        rank_sb = ar.alloc([NT, 16], F32)
        b_rank = [Buf() for _ in range(NT)]
        carry_sb = ar.alloc([16], F32)
        b_carry = Buf()
        idx_i = ar.alloc([NT, 2], I32)
        wts_sb = ar.alloc([NT, 2], F32)
        b_idx = Buf()
        ej_i = ar.alloc([32], I32)
        b_ej = Buf()
        wbase_sb = ar.alloc([8], F32)
        b_wbase = Buf()
        P.dma('sp', lambda e: e.dma_start(out=wbase_sb, in_=wbase_d[:, :]), w=[b_wbase])
        idxw = ar.alloc([32], I32)
        ejc = ar.alloc([32], F32)
        ejs = ar.alloc([32], F32)
        base_mark = ar.mark()
        vsb = ar.alloc([NT, NH, 65], BF16)
        b_v = [Buf('v%d' % t) for t in range(NT)]
        v_mark = ar.mark()
        attn_sb = ar.alloc([NT, 512], BF16)
        b_attn = [[Buf() for _ in range(NH)] for _ in range(NT)]
        mid_mark = ar.mark()
        b_out_done = Buf('out_done')
        b_xres = [Buf() for _ in range(NT)]
        b_qTa = [[Buf() for _ in range(NG)] for _ in range(NH)]
        b_kT = [[Buf() for _ in range(NG)] for _ in range(NH)]
        b_recT = [Buf() for _ in range(NG)]
        b_xs_w, b_xs, b_ys_w, b_ys = Buf(), Buf(), Buf(), Buf()
        b_wc_w, b_wc = Buf(), Buf()
        b_xs_z = [Buf() for _ in range(NSLOT // 1024)]

        def do_layer(l):
            xsrc = x_d if l == 0 else xres_d
            xdst2 = out_d if l == depth - 1 else xres_d

            if l >= 1 and '1' in SKIP1:
                return
            ar.release(v_mark)
            w_in_sb = ar.alloc([8, 2560], BF16)
            b_win = [Buf(), Buf()]
            for hh in range(2):
                P.dma('pool', lambda e, hh=hh: e.dma_start(
                    out=w_in_sb[:, :, hh * 1280:(hh + 1) * 1280],
                    in_=w_in_d[l].rearrange("(k p) n -> p k n", p=128)[:, :, hh * 1280:(hh + 1) * 1280]),
                    w=[b_win[hh]])
            cos_sb = ar.alloc([NT, 8], F32)
            sin_sb = ar.alloc([NT, 8], F32)
            gcaus_sb = ar.alloc([NT, 16], F32)
            gown_sb = ar.alloc([NT, 16], F32)
            b_tab = Buf()
            P.dma('sp', lambda e: e.dma_start(out=cos_sb, in_=cos_d[:, :, :]), w=[b_tab])
            b_tab2, b_tab3, b_tab4 = Buf(), Buf(), Buf()
            P.dma('sp', lambda e: e.dma_start(out=sin_sb, in_=sin_d[:, :, :]), w=[b_tab2])
            P.dma('sp', lambda e: e.dma_start(out=gcaus_sb, in_=gcaus_d[:, :, :]), w=[b_tab3])
            P.dma('sp', lambda e: e.dma_start(out=gown_sb, in_=gown_d[:, :, :]), w=[b_tab4])
            convw_sb = ar.alloc([4, 4], F32)
            convb_sb = ar.alloc([4], F32)
            ba_sb = ar.alloc([4], F32)
            bi_sb = ar.alloc([4], F32)
            lam_sb = ar.alloc([4], F32)
            cdec_sb = ar.alloc([4], F32)
            grec_sb = ar.alloc([4], F32)
            wa_sb = ar.alloc([4, 128], BF16)
            wi_sb = ar.alloc([4, 128], BF16)
            b_par = Buf()
            b_wa, b_wi, b_waf, b_wif, b_cdec, b_lam = Buf(), Buf(), Buf(), Buf(), Buf(), Buf()
            b_p = [Buf() for _ in range(6)]
            P.dma('sp', lambda e: e.dma_start(out=convw_sb, in_=convw_d[l]), w=[b_p[0]])
            P.dma('sp', lambda e: e.dma_start(out=convb_sb, in_=convb_d[l]), w=[b_p[1]])
            P.dma('sp', lambda e: e.dma_start(out=ba_sb, in_=ba_d[l]), w=[b_p[2]])
            P.dma('sp', lambda e: e.dma_start(out=bi_sb, in_=bi_d[l]), w=[b_p[3]])
            P.dma('sp', lambda e: e.dma_start(out=lam_sb, in_=lam_d[l]), w=[b_lam])
            P.dma('sp', lambda e: e.dma_start(out=grec_sb, in_=grec_d[l]), w=[b_p[5]])
            P.dma('pool', lambda e: e.dma_start(out=wa_sb, in_=wa_d[l].rearrange("c p n -> p c n")), w=[b_wa])
            P.dma('pool', lambda e: e.dma_start(out=wi_sb, in_=wi_d[l].rearrange("c p n -> p c n")), w=[b_wi])
            P.act(lambda e: e.activation(out=cdec_sb, in_=lam_sb, func=AF.Exp, scale=-1.0), r=[b_lam], w=[b_cdec])
            P.act(lambda e: e.activation(out=cdec_sb, in_=cdec_sb, func=AF.Ln, bias=1.0), r=[b_cdec], w=[b_cdec])
            P.dve(lambda e: e.tensor_scalar(out=cdec_sb, in0=cdec_sb, scalar1=-8.0, scalar2=None, op0=ALU.mult),
                  r=[b_cdec], w=[b_cdec])
            b_par_all = b_p + [b_cdec]

            kmeanBD = ar.alloc([4, 2, 16], BF16)
            b_kmean = Buf()
            P.dve(lambda e: e.memset(kmeanBD, 0.0), w=[b_kmean])

            xbf = ar.alloc([4, D], BF16)
            b_xbf = [Buf() for _ in range(4)]
            oddk = ar.alloc([8], BF16, parts=64)
            b_oddk = Buf()
            xT2 = [ar.alloc([8, 512], BF16) for _ in range(2)]
            b_xT2 = [[Buf() for _ in range(4)] for _ in range(2)]
            qb = ar.alloc([4, NH, 80], BF16)
            b_qb = [Buf() for _ in range(4)]
            kb = ar.alloc([4, NH, 64], BF16)
            b_kb = [Buf() for _ in range(4)]
            qTp = ar.alloc([4, 512], BF16)
            b_qTp = [Buf() for _ in range(4)]
            kTh = ar.alloc([NH, 512], BF16, parts=64)
            b_kTh = [Buf() for _ in range(4)]
            qTa = ar.alloc([NH, 512], BF16, parts=80)
            b_qTah = [Buf() for _ in range(4)]
            ksum = ar.alloc([NH, 2], F32, parts=64)
            b_ksum = Buf()
            rt = [ar.alloc([NH, 8], F32) for _ in range(4)]
            b_rt = [Buf() for _ in range(4)]
            gm = ar.alloc([4, NH], F32)
            b_gm = Buf()
            xr_sb = ar.alloc([4, 515], F32)
            b_xr = [Buf() for _ in range(4)]
            P.dve(lambda e: e.memset(xr_sb, 0.0), w=b_xr)
            hst = ar.alloc([4, 1], F32)
            b_h = [Buf() for _ in range(4)]
            XC = [ar.alloc([512], F32) for _ in range(3)]
            b_XC = [Buf() for _ in range(3)]
            RRb = [ar.alloc([512], F32) for _ in range(2)]
            b_RR = [Buf() for _ in range(2)]
            IIb = [ar.alloc([512], F32) for _ in range(2)]
            b_II = [Buf() for _ in range(2)]
            AAb = [ar.alloc([512], F32) for _ in range(2)]
            b_AA = [Buf() for _ in range(2)]
            HHb = ar.alloc([512], F32)
            b_HH = Buf()
            RQb = ar.alloc([512], F32)
            b_RQ = Buf()
            GEb = [ar.alloc([512], BF16) for _ in range(5)]
            b_GE = [Buf() for _ in range(5)]
            g0 = ar.alloc([4, NH, 16], F32)
            g1 = ar.alloc([4, NH, 16], F32)
            ge = ar.alloc([4, NH, 16], F32)
            b_g0, b_g1, b_ge = Buf(), Buf(), Buf()
            xcb = [ar.alloc([512], BF16) for _ in range(2)]
            b_xcb = [Buf(), Buf()]
            recf = ar.alloc([4, 512], F32)
            b_recf = [Buf() for _ in range(4)]
            recn = ar.alloc([4, 512], BF16)
            b_recn = Buf()
            rstd = ar.alloc([512], F32)
            b_rstd = Buf()
            gsb = ar.alloc([512], F32)
            b_gsb = Buf()

            def p1_xload(g, j):
                t_ = 4 * g + j
                P.dma('pool', lambda e, t_=t_, j=j: e.dma_start(out=xbf[:, j, :], in_=xsrc[t_ * 128:(t_ + 1) * 128, :]),
                      r=[b_xres[t_]] if l > 0 else [], w=[b_xbf[j]])

            def A_tile(g, j):
                xi = xbf
                bxi = b_xbf[j]
                t = 4 * g + j
                xT = xT2[g % 2]
                b_xT = b_xT2[g % 2]
                pb = bank_bf(0)
                for k in range(8):
                    P.pe(lambda e, j=j, k=k, pb=pb, xi=xi: e.transpose(out=pb[:, k * 128:(k + 1) * 128],
                                                                     in_=xi[:, j, k * 128:(k + 1) * 128], identity=idb),
                         r=[bxi, b_idb], w=[bb[0]])
                P.act(lambda e, j=j, pb=pb, xT=xT: e.copy(out=xT[:, :, j * 128:(j + 1) * 128],
                                                         in_=pb.rearrange("p (k t) -> p k t", t=128)),
                      r=[bb[0]], w=[b_xT[j]])
                if g + 1 < NG:
                    p1_xload(g + 1, j)
                for which in range(3):
                    bk = 2 + which
                    for k in range(8):
                        P.pe(lambda e, j=j, k=k, which=which, bk=bk: e.matmul(
                            banks[bk][:, :], lhsT=xT[:, k, j * 128:(j + 1) * 128],
                            rhs=w_in_sb[:, k, which * 512:(which + 1) * 512], start=(k == 0), stop=(k == 7)),
                            r=[b_xT[j]] + b_win, w=[bb[bk]])
                for which, dst in ((0, qb), (1, kb)):
                    bk = 2 + which
                    pv = banks[bk][:, :].rearrange("p (h d) -> p h d", d=64)
                    t1 = pv[:, :, 0:8]
                    t2 = pv[:, :, 8:16]
                    cs = cos_sb[:, t, :].unsqueeze(1).to_broadcast([128, NH, 8])
                    sn = sin_sb[:, t, :].unsqueeze(1).to_broadcast([128, NH, 8])
                    bdst = b_qb[j] if which == 0 else b_kb[j]
                    P.dve(lambda e, t1=t1, cs=cs: e.tensor_tensor(out=rt[0], in0=t1, in1=cs, op=ALU.mult),
                          r=[bb[bk], b_tab], w=[b_rt[0]])
                    P.dve(lambda e, t2=t2, sn=sn: e.tensor_tensor(out=rt[1], in0=t2, in1=sn, op=ALU.mult),
                          r=[bb[bk], b_tab2], w=[b_rt[1]])
                    P.dve(lambda e, t2=t2, cs=cs: e.tensor_tensor(out=rt[2], in0=t2, in1=cs, op=ALU.mult),
                          r=[bb[bk], b_tab], w=[b_rt[2]])
                    P.dve(lambda e, t1=t1, sn=sn: e.tensor_tensor(out=rt[3], in0=t1, in1=sn, op=ALU.mult),
                          r=[bb[bk], b_tab2], w=[b_rt[3]])
                    P.dve(lambda e, dst=dst, j=j: e.tensor_tensor(out=dst[:, j, :, 0:8], in0=rt[0], in1=rt[1], op=ALU.subtract),
                          r=[b_rt[0], b_rt[1]], w=[bdst])
                    P.dve(lambda e, dst=dst, j=j: e.tensor_tensor(out=dst[:, j, :, 8:16], in0=rt[2], in1=rt[3], op=ALU.add),
                          r=[b_rt[2], b_rt[3]], w=[bdst])
                    P.act(lambda e, dst=dst, j=j, pv=pv: e.copy(out=dst[:, j, :, 16:64], in_=pv[:, :, 16:64]),
                          r=[bb[bk]], w=[bdst])
                pvv = banks[4][:, :].rearrange("p (h d) -> p h d", d=64)
                P.act(lambda e, t=t, pvv=pvv: e.copy(out=vsb[:, t, :, 0:64], in_=pvv), r=[bb[4]], w=[b_v[t]])
                P.pool(lambda e, t=t: e.memset(vsb[:, t, :, 64:65], 1.0), r=[], w=[b_v[t]])
                pb = bank_bf(7)
                for h in range(NH):
                    P.pe(lambda e, j=j, h=h, pb=pb: e.transpose(out=pb[0:64, h * 128:(h + 1) * 128],
                                                              in_=kb[:, j, h, :], identity=idb),
                         r=[b_kb[j], b_idb], w=[bb[7]])
                P.act(lambda e, j=j, pb=pb: e.copy(out=kTh[:, :, j * 128:(j + 1) * 128],
                                                  in_=pb[0:64, :].rearrange("p (h t) -> p h t", t=128)),
                      r=[bb[7]], w=[b_kTh[j]])
                pb6 = bank_bf(6)
                for pr in range(4):
                    for w2 in range(2):
                        P.pe(lambda e, j=j, pr=pr, w2=w2, pb6=pb6: e.transpose(
                            out=pb6[w2 * 64:(w2 + 1) * 64, pr * 128:(pr + 1) * 128],
                            in_=qb[:, j, 2 * pr + w2, 0:64], identity=idb),
                            r=[b_qb[j], b_idb], w=[bb[6]])
                P.act(lambda e, j=j, pb6=pb6: e.copy(out=qTp[:, :, j * 128:(j + 1) * 128],
                                                    in_=pb6[:, 0:512].rearrange("p (h t) -> p h t", t=128)),
                      r=[bb[6]], w=[b_qTp[j]])

            def B_grp(g):
                for h in range(NH):
                    P.dma('sp', lambda e, h=h, g=g: e.dma_start(out=kT_d[h, :, g * 512:(g + 1) * 512], in_=kTh[:, h, :]),
                          r=b_kTh, w=[b_kT[h][g]])
                P.dve(lambda e: e.tensor_reduce(out=ksum, in_=kTh.rearrange("p h (b t) -> p h b t", t=256),
                                                 axis=AX.X, op=ALU.add), r=b_kTh, w=[b_ksum])
                ks4 = ksum.rearrange("p (pr w) b -> p pr w b", w=2)
                P.dve(lambda e, g=g, ks4=ks4: e.tensor_scalar(
                    out=kmeanBD[0:64, :, 0, 2 * g:2 * g + 2], in0=ks4[:, :, 0, :], scalar1=1.0 / 256.0, scalar2=None,
                    op0=ALU.mult), r=[b_ksum], w=[b_kmean])
                odd_bf = oddk
                P.dve(lambda e, ks4=ks4, odd_bf=odd_bf: e.tensor_scalar(
                    out=odd_bf.rearrange("p (pr b) -> p pr b", b=2), in0=ks4[:, :, 1, :], scalar1=1.0 / 256.0,
                    scalar2=None, op0=ALU.mult), r=[b_ksum], w=[b_oddk])
                P.pe(lambda e, odd_bf=odd_bf: e.matmul(banks[5][:, 0:8], lhsT=shiftb[0:64, :], rhs=odd_bf,
                                                       start=True, stop=True),
                     r=[b_oddk, b_shift], w=[bb[5]])
                P.act(lambda e, g=g: e.copy(out=kmeanBD[64:128, :, 1, 2 * g:2 * g + 2],
                                            in_=banks[5][64:128, 0:8].rearrange("p (pr b) -> p pr b", b=2)),
                      r=[bb[5]], w=[b_kmean])
                for j in range(4):
                    for pr in range(4):
                        P.pe(lambda e, j=j, pr=pr: e.matmul(
                            banks[5][:, (j * 4 + pr) * 32:(j * 4 + pr + 1) * 32],
                            lhsT=qTp[:, pr, j * 128:(j + 1) * 128],
                            rhs=kmeanBD[:, pr, :, :].rearrange("p w n -> p (w n)"), start=True, stop=True),
                            r=[b_qTp[j], b_kmean], w=[bb[5]])
                P.act(lambda e: e.copy(out=gsb, in_=banks[5][:, :]), r=[bb[5]], w=[b_gsb])
                yield
                gps = gsb.rearrange("p (j h n) -> p j h n", h=NH, n=16)
                gc = gcaus_sb[:, 4 * g:4 * g + 4, :].unsqueeze(2).to_broadcast([128, 4, NH, 16])
                go = gown_sb[:, 4 * g:4 * g + 4, :].unsqueeze(2).to_broadcast([128, 4, NH, 16])
                P.dve(lambda e, gps=gps, gc=gc: e.tensor_tensor(out=g0, in0=gps, in1=gc, op=ALU.add),
                      r=[b_gsb, b_tab3], w=[b_g0])
                P.dve(lambda e: e.tensor_copy(out=g1, in_=g0), r=[b_g0], w=[b_g1])
                for rnd in range(3):
                    P.dve(lambda e: e.tensor_reduce(out=gm, in_=g1, axis=AX.X, op=ALU.max), r=[b_g1], w=[b_gm])
                    P.dve(lambda e: e.tensor_tensor(out=ge, in0=g1, in1=gm.unsqueeze(3).to_broadcast([128, 4, NH, 16]),
                                                     op=ALU.is_ge), r=[b_g1, b_gm], w=[b_ge])
                    P.dve(lambda e: e.scalar_tensor_tensor(
                        out=g1.rearrange("p j h n -> p (j h n)"), in0=ge.rearrange("p j h n -> p (j h n)"),
                        scalar=-1e9, in1=g1.rearrange("p j h n -> p (j h n)"), op0=ALU.mult, op1=ALU.add),
                        r=[b_ge, b_g1], w=[b_g1])
                P.dve(lambda e: e.scalar_tensor_tensor(
                    out=ge.rearrange("p j h n -> p (j h n)"), in0=g0.rearrange("p j h n -> p (j h n)"),
                    scalar=-5e8, in1=g1.rearrange("p j h n -> p (j h n)"), op0=ALU.add, op1=ALU.is_lt),
                    r=[b_g0, b_g1], w=[b_ge])
                for j in range(4):
                    P.dve(lambda e, j=j, go=go: e.tensor_tensor(out=qb[:, j, :, 64:80], in0=ge[:, j, :, :],
                                                                 in1=go[:, j, :, :], op=ALU.mult),
                          r=[b_ge, b_tab4], w=[b_qb[j]])
                yield
                for j in range(4):
                    pb = bank_bf(7)
                    for h in range(NH):
                        P.pe(lambda e, j=j, h=h, pb=pb: e.transpose(out=pb[0:80, h * 128:(h + 1) * 128],
                                                                  in_=qb[:, j, h, :], identity=idb),
                             r=[b_qb[j], b_idb], w=[bb[7]])
                    P.act(lambda e, j=j, pb=pb: e.copy(out=qTa[:, :, j * 128:(j + 1) * 128],
                                                      in_=pb[0:80, :].rearrange("p (h t) -> p h t", t=128)),
                          r=[bb[7]], w=[b_qTah[j]])
                for h in range(NH):
                    P.dma('sp', lambda e, h=h, g=g: e.dma_start(out=qTa_d[h, :, g * 512:(g + 1) * 512], in_=qTa[:, h, :]),
                          r=b_qTah, w=[b_qTa[h][g]])


            def R0(q):
                g, c = divmod(q, 4)
                xT = xT2[g % 2]
                b_xT = b_xT2[g % 2]
                bkx = 2 + (q % 2)
                for k in range(8):
                    P.pe(lambda e, c=c, k=k, bkx=bkx, xT=xT: e.matmul(
                        banks[bkx][:, :], lhsT=w_in_sb[:, k, 1536 + c * 128:1536 + (c + 1) * 128],
                        rhs=xT[:, k, :], start=(k == 0), stop=(k == 7)),
                        r=b_xT + b_win, w=[bb[bkx]])
                P.act(lambda e, c=c, bkx=bkx: e.copy(out=xr_sb[:, c, 3:515], in_=banks[bkx][:, :]),
                      r=[bb[bkx]], w=[b_xr[c]])
                bkr = 2 + ((q + 1) % 2)
                for k in range(8):
                    P.pe(lambda e, c=c, k=k, bkr=bkr, xT=xT: e.matmul(
                        banks[bkr][:, :], lhsT=w_in_sb[:, k, 2048 + c * 128:2048 + (c + 1) * 128],
                        rhs=xT[:, k, :], start=(k == 0), stop=(k == 7)),
                        r=b_xT + b_win, w=[bb[bkr]])
                gi = q % 5
                P.act(lambda e, gi=gi, bkr=bkr: e.activation(out=GEb[gi], in_=banks[bkr][:, :], func=AF.Gelu_apprx_tanh),
                      r=[bb[bkr]], w=[b_GE[gi]])

            def R1(q):
                g, c = divmod(q, 4)
                xc = XC[q % 3]
                bxc = b_XC[q % 3]
                P.dve(lambda e, c=c, xc=xc: e.tensor_scalar(
                    out=xc, in0=xr_sb[:, c, 0:512], scalar1=convw_sb[:, c, 0:1], scalar2=convb_sb[:, c:c + 1],
                    op0=ALU.mult, op1=ALU.add), r=[b_xr[c], b_p[0], b_p[1]], w=[bxc])
                for jj in range(1, 4):
                    P.dve(lambda e, c=c, xc=xc, jj=jj: e.scalar_tensor_tensor(
                        out=xc, in0=xr_sb[:, c, jj:jj + 512], scalar=convw_sb[:, c, jj:jj + 1], in1=xc,
                        op0=ALU.mult, op1=ALU.add), r=[b_xr[c], b_p[0], bxc], w=[bxc])
                P.pool(lambda e, c=c: e.tensor_copy(out=xr_sb[:, c, 0:3], in_=xr_sb[:, c, 512:515]),
                       r=[b_xr[c]], w=[b_xr[c]])
                s2 = q % 2
                P.act(lambda e, xc=xc, s2=s2: e.copy(out=xcb[s2], in_=xc), r=[bxc], w=[b_xcb[s2]])

            def R2(q):
                g, c = divmod(q, 4)
                s2 = q % 2
                P.pe(lambda e, c=c, s2=s2: e.matmul(banks[5][:, :], lhsT=wa_sb[:, c, :], rhs=xcb[s2], start=True, stop=True),
                     r=[b_wa, b_xcb[s2]], w=[bb[5]])
                P.act(lambda e, c=c, s2=s2: e.activation(out=RRb[s2], in_=banks[5][:, :], func=AF.Sigmoid,
                                                         bias=ba_sb[:, c:c + 1]), r=[bb[5], b_p[2]], w=[b_RR[s2]])
                P.pe(lambda e, c=c, s2=s2: e.matmul(banks[6][:, :], lhsT=wi_sb[:, c, :], rhs=xcb[s2], start=True, stop=True),
                     r=[b_wi, b_xcb[s2]], w=[bb[6]])
                P.act(lambda e, c=c, s2=s2: e.activation(out=IIb[s2], in_=banks[6][:, :], func=AF.Sigmoid,
                                                         bias=bi_sb[:, c:c + 1]), r=[bb[6], b_p[3]], w=[b_II[s2]])

            def R3a(q):
                g, c = divmod(q, 4)
                s2 = q % 2
                xc = XC[q % 3]
                bxc = b_XC[q % 3]
                P.act(lambda e, c=c, s2=s2: e.activation(out=AAb[s2], in_=RRb[s2], func=AF.Exp, scale=cdec_sb[:, c:c + 1]),
                      r=[b_RR[s2], b_cdec], w=[b_AA[s2]])
                P.act(lambda e, s2=s2: e.activation(out=RRb[s2], in_=AAb[s2], func=AF.Square), r=[b_AA[s2]], w=[b_RR[s2]])
                P.act(lambda e, s2=s2: e.activation(out=RRb[s2], in_=RRb[s2], func=AF.Sqrt, scale=-1.0, bias=1.0),
                      r=[b_RR[s2]], w=[b_RR[s2]])
                P.pool(lambda e, s2=s2, xc=xc: e.tensor_tensor(out=IIb[s2], in0=IIb[s2], in1=xc, op=ALU.mult),
                       r=[b_II[s2], bxc], w=[b_II[s2]])

            def R3d(q):
                s2 = q % 2
                P.dve(lambda e, s2=s2: e.tensor_tensor(out=IIb[s2], in0=IIb[s2], in1=RRb[s2], op=ALU.mult),
                      r=[b_II[s2], b_RR[s2]], w=[b_II[s2]])

            def R4d(q):
                g, c = divmod(q, 4)
                s2 = q % 2
                gi = q % 5
                if g == 0:
                    P.dve(lambda e, s2=s2: e.tensor_tensor_scan(out=HHb, data0=AAb[s2], data1=IIb[s2], initial=0.0,
                                                                op0=ALU.mult, op1=ALU.add),
                          r=[b_AA[s2], b_II[s2]], w=[b_HH])
                else:
                    P.dve(lambda e, s2=s2, c=c: e.tensor_tensor_scan(out=HHb, data0=AAb[s2], data1=IIb[s2],
                                                                     initial=hst[:, c, 0:1], op0=ALU.mult, op1=ALU.add),
                          r=[b_AA[s2], b_II[s2], b_h[c]], w=[b_HH])
                P.pool(lambda e, c=c: e.tensor_copy(out=hst[:, c, 0:1], in_=HHb[:, 511:512]), r=[b_HH], w=[b_h[c]])
                P.dve(lambda e, c=c, gi=gi: e.tensor_tensor(out=recf[:, c, :], in0=HHb, in1=GEb[gi], op=ALU.mult),
                      r=[b_HH, b_GE[gi]], w=[b_recf[c]])

            def R4a(q):
                g, c = divmod(q, 4)
                P.act(lambda e, c=c: e.activation(out=RQb, in_=recf[:, c, :], func=AF.Square), r=[b_recf[c]], w=[b_RQ])

            def R4p(q):
                g, c = divmod(q, 4)
                P.pe(lambda e, c=c: e.matmul(banks[1][:, :], lhsT=ones_f, rhs=RQb, start=(c == 0), stop=(c == 3)),
                     r=[b_ones, b_RQ], w=[bb[1]])
                if c == 3:
                    C_final(g)

            def C_final(g):
                P.act(lambda e: e.activation(out=rstd, in_=banks[1][:, :], func=AF.Sqrt, scale=1.0 / 512.0, bias=RMS_EPS),
                      r=[bb[1]], w=[b_rstd])
                P.dve(lambda e: e.reciprocal(out=rstd, in_=rstd), r=[b_rstd], w=[b_rstd])
                for c in range(4):
                    P.dve(lambda e, c=c: e.scalar_tensor_tensor(out=recn[:, c, :], in0=recf[:, c, :], scalar=grec_sb[:, c:c + 1],
                                                                 in1=rstd, op0=ALU.mult, op1=ALU.mult),
                          r=[b_recf[c], b_rstd, b_p[5]], w=[b_recn])
                P.dma('sp', lambda e, g=g: e.dma_start(out=recT_d[:, :, g * 512:(g + 1) * 512].rearrange("c p t -> p c t"),
                                                      in_=recn), r=[b_recn], w=[b_recT[g]])

            for j in range(4):
                p1_xload(0, j)
            for j in range(4):
                A_tile(0, j)
            NQ = 4 * NG
            bgen = None
            for it in range(NQ + 4):
                if 0 <= it - 3 < NQ:
                    R3a(it - 3)
                if 0 <= it - 4 < NQ:
                    R4d(it - 4)
                if 0 <= it - 3 < NQ:
                    R3d(it - 3)
                if 0 <= it - 4 < NQ:
                    R4a(it - 4)
                if 0 <= it - 2 < NQ:
                    R2(it - 2)
                if 0 <= it - 1 < NQ:
                    R1(it - 1)
                if it < NQ:
                    R0(it)
                if 0 <= it - 4 < NQ:
                    R4p(it - 4)
                gq, m = divmod(it, 4)
                if gq < NG:
                    if m == 0:
                        bgen = B_grp(gq)
                    if bgen is not None:
                        next(bgen, None)
                    if m == 2:
                        for _ in bgen:
                            pass
                        bgen = None
                    if gq + 1 < NG:
                        if m == 2:
                            A_tile(gq + 1, 0)
                            A_tile(gq + 1, 1)
                        if m == 3:
                            A_tile(gq + 1, 2)
                            A_tile(gq + 1, 3)
            P.barrier()

            if l >= 1 and '2' in SKIP1:
                return
            ar.release(mid_mark)
            qh = [ar.alloc([S], BF16, parts=80) for _ in range(2)]
            kh = [ar.alloc([S], BF16, parts=80) for _ in range(2)]
            b_qh = [Buf(), Buf()]
            b_kh = [Buf(), Buf()]
            b_khot = [Buf(), Buf()]
            khot_f = ar.alloc([S], F32, parts=80)
            b_khf = Buf()
            P.dma('sp', lambda e: e.dma_start(out=khot_f[64:80, :], in_=khot_d[:, :]), w=[b_khf])
            for i in range(2):
                P.dve(lambda e, i=i: e.tensor_copy(out=kh[i][64:80, :], in_=khot_f[64:80, :]), r=[b_khf], w=[b_khot[i]])
            zt = ar.alloc([8, D], BF16)
            b_zt = Buf()
            P.dve(lambda e: e.memset(zt, 0.0), w=[b_zt])
            NPT = 4
            pT = [ar.alloc([2, 256], BF16) for _ in range(NPT)]
            b_pT = [Buf() for _ in range(NPT)]
            rcp = [ar.alloc([2], F32) for _ in range(2)]
            b_rcp = [Buf(), Buf()]
            SB = (0, 1, 2, 7)
            OB = ((3, 4), (5, 6))
            units = []
            for h in range(NH):
                for bi in range(NB):
                    for n in range(bi + 1):
                        units.append((h, bi, n))
            cnt_state = {}

            def stageA(u, ui):
                h, bi, n = u
                i = h % 2
                if bi == 0 and n == 0:
                    P.dma('sp', lambda e, h=h, i=i: e.dma_start(out=qh[i], in_=qTa_d[h, :, :]), r=b_qTa[h], w=[b_qh[i]])
                    P.dma('sp', lambda e, h=h, i=i: e.dma_start(out=kh[i][0:64, :], in_=kT_d[h, :, :]), r=b_kT[h], w=[b_kh[i]])
                q0 = bi * 256
                sb_i = SB[ui % 4]
                sps = banks[sb_i][:, :].rearrange("p (t q) -> p t q", q=256)
                if n != bi:
                    for tt in range(2):
                        kt = 2 * n + tt
                        P.pe(lambda e, i=i, kt=kt, tt=tt, sps=sps, q0=q0: e.matmul(
                            sps[:, tt, :], lhsT=kh[i][:, kt * 128:(kt + 1) * 128], rhs=qh[i][:, q0:q0 + 256],
                            start=True, stop=True), r=[b_kh[i], b_khot[i], b_qh[i]], w=[bb[sb_i]])
                else:
                    kt0 = 2 * bi
                    kt1 = 2 * bi + 1
                    P.pe(lambda e, i=i, kt0=kt0, sps=sps, q0=q0: e.matmul(
                        sps[:, 0, :], lhsT=kh[i][:, kt0 * 128:(kt0 + 1) * 128], rhs=qh[i][:, q0:q0 + 256],
                        start=True, stop=False), r=[b_kh[i], b_khot[i], b_qh[i]], w=[bb[sb_i]])
                    P.pe(lambda e, sps=sps: e.matmul(sps[:, 0, 0:128], lhsT=idb, rhs=trib, start=False, stop=True),
                         r=[b_idb, b_trib], w=[bb[sb_i]])
                    P.pe(lambda e, i=i, kt1=kt1, sps=sps, q0=q0: e.matmul(
                        sps[:, 1, 128:256], lhsT=kh[i][:, kt1 * 128:(kt1 + 1) * 128],
                        rhs=qh[i][:, q0 + 128:q0 + 256], start=True, stop=False),
                        r=[b_kh[i], b_khot[i], b_qh[i]], w=[bb[sb_i]])
                    P.pe(lambda e, sps=sps: e.matmul(sps[:, 1, 128:256], lhsT=idb, rhs=trib, start=False, stop=True),
                         r=[b_idb, b_trib], w=[bb[sb_i]])

            def stageB(u, ui):
                h, bi, n = u
                sb_i = SB[ui % 4]
                sps = banks[sb_i][:, :].rearrange("p (t q) -> p t q", q=256)
                pt = pT[ui % NPT]
                bpt = b_pT[ui % NPT]
                if n != bi:
                    P.act(lambda e, pt=pt, sps=sps: e.activation(out=pt, in_=sps, func=AF.Exp, scale=0.125),
                          r=[bb[sb_i]], w=[bpt])
                else:
                    P.act(lambda e, pt=pt, sps=sps: e.activation(out=pt[:, 0, :], in_=sps[:, 0, :], func=AF.Exp, scale=0.125),
                          r=[bb[sb_i]], w=[bpt])
                    P.act(lambda e, pt=pt, sps=sps: e.activation(out=pt[:, 1, 128:256], in_=sps[:, 1, 128:256],
                                                                func=AF.Exp, scale=0.125),
                          r=[bb[sb_i]], w=[bpt])

            def stageC(u, ui):
                h, bi, n = u
                ob = OB[bi % 2]
                pt = pT[ui % NPT]
                bpt = b_pT[ui % NPT]
                nmm = [2 * bi + 1, 2 * bi + 2]
                if n == 0:
                    cnt_state[(h, bi)] = [0, 0]
                cntj = cnt_state[(h, bi)]
                if n != bi:
                    lst = [(jq, tt, 2 * n + tt) for jq in range(2) for tt in range(2)]
                else:
                    lst = [(0, 0, 2 * bi), (1, 0, 2 * bi), (1, 1, 2 * bi + 1)]
                for (jq, tt, kt) in lst:
                    first = (cntj[jq] == 0)
                    cntj[jq] += 1
                    last = (cntj[jq] == nmm[jq])
                    P.pe(lambda e, jq=jq, tt=tt, kt=kt, pt=pt, h=h, first=first, last=last, ob=ob: e.matmul(
                        banks[ob[jq]][:, 0:65], lhsT=pt[:, tt, jq * 128:(jq + 1) * 128],
                        rhs=vsb[:, kt, h, :], start=first, stop=last),
                        r=[bpt, b_v[kt]], w=[bb[ob[jq]]])
                if n == bi:
                    assert cntj == nmm
                    rc = rcp[bi % 2]
                    brc = b_rcp[bi % 2]
                    for jq in range(2):
                        t = 2 * bi + jq
                        P.dve(lambda e, jq=jq, rc=rc, ob=ob: e.reciprocal(out=rc[:, jq:jq + 1], in_=banks[ob[jq]][:, 64:65]),
                              r=[bb[ob[jq]]], w=[brc])
                        P.dve(lambda e, jq=jq, rc=rc, ob=ob, t=t, h=h: e.tensor_scalar(
                            out=attn_sb[:, t, h * 64:(h + 1) * 64], in0=banks[ob[jq]][:, 0:64], scalar1=rc[:, jq:jq + 1],
                            scalar2=None, op0=ALU.mult), r=[bb[ob[jq]], brc], w=[b_attn[t][h]])

            LAG = 2
            NU = len(units)
            casts = []
            for ex in range(NEXP):
                casts.append((wgb_d, wg_d, ex, D))
                casts.append((wub_d, wu_d, ex, D))
                casts.append((wdb_d, wd_d, ex, DE))
            cast_every = max(1, (NU - 8) // len(casts))
            ci = 0
            zi_c = [0]
            zero_every = max(6, (NU - 16) // (NSLOT // 1024))
            for ui in range(NU + LAG):
                if ui < NU:
                    stageA(units[ui], ui)
                    stageB(units[ui], ui)
                if ui >= LAG:
                    stageC(units[ui - LAG], ui - LAG)
                if ui % zero_every == 5 and zi_c[0] < NSLOT // 1024:
                    zi = zi_c[0]
                    zi_c[0] += 1
                    P.dma('sp', lambda e, zi=zi: e.dma_start(
                        out=xs_d[zi * 1024:(zi + 1) * 1024, :].rearrange("(a p) d -> p a d", p=128), in_=zt),
                        r=[b_zt], w=[b_xs_z[zi]])
                if ui % cast_every == 0 and ci < len(casts):
                    dst_t, src_t, ex, rows = casts[ci]
                    ci += 1
                    P.dma('pool', lambda e, dst_t=dst_t, src_t=src_t, ex=ex, rows=rows: e.dma_start(
                        out=dst_t[l][ex].rearrange("p (k n) -> p k n", k=rows // 128),
                        in_=src_t[l, ex].rearrange("(k p) n -> p k n", p=128)), r=[b_wc_w])
            while zi_c[0] < NSLOT // 1024:
                zi = zi_c[0]
                zi_c[0] += 1
                P.dma('sp', lambda e, zi=zi: e.dma_start(
                    out=xs_d[zi * 1024:(zi + 1) * 1024, :].rearrange("(a p) d -> p a d", p=128), in_=zt),
                    r=[b_zt], w=[b_xs_z[zi]])
            while ci < len(casts):
                dst_t, src_t, ex, rows = casts[ci]
                ci += 1
                P.dma('pool', lambda e, dst_t=dst_t, src_t=src_t, ex=ex, rows=rows: e.dma_start(
                    out=dst_t[l][ex].rearrange("p (k n) -> p k n", k=rows // 128),
                    in_=src_t[l, ex].rearrange("(k p) n -> p k n", p=128)), r=[b_wc_w])
            P.op('sp', lambda e: e.nop(), w=[b_wc_w, b_wc])
            P.barrier()

            if l >= 1 and '3' in SKIP1:
                return
            ar.release(mid_mark)
            w_out_sb = ar.alloc([8, D], BF16)
            b_wout = [Buf(), Buf()]
            for hh in range(2):
                P.dma('pool', lambda e, hh=hh: e.dma_start(
                    out=w_out_sb[:, hh * 4:(hh + 1) * 4, :],
                    in_=w_out_d[l].rearrange("(k p) n -> p k n", p=128)[:, hh * 4:(hh + 1) * 4, :]), w=[b_wout[hh]])
            gattn_sb = ar.alloc([512], F32)
            ln1g_sb = ar.alloc([D], F32)
            ln1b_sb = ar.alloc([D], F32)
            wr_sb = ar.alloc([8, 20], BF16)
            br_sb = ar.alloc([20], F32)
            b_c3 = [Buf() for _ in range(5)]
            P.dma('sp', lambda e: e.dma_start(out=gattn_sb, in_=gattn_d[l]), w=[b_c3[0]])
            P.dma('sp', lambda e: e.dma_start(out=ln1g_sb, in_=ln1g_d[l]), w=[b_c3[1]])
            P.dma('sp', lambda e: e.dma_start(out=ln1b_sb, in_=ln1b_d[l]), w=[b_c3[2]])
            P.dma('pool', lambda e: e.dma_start(out=wr_sb, in_=wr_d[l].rearrange("(k p) n -> p k n", p=128)), w=[b_c3[3]])
            P.dma('sp', lambda e: e.dma_start(out=br_sb, in_=br_d[l]), w=[b_c3[4]])
            NBUF = 4
            junk = [ar.alloc([512], BF16) for _ in range(NBUF)]
            ssq = [ar.alloc([2], F32) for _ in range(NBUF)]
            an = [ar.alloc([512], BF16) for _ in range(NBUF)]
            mixT = [ar.alloc([8, 128], BF16) for _ in range(NBUF)]
            xt3 = [ar.alloc([D], F32) for _ in range(NBUF)]
            y3 = [ar.alloc([D], F32) for _ in range(NBUF)]
            st3 = [ar.alloc([2, 6], F32) for _ in range(NBUF)]
            mv3 = [ar.alloc([2], F32) for _ in range(NBUF)]
            x1f = [ar.alloc([D], F32) for _ in range(NBUF)]
            x1b = [ar.alloc([D], BF16) for _ in range(NBUF)]
            x1T = [ar.alloc([8, 128], BF16) for _ in range(NBUF)]
            B3 = [{k: Buf() for k in ('junk', 'ssq', 'an', 'mixa', 'mixr', 'xt', 'y', 'st', 'mv', 'x1f', 'x1b', 'x1T')}
                  for _ in range(NBUF)]

            def p3_load(t):
                s = t % NBUF
                B = B3[s]
                g = t // 4
                P.dma('sp', lambda e, t=t, s=s: e.dma_start(
                    out=mixT[s][:, 4:8, :], in_=recT_d[:, :, t * 128:(t + 1) * 128].rearrange("c p t -> p c t")),
                    r=[b_recT[g]], w=[B['mixr']])
                P.dma('sp', lambda e, t=t, s=s: e.dma_start(out=xt3[s], in_=xsrc[t * 128:(t + 1) * 128, :]),
                      r=[b_xres[t]] if l > 0 else [], w=[B['xt']])

            def p3_A1(t):
                s = t % NBUF
                B = B3[s]
                P.act(lambda e, t=t, s=s: e.activation(out=junk[s], in_=attn_sb[:, t, :], func=AF.Square,
                                                       accum_out=ssq[s][:, 0:1]), r=b_attn[t], w=[B['junk'], B['ssq']])
                P.act(lambda e, s=s: e.activation(out=ssq[s][:, 1:2], in_=ssq[s][:, 0:1], func=AF.Sqrt,
                                                  scale=1.0 / 512.0, bias=RMS_EPS), r=[B['ssq']], w=[B['ssq']])
                P.dve(lambda e, s=s: e.reciprocal(out=ssq[s][:, 1:2], in_=ssq[s][:, 1:2]), r=[B['ssq']], w=[B['ssq']])
                P.dve(lambda e, t=t, s=s: e.scalar_tensor_tensor(out=an[s], in0=attn_sb[:, t, :], scalar=ssq[s][:, 1:2],
                                                                  in1=gattn_sb, op0=ALU.mult, op1=ALU.mult),
                      r=b_attn[t] + [B['ssq'], b_c3[0]], w=[B['an']])
                tb = 0 if t % 2 == 0 else 7
                pb = bank_bf(tb)
                for c in range(4):
                    P.pe(lambda e, c=c, s=s, pb=pb: e.transpose(out=pb[:, c * 128:(c + 1) * 128],
                                                              in_=an[s][:, c * 128:(c + 1) * 128], identity=idb),
                         r=[B['an'], b_idb], w=[bb[tb]])
                P.act(lambda e, s=s, pb=pb: e.copy(out=mixT[s][:, 0:4, :], in_=pb[:, 0:512].rearrange("p (c t) -> p c t", t=128)),
                      r=[bb[tb]], w=[B['mixa']])

            def p3_A2(t):
                s = t % NBUF
                B = B3[s]
                ob = 1 if t % 2 == 0 else 5
                for nh in range(2):
                    for k in range(8):
                        P.pe(lambda e, k=k, nh=nh, s=s, ob=ob: e.matmul(banks[ob + nh][:, :], lhsT=mixT[s][:, k, :],
                                                                        rhs=w_out_sb[:, k, nh * 512:(nh + 1) * 512],
                                                                        start=(k == 0), stop=(k == 7)),
                             r=[B['mixa'], B['mixr'], b_wout[0], b_wout[1]], w=[bb[ob + nh]])
                for nh in range(2):
                    P.dve(lambda e, nh=nh, s=s, ob=ob: e.scalar_tensor_tensor(
                        out=y3[s][:, nh * 512:(nh + 1) * 512], in0=xt3[s][:, nh * 512:(nh + 1) * 512], scalar=ALPHA,
                        in1=banks[ob + nh][:, :], op0=ALU.mult, op1=ALU.add), r=[B['xt'], bb[ob + nh]], w=[B['y']])
                ln_stats(P, y3[s], B['y'], st3[s], B['st'], mv3[s], B['mv'])

            def p3_A3(t):
                s = t % NBUF
                B = B3[s]
                ln_apply(P, y3[s], B['y'], mv3[s], B['mv'], ln1g_sb, b_c3[1], ln1b_sb, b_c3[2], x1f[s], B['x1f'])
                P.dma('sp', lambda e, t=t, s=s: e.dma_start(out=xres_d[t * 128:(t + 1) * 128, :], in_=x1f[s]),
                      r=[B['x1f']], w=[b_xres[t]])
                if dbg and l == DBG_LAYER:
                    P.dma('sp', lambda e, t=t, s=s: e.dma_start(out=dbg_d[t * 128:(t + 1) * 128, :], in_=x1f[s]),
                          r=[B['x1f'], b_out_done])
                P.act(lambda e, s=s: e.copy(out=x1b[s], in_=x1f[s]), r=[B['x1f']], w=[B['x1b']])

            def p3_B(t):
                s = t % NBUF
                B = B3[s]
                pb = bank_bf(3)
                for k in range(8):
                    P.pe(lambda e, k=k, s=s, pb=pb: e.transpose(out=pb[:, k * 128:(k + 1) * 128],
                                                              in_=x1b[s][:, k * 128:(k + 1) * 128], identity=idb),
                         r=[B['x1b'], b_idb], w=[bb[3]])
                P.act(lambda e, s=s, pb=pb: e.copy(out=x1T[s], in_=pb.rearrange("p (k t) -> p k t", t=128)),
                      r=[bb[3]], w=[B['x1T']])
                for k in range(8):
                    P.pe(lambda e, k=k, s=s: e.matmul(banks[4][:, 0:20], lhsT=x1T[s][:, k, :], rhs=wr_sb[:, k, :],
                                                      start=(k == 0), stop=(k == 7)),
                         r=[B['x1T'], b_c3[3]], w=[bb[4]])
                P.dve(lambda e, t=t: e.tensor_tensor(out=lg_sb[:, t, :], in0=banks[4][:, 0:20], in1=br_sb, op=ALU.add),
                      r=[bb[4], b_c3[4]], w=[b_lg[t]])

            p3_load(0)
            for i in range(NT + 3):
                if i + 1 < NT:
                    p3_load(i + 1)
                if 0 <= i - 3 < NT:
                    p3_B(i - 3)
                if 0 <= i - 2 < NT:
                    p3_A3(i - 2)
                if 0 <= i - 1 < NT:
                    p3_A2(i - 1)
                if i < NT:
                    p3_A1(i)
            P.barrier()
            ar.release(base_mark)
            NQ = NT * 4
            LE = ar.alloc([NQ, 4], F32)
            EQ = ar.alloc([NQ, 4], F32)
            gsm = ar.alloc([6, NT, 4], F32)
            exg = gsm[:, 0, :, :]
            gm1 = gsm[:, 1, :, :]
            gmask = gsm[:, 2, :, :]
            m1 = gsm[:, 3, :, :].rearrange("p t g -> p (t g)")
            m2 = gsm[:, 4, :, :].rearrange("p t g -> p (t g)")
            den = gsm[:, 5, :, :].rearrange("p t g -> p (t g)")
            gs2 = ar.alloc([2, NT], F32)
            mg = gs2[:, 0, :]
            sgm = gs2[:, 1, :]
            RR = [Buf()]
            Lg = lg_sb[:, :, 0:4]
            P.dve(lambda e: e.tensor_reduce(out=mg, in_=Lg, axis=AX.X, op=ALU.max), r=b_lg, w=RR)
            mgb = mg.unsqueeze(2).to_broadcast([128, NT, 4])
            P.dve(lambda e: e.tensor_tensor(out=gm1, in0=Lg, in1=mgb, op=ALU.is_ge), r=b_lg + RR, w=RR)
            P.dve(lambda e: e.tensor_tensor(out=exg, in0=Lg, in1=mgb, op=ALU.subtract), r=b_lg + RR, w=RR)
            P.act(lambda e: e.activation(out=exg, in_=exg, func=AF.Exp), r=RR, w=RR)
            P.dve(lambda e: e.tensor_reduce(out=sgm, in_=exg, axis=AX.X, op=ALU.add), r=RR, w=RR)
            P.dve(lambda e: e.reciprocal(out=sgm, in_=sgm), r=RR, w=RR)
            P.dve(lambda e: e.tensor_tensor(out=gmask, in0=gm1, in1=sgm.unsqueeze(2).to_broadcast([128, NT, 4]), op=ALU.mult),
                  r=RR, w=RR)
            LEt = LE.rearrange("p (t g) e -> p t (g e)", g=4)
            LEf = LE.rearrange("p q e -> p (q e)")
            EQf = EQ.rearrange("p q e -> p (q e)")
            P.dve(lambda e: e.tensor_copy(out=LEt, in_=lg_sb[:, :, 4:20]), r=b_lg, w=RR)
            m1b = m1.unsqueeze(2).to_broadcast([128, NQ, 4])
            m2b = m2.unsqueeze(2).to_broadcast([128, NQ, 4])
            P.dve(lambda e: e.tensor_reduce(out=m1, in_=LE, axis=AX.X, op=ALU.max), r=RR, w=RR)
            P.dve(lambda e: e.tensor_tensor(out=EQ, in0=LE, in1=m1b, op=ALU.is_ge), r=RR, w=RR)
            P.dve(lambda e: e.scalar_tensor_tensor(out=EQf, in0=EQf, scalar=-1e9, in1=LEf, op0=ALU.mult, op1=ALU.add), r=RR, w=RR)
            P.dve(lambda e: e.tensor_reduce(out=m2, in_=EQ, axis=AX.X, op=ALU.max), r=RR, w=RR)
            P.dve(lambda e: e.tensor_tensor(out=EQ, in0=LE, in1=m2b, op=ALU.is_ge), r=RR, w=RR)
            gm1b = gm1.rearrange("p t g -> p (t g)").unsqueeze(2).to_broadcast([128, NQ, 4])
            P.dve(lambda e: e.tensor_tensor(out=sel_sb.rearrange("p t (g e) -> p (t g) e", e=4), in0=EQ, in1=gm1b, op=ALU.mult),
                  r=RR, w=b_sel)
            P.dve(lambda e: e.tensor_tensor(out=LE, in0=LE, in1=m1b, op=ALU.subtract), r=RR, w=RR)
            P.act(lambda e: e.activation(out=LEf, in_=LEf, func=AF.Exp), r=RR, w=RR)
            P.dve(lambda e: e.tensor_tensor(out=LEf, in0=LEf, in1=EQf, op=ALU.mult), r=RR, w=RR)
            P.dve(lambda e: e.tensor_reduce(out=den, in_=LE, axis=AX.X, op=ALU.add), r=RR, w=RR)
            P.dve(lambda e: e.reciprocal(out=den, in_=den), r=RR, w=RR)
            P.dve(lambda e: e.tensor_tensor(out=den, in0=den, in1=gmask.rearrange("p t g -> p (t g)"), op=ALU.mult), r=RR, w=RR)
            P.dve(lambda e: e.tensor_tensor(out=comb_sb.rearrange("p t (g e) -> p (t g) e", e=4), in0=LE,
                                             in1=den.unsqueeze(2).to_broadcast([128, NQ, 4]), op=ALU.mult), r=RR, w=b_comb)
            selflat = sel_sb.rearrange("p t e -> p (t e)")
            P.pe(lambda e: e.matmul(banks[0][:, 0:NT * 16], lhsT=ltrib, rhs=selflat, start=True, stop=True),
                 r=[b_ltri] + b_sel, w=[bb[0]])
            P.pe(lambda e: e.matmul(banks[1][:, 0:NT * 16], lhsT=onesb, rhs=selflat, start=True, stop=True),
                 r=[b_onesb] + b_sel, w=[bb[1]])
            cinc = ar.alloc([16, NT], F32)
            onesT = ar.alloc([NT], F32)
            b_cinc = Buf()
            P.dve(lambda e: e.memset(onesT, 1.0), w=[b_cinc])
            tot_et = banks[1][:, 0:NT * 16].rearrange("p (t e) -> p e t", e=16)
            for ee in range(16):
                P.dve(lambda e, ee=ee: e.tensor_tensor_scan(out=cinc[:, ee, :], data0=onesT, data1=tot_et[:, ee, :], initial=0.0,
                                                            op0=ALU.mult, op1=ALU.add), r=[bb[1], b_cinc], w=[b_cinc])
            P.dve(lambda e: e.tensor_copy(out=carry_sb, in_=cinc[:, :, NT - 1]), r=[b_cinc], w=[b_carry])
            P.dve(lambda e: e.tensor_tensor(out=rank_sb, in0=banks[0][:, 0:NT * 16].rearrange("p (t e) -> p t e", e=16),
                                             in1=cinc.rearrange("p e t -> p t e"), op=ALU.add), r=[bb[0], b_cinc], w=b_rank)
            P.dve(lambda e: e.tensor_tensor(out=rank_sb, in0=rank_sb, in1=banks[1][:, 0:NT * 16].rearrange("p (t e) -> p t e", e=16),
                                             op=ALU.subtract), r=[bb[1]] + b_rank, w=b_rank)
            jstart_sb = ar.alloc([32, 16], F32)
            b_jstart = Buf()
            P.dma('sp', lambda e: e.dma_start(out=jstart_sb, in_=jstart_d[:, :, :]), w=[b_jstart])
            seg = ar.alloc([8, 16], F32)
            segi = ar.alloc([16], I32)
            cmp3 = ar.alloc([32, 16], F32)
            ejf = ar.alloc([32], F32)
            big = [ar.alloc([NT, 16], F32) for _ in range(4)]
            red = ar.alloc([4, NT], F32)
            b_seg = Buf()
            RS = [b_seg]
            m_e = seg[:, 0, :]
            cend = seg[:, 1, :]
            s_e = seg[:, 2, :]
            ones16 = seg[:, 3, :]
            P.dve(lambda e: e.tensor_scalar(out=segi, in0=carry_sb, scalar1=511.0, scalar2=None, op0=ALU.add), r=[b_carry], w=RS)
            P.dve(lambda e: e.tensor_scalar(out=segi, in0=segi, scalar1=9, scalar2=9, op0=ALU.arith_shift_right,
                                             op1=ALU.logical_shift_left), r=RS, w=RS)
            P.dve(lambda e: e.tensor_copy(out=m_e, in_=segi), r=RS, w=RS)
            P.dve(lambda e: e.memset(ones16, 1.0), r=RS, w=RS)
            P.dve(lambda e: e.tensor_tensor_scan(out=cend, data0=ones16, data1=m_e, initial=0.0, op0=ALU.mult, op1=ALU.add),
                  r=RS, w=RS)
            P.dve(lambda e: e.tensor_tensor(out=s_e, in0=cend, in1=m_e, op=ALU.subtract), r=RS, w=RS)
            P.dve(lambda e: e.tensor_tensor(out=cmp3, in0=cend.unsqueeze(1).to_broadcast([128, 32, 16]), in1=jstart_sb,
                                             op=ALU.is_le), r=RS + [b_jstart], w=RS)
            P.dve(lambda e: e.tensor_reduce(out=ejf, in_=cmp3, axis=AX.X, op=ALU.add), r=RS, w=RS)
            P.dve(lambda e: e.tensor_scalar(out=ejc, in0=ejf, scalar1=15.0, scalar2=None, op0=ALU.min), r=RS, w=[b_ej])
            P.dve(lambda e: e.tensor_scalar(out=ejs, in0=ejc, scalar1=128.0, scalar2=0.0, op0=ALU.mult, op1=ALU.add),
                  r=[b_ej], w=[b_ej])
            P.dve(lambda e: e.tensor_scalar(out=idxw, in0=ejs, scalar1=wbase_sb[:, 0:1], scalar2=None, op0=ALU.add),
                  r=[b_ej, b_wbase], w=[b_ej])
            rk = rank_sb.rearrange("p t e -> p (t e)")
            sl2 = sel_sb.rearrange("p t e -> p (t e)")
            cb2 = comb_sb.rearrange("p t e -> p (t e)")
            v_, mk, tm, v2 = [b.rearrange("p t e -> p (t e)") for b in big]
            v3, mk3, tm3, v23 = big
            i0, i1, w0, w1 = red[:, 0, :], red[:, 1, :], red[:, 2, :], red[:, 3, :]
            RB = [Buf()]
            allr = b_rank + b_sel + b_comb
            P.dve(lambda e: e.tensor_tensor(out=v3, in0=rank_sb, in1=s_e.unsqueeze(1).to_broadcast([128, NT, 16]), op=ALU.add),
                  r=allr + RS, w=RB)
            P.dve(lambda e: e.scalar_tensor_tensor(out=v_, in0=v_, scalar=1.0, in1=sl2, op0=ALU.add, op1=ALU.mult), r=RB + allr, w=RB)
            P.dve(lambda e: e.tensor_reduce(out=i0, in_=v3, axis=AX.X, op=ALU.max), r=RB, w=RB)
            P.dve(lambda e: e.tensor_tensor(out=mk3, in0=v3, in1=i0.unsqueeze(2).to_broadcast([128, NT, 16]), op=ALU.is_equal),
                  r=RB, w=RB)
            P.dve(lambda e: e.tensor_tensor(out=tm, in0=mk, in1=cb2, op=ALU.mult), r=RB + allr, w=RB)
            P.dve(lambda e: e.tensor_reduce(out=w0, in_=tm3, axis=AX.X, op=ALU.add), r=RB, w=RB)
            P.dve(lambda e: e.tensor_tensor(out=tm, in0=mk, in1=v_, op=ALU.mult), r=RB, w=RB)
            P.dve(lambda e: e.tensor_tensor(out=v2, in0=v_, in1=tm, op=ALU.subtract), r=RB, w=RB)
            P.dve(lambda e: e.tensor_reduce(out=i1, in_=v23, axis=AX.X, op=ALU.max), r=RB, w=RB)
            P.dve(lambda e: e.tensor_tensor(out=mk3, in0=v23, in1=i1.unsqueeze(2).to_broadcast([128, NT, 16]), op=ALU.is_equal),
                  r=RB, w=RB)
            P.dve(lambda e: e.tensor_tensor(out=tm, in0=mk, in1=cb2, op=ALU.mult), r=RB + allr, w=RB)
            P.dve(lambda e: e.tensor_reduce(out=w1, in_=tm3, axis=AX.X, op=ALU.add), r=RB, w=RB)
            P.dve(lambda e: e.tensor_scalar(out=idx_i[:, :, 0], in0=i0, scalar1=-1.0, scalar2=None, op0=ALU.add), r=RB, w=[b_idx])
            P.dve(lambda e: e.tensor_scalar(out=idx_i[:, :, 1], in0=i1, scalar1=-1.0, scalar2=None, op0=ALU.add), r=RB, w=[b_idx])
            P.dve(lambda e: e.tensor_copy(out=wts_sb[:, :, 0], in_=w0), r=RB, w=[b_idx])
            P.dve(lambda e: e.tensor_copy(out=wts_sb[:, :, 1], in_=w1), r=RB, w=[b_idx])
            xsf = [ar.alloc([D], F32) for _ in range(2)]
            xsb = [ar.alloc([D], BF16) for _ in range(2)]
            b_xsf = [Buf(), Buf()]
            b_xsb = [Buf(), Buf()]
            for t in range(NT):
                s = t % 2
                P.dma('sp', lambda e, t=t, s=s: e.dma_start(out=xsf[s], in_=xres_d[t * 128:(t + 1) * 128, :]),
                      r=[b_xres[t]], w=[b_xsf[s]])
                P.act(lambda e, s=s: e.copy(out=xsb[s], in_=xsf[s]), r=[b_xsf[s]], w=[b_xsb[s]])
                for kk in range(2):
                    P.dma('pool', lambda e, t=t, s=s, kk=kk: e.indirect_dma_start(
                        out=xs_d[:, :], out_offset=bass.IndirectOffsetOnAxis(ap=idx_i[:, t, kk:kk + 1], axis=0),
                        in_=xsb[s], in_offset=None), r=[b_xsb[s], b_idx, b_xs_w] + b_xs_z)
            P.op('sp', lambda e: e.nop(), w=[b_xs_w, b_xs])
            P.barrier()

            if l >= 1 and '4' in SKIP1:
                return
            ar.release(base_mark)
            wgs = [ar.alloc([8, DE], BF16) for _ in range(2)]
            wus = [ar.alloc([8, DE], BF16) for _ in range(2)]
            wds = [ar.alloc([4, D], BF16) for _ in range(2)]
            b_wg = [Buf(), Buf()]
            b_wu = [Buf(), Buf()]
            b_wd = [Buf(), Buf()]
            ln2g_sb = ar.alloc([D], F32)
            ln2b_sb = ar.alloc([D], F32)
            b_c4 = [Buf(), Buf()]
            P.dma('sp', lambda e: e.dma_start(out=ln2g_sb, in_=ln2g_d[l]), w=[b_c4[0]])
            P.dma('sp', lambda e: e.dma_start(out=ln2b_sb, in_=ln2b_d[l]), w=[b_c4[1]])
            xst = [ar.alloc([4, D], BF16) for _ in range(2)]
            b_xst = [Buf(), Buf()]
            xTs = [ar.alloc([8, 512], BF16) for _ in range(2)]
            b_xTs = [[Buf() for _ in range(4)] for _ in range(2)]
            sg = [ar.alloc([512], F32) for _ in range(2)]
            b_sg = [Buf(), Buf()]
            hT = [ar.alloc([4, 512], BF16) for _ in range(2)]
            b_hT = [[Buf() for _ in range(4)] for _ in range(2)]
            NYS = 3
            yst = [ar.alloc([D], F32) for _ in range(NYS)]
            b_yst = [Buf() for _ in range(NYS)]
            gu_i = 0
            yb_i = 0
            ys_i = 0
            tp_i = 0
            wg_rows = wgb_d[l].rearrange("e p m -> (e p) m")
            wu_rows = wub_d[l].rearrange("e p m -> (e p) m")
            wd_rows = wdb_d[l].rearrange("e p m -> (e p) m")

            def stageW(j):
                wb = j % 2
                io = bass.IndirectOffsetOnAxis(ap=idxw[:, j:j + 1], axis=0)
                P.dma('pool', lambda e, wb=wb, io=io: e.indirect_dma_start(
                    out=wgs[wb].rearrange("p k n -> p (k n)"), out_offset=None, in_=wg_rows[:, :], in_offset=io),
                    r=[b_ej, b_wc], w=[b_wg[wb]])
                P.dma('pool', lambda e, wb=wb, io=io: e.indirect_dma_start(
                    out=wus[wb].rearrange("p k n -> p (k n)"), out_offset=None, in_=wu_rows[:, :], in_offset=io),
                    r=[b_ej, b_wc], w=[b_wu[wb]])
                P.dma('pool', lambda e, wb=wb, io=io: e.indirect_dma_start(
                    out=wds[wb].rearrange("p k n -> p (k n)"), out_offset=None, in_=wd_rows[:, :], in_offset=io),
                    r=[b_ej, b_wc], w=[b_wd[wb]])

            def stageXload(j):
                wb = j % 2
                P.dma('sp', lambda e, j=j, wb=wb: e.dma_start(
                    out=xst[wb], in_=xs_d[j * 512:(j + 1) * 512, :].rearrange("(a p) d -> p a d", p=128)),
                    r=[b_xs], w=[b_xst[wb]])

            tpc = [0]

            def stageT(j):
                wb = j % 2
                for a in range(4):
                    bk = 4 + (tpc[0] % 2)
                    tpc[0] += 1
                    pb = bank_bf(bk)
                    for k in range(8):
                        P.pe(lambda e, a=a, k=k, wb=wb, pb=pb: e.transpose(out=pb[:, k * 128:(k + 1) * 128],
                                                                         in_=xst[wb][:, a, k * 128:(k + 1) * 128], identity=idb),
                             r=[b_xst[wb], b_idb], w=[bb[bk]])
                    if a % 2 == 0:
                        P.act(lambda e, a=a, wb=wb, pb=pb: e.copy(out=xTs[wb][:, :, a * 128:(a + 1) * 128],
                                                                  in_=pb.rearrange("p (k t) -> p k t", t=128)),
                              r=[bb[bk]], w=[b_xTs[wb][a]])
                    else:
                        P.dve(lambda e, a=a, wb=wb, pb=pb: e.tensor_copy(out=xTs[wb][:, :, a * 128:(a + 1) * 128],
                                                                         in_=pb.rearrange("p (k t) -> p k t", t=128)),
                              r=[bb[bk]], w=[b_xTs[wb][a]])

            guc = [0]

            def stageGU(j):
                wb = j % 2
                hb = j % 2
                for c in range(4):
                    bg = (guc[0] % 2) * 2
                    guc[0] += 1
                    for k in range(8):
                        P.pe(lambda e, k=k, c=c, wb=wb, bg=bg: e.matmul(
                            banks[bg][:, :], lhsT=wgs[wb][:, k, c * 128:(c + 1) * 128],
                            rhs=xTs[wb][:, k, :], start=(k == 0), stop=(k == 7)),
                            r=[b_wg[wb]] + b_xTs[wb], w=[bb[bg]])
                    for k in range(8):
                        P.pe(lambda e, k=k, c=c, wb=wb, bg=bg: e.matmul(
                            banks[bg + 1][:, :], lhsT=wus[wb][:, k, c * 128:(c + 1) * 128],
                            rhs=xTs[wb][:, k, :], start=(k == 0), stop=(k == 7)),
                            r=[b_wu[wb]] + b_xTs[wb], w=[bb[bg + 1]])
                    sgi = guc[0] % 2
                    P.act(lambda e, bg=bg, sgi=sgi: e.activation(out=sg[sgi], in_=banks[bg][:, :], func=AF.Silu),
                          r=[bb[bg]], w=[b_sg[sgi]])
                    P.dve(lambda e, bg=bg, sgi=sgi, hb=hb, c=c: e.tensor_tensor(
                        out=hT[hb][:, c, :], in0=sg[sgi], in1=banks[bg + 1][:, :], op=ALU.mult),
                        r=[b_sg[sgi], bb[bg + 1]], w=[b_hT[hb][c]])

            ybc = [0, 0]

            def stageY(j):
                wb = j % 2
                hb = j % 2
                for a in range(4):
                    ysb = ybc[1] % NYS
                    ybc[1] += 1
                    for nh in range(2):
                        yb = 6 + (ybc[0] % 2)
                        ybc[0] += 1
                        for c in range(4):
                            P.pe(lambda e, c=c, a=a, nh=nh, hb=hb, wb=wb, yb=yb: e.matmul(
                                banks[yb][:, :], lhsT=hT[hb][:, c, a * 128:(a + 1) * 128],
                                rhs=wds[wb][:, c, nh * 512:(nh + 1) * 512], start=(c == 0), stop=(c == 3)),
                                r=[b_hT[hb][c], b_wd[wb]], w=[bb[yb]])
                        if nh == 0:
                            P.act(lambda e, ysb=ysb, yb=yb: e.copy(out=yst[ysb][:, 0:512], in_=banks[yb][:, :]),
                                  r=[bb[yb]], w=[b_yst[ysb]])
                        else:
                            P.dve(lambda e, ysb=ysb, yb=yb: e.tensor_copy(out=yst[ysb][:, 512:1024], in_=banks[yb][:, :]),
                                  r=[bb[yb]], w=[b_yst[ysb]])
                    row0 = (j * 4 + a) * 128
                    P.dma('sp', lambda e, ysb=ysb, row0=row0: e.dma_start(out=ys_d[row0:row0 + 128, :], in_=yst[ysb]),
                          r=[b_yst[ysb], b_ys_w])

            stageW(0)
            stageXload(0)
            stageT(0)
            for j in range(NTILE):
                if j + 1 < NTILE:
                    stageW(j + 1)
                    stageXload(j + 1)
                stageGU(j)
                if j + 1 < NTILE:
                    stageT(j + 1)
                stageY(j)
            P.op('sp', lambda e: e.nop(), w=[b_ys_w, b_ys])
            NB5 = 4
            g0t = [ar.alloc([D], F32) for _ in range(NB5)]
            g1t = [ar.alloc([D], F32) for _ in range(NB5)]
            x4 = [ar.alloc([D], F32) for _ in range(NB5)]
            st4 = [ar.alloc([2, 6], F32) for _ in range(NB5)]
            mv4 = [ar.alloc([2], F32) for _ in range(NB5)]
            o4 = [ar.alloc([D], F32) for _ in range(NB5)]
            B4 = [{k: Buf() for k in ('g0', 'g1', 'x', 'st', 'mv', 'o')} for _ in range(NB5)]
            def p5_load(t):
                s = t % NB5
                B = B4[s]
                P.dma('pool', lambda e, t=t, s=s: e.indirect_dma_start(
                    out=g0t[s], out_offset=None, in_=ys_d[:, :],
                    in_offset=bass.IndirectOffsetOnAxis(ap=idx_i[:, t, 0:1], axis=0)), r=[b_ys, b_idx], w=[B['g0']])
                P.dma('pool', lambda e, t=t, s=s: e.indirect_dma_start(
                    out=g1t[s], out_offset=None, in_=ys_d[:, :],
                    in_offset=bass.IndirectOffsetOnAxis(ap=idx_i[:, t, 1:2], axis=0)), r=[b_ys, b_idx], w=[B['g1']])
                P.dma('sp', lambda e, t=t, s=s: e.dma_start(out=x4[s], in_=xres_d[t * 128:(t + 1) * 128, :]),
                      r=[b_xres[t]], w=[B['x']])

            def p5_compute(t):
                s = t % NB5
                B = B4[s]
                P.act(lambda e, t=t, s=s: e.activation(out=g0t[s], in_=g0t[s], func=AF.Copy, scale=wts_sb[:, t, 0:1]),
                      r=[B['g0'], b_idx], w=[B['g0']])
                P.dve(lambda e, t=t, s=s: e.scalar_tensor_tensor(out=g0t[s], in0=g1t[s], scalar=wts_sb[:, t, 1:2], in1=g0t[s],
                                                                  op0=ALU.mult, op1=ALU.add),
                      r=[B['g0'], B['g1'], b_idx], w=[B['g0']])
                P.dve(lambda e, s=s: e.scalar_tensor_tensor(out=x4[s], in0=x4[s], scalar=ALPHA, in1=g0t[s],
                                                            op0=ALU.mult, op1=ALU.add),
                      r=[B['x'], B['g0']], w=[B['x']])
                ln_tile(P, x4[s], B['x'], st4[s], B['st'], mv4[s], B['mv'], ln2g_sb, b_c4[0], ln2b_sb, b_c4[1],
                        o4[s], B['o'], mul_eng='act_dve')
                if l == depth - 1:
                    P.dma('sp', lambda e, t=t, s=s: e.dma_start(out=out_d[t * 128:(t + 1) * 128, :], in_=o4[s]),
                          r=[B['o'], b_out_done])
                else:
                    P.dma('sp', lambda e, t=t, s=s: e.dma_start(out=xres_d[t * 128:(t + 1) * 128, :], in_=o4[s]),
                          r=[B['o'], B['x']], w=[b_xres[t]])

            LA5 = 2
            for t in range(min(LA5, NT)):
                p5_load(t)
            for t in range(NT):
                if t + LA5 < NT:
                    p5_load(t + LA5)
                p5_compute(t)
            P.barrier()
            if dbg and l == 0 and depth > 1:
                P.dma('sp', lambda e: e.dma_start(out=dbg2_d[:, :], in_=xres_d[:, :]), r=b_xres + [b_out_done])
                P.barrier()
        for l_ in range(depth if NLAYERS_RUN is None else NLAYERS_RUN):
            do_layer(l_)
        P.op('sp', lambda e: e.nop(), w=[b_out_done])
        P.emit(st)
    return nc, P, ar


def ln_stats(P, y, b_y, st, b_st, mv, b_mv):
    for hh in range(2):
        P.dve(lambda e, hh=hh: e.bn_stats(out=st[:, hh, :], in_=y[:, hh * 512:(hh + 1) * 512]), r=[b_y], w=[b_st])
    P.dve(lambda e: e.bn_aggr(out=mv, in_=st.rearrange("p a b -> p (a b)")), r=[b_st], w=[b_mv])


def ln_apply(P, y, b_y, mv, b_mv, g_sb, b_g, b_sb, b_b, out, b_out):
    P.act(lambda e: e.activation(out=mv[:, 1:2], in_=mv[:, 1:2], func=AF.Sqrt, bias=LN_EPS, scale=1.0), r=[b_mv], w=[b_mv])
    P.dve(lambda e: e.reciprocal(out=mv[:, 1:2], in_=mv[:, 1:2]), r=[b_mv], w=[b_mv])
    P.dve(lambda e: e.scalar_tensor_tensor(out=out, in0=y, scalar=mv[:, 0:1], in1=g_sb, op0=ALU.subtract, op1=ALU.mult),
          r=[b_y, b_mv, b_g], w=[b_out])
    P.dve(lambda e: e.scalar_tensor_tensor(out=out, in0=out, scalar=mv[:, 1:2], in1=b_sb, op0=ALU.mult, op1=ALU.add),
          r=[b_out, b_mv, b_b], w=[b_out])


def ln_tile(P, y, b_y, st, b_st, mv, b_mv, g_sb, b_g, b_sb, b_b, out, b_out, mul_eng='pool'):
    ln_stats(P, y, b_y, st, b_st, mv, b_mv)
    ln_apply(P, y, b_y, mv, b_mv, g_sb, b_g, b_sb, b_b, out, b_out)


def router_tile(P, lg_ps, b_ps, br_sb, b_br, rl, b_rl, comb, b_comb, sel, b_sel):
    L = rl[:, 0:20]
    mg = rl[:, 20:21]
    sgm = rl[:, 21:22]
    gmask = rl[:, 22:26]
    m1 = rl[:, 26:30]
    m2 = rl[:, 30:34]
    eq = rl[:, 34:50]
    ex = rl[:, 50:54]
    den = rl[:, 54:58]
    gm1 = rl[:, 58:62]
    le = L[:, 4:20]
    le3 = le.rearrange("p (g e) -> p g e", e=4)
    eq3 = eq.rearrange("p (g e) -> p g e", e=4)
    R = [b_rl]
    P.dve(lambda e: e.tensor_tensor(out=L, in0=lg_ps, in1=br_sb, op=ALU.add), r=[b_ps, b_br], w=R)
    P.dve(lambda e: e.tensor_reduce(out=mg, in_=L[:, 0:4], axis=AX.X, op=ALU.max), r=R, w=R)
    P.dve(lambda e: e.tensor_scalar(out=gm1, in0=L[:, 0:4], scalar1=mg, scalar2=None, op0=ALU.is_ge), r=R, w=R)
    P.dve(lambda e: e.tensor_scalar(out=ex, in0=L[:, 0:4], scalar1=mg, scalar2=None, op0=ALU.subtract), r=R, w=R)
    P.act(lambda e: e.activation(out=ex, in_=ex, func=AF.Exp, accum_out=sgm), r=R, w=R)
    P.dve(lambda e: e.reciprocal(out=sgm, in_=sgm), r=R, w=R)
    P.dve(lambda e: e.tensor_scalar(out=gmask, in0=gm1, scalar1=sgm, scalar2=None, op0=ALU.mult), r=R, w=R)
    P.dve(lambda e: e.tensor_reduce(out=m1, in_=le3, axis=AX.X, op=ALU.max), r=R, w=R)
    m1b = m1.unsqueeze(2).to_broadcast([128, 4, 4])
    P.dve(lambda e: e.tensor_tensor(out=eq3, in0=le3, in1=m1b, op=ALU.is_ge), r=R, w=R)
    P.dve(lambda e: e.scalar_tensor_tensor(out=eq, in0=eq, scalar=-1e9, in1=le, op0=ALU.mult, op1=ALU.add), r=R, w=R)
    P.dve(lambda e: e.tensor_reduce(out=m2, in_=eq3, axis=AX.X, op=ALU.max), r=R, w=R)
    m2b = m2.unsqueeze(2).to_broadcast([128, 4, 4])
    P.dve(lambda e: e.tensor_tensor(out=eq3, in0=le3, in1=m2b, op=ALU.is_ge), r=R, w=R)
    gm1b = gm1.unsqueeze(2).to_broadcast([128, 4, 4])
    P.dve(lambda e: e.tensor_tensor(out=sel.rearrange("p (g e) -> p g e", e=4), in0=eq3, in1=gm1b, op=ALU.mult),
          r=R, w=[b_sel])
    P.dve(lambda e: e.tensor_tensor(out=le3, in0=le3, in1=m1b, op=ALU.subtract), r=R, w=R)
    P.act(lambda e: e.activation(out=le, in_=le, func=AF.Exp), r=R, w=R)
    P.dve(lambda e: e.tensor_tensor(out=le, in0=le, in1=eq, op=ALU.mult), r=R, w=R)
    P.dve(lambda e: e.tensor_reduce(out=den, in_=le3, axis=AX.X, op=ALU.add), r=R, w=R)
    P.dve(lambda e: e.reciprocal(out=den, in_=den), r=R, w=R)
    P.dve(lambda e: e.tensor_tensor(out=den, in0=den, in1=gmask, op=ALU.mult), r=R, w=R)
    denb = den.unsqueeze(2).to_broadcast([128, 4, 4])
    P.dve(lambda e: e.tensor_tensor(out=comb.rearrange("p (g e) -> p g e", e=4), in0=le3, in1=denb, op=ALU.mult),
          r=R, w=[b_comb])


def host_consts(S):
    NT = S // 128
    pos = np.arange(S, dtype=np.float32)
    half = 8
    inv_freq = (np.float32(500000.0) ** (-np.arange(half, dtype=np.float32) * np.float32(2.0) / np.float32(16))).astype(np.float32)
    ang = pos[:, None] * inv_freq[None, :]
    cos = np.cos(ang).astype(np.float32)
    sin = np.sin(ang).astype(np.float32)

    def tab(a):
        return np.ascontiguousarray(a.reshape(NT, 128, 8).transpose(1, 0, 2))
    n = np.arange(16)[None, :]
    bi_t = (np.arange(NT) // 2)[:, None]
    gcaus = np.where(n < bi_t, 0.0, -1e30).astype(np.float32)
    gown = np.where(n == bi_t, 0.0, NEG).astype(np.float32)
    gcaus = np.ascontiguousarray(np.broadcast_to(gcaus[None], (128, NT, 16)))
    gown = np.ascontiguousarray(np.broadcast_to(gown[None], (128, NT, 16)))
    kk = np.arange(128)[:, None]
    qq = np.arange(128)[None, :]
    tri = np.where(kk <= qq, 0.0, NEG).astype(np.float32)
    khot = (np.arange(S)[None, :] // 256 == np.arange(16)[:, None]).astype(np.float32)
    ltri = (np.arange(128)[:, None] < np.arange(128)[None, :]).astype(np.float32)
    jstart = np.ascontiguousarray(np.broadcast_to((512.0 * np.arange(32, dtype=np.float32))[None, :, None], (128, 32, 16)))
    wbase = (np.arange(128, dtype=np.float32)[:, None] + 128.0 * np.arange(8, dtype=np.float32)[None, :]).astype(np.float32)
    shift = np.zeros((128, 128), np.float32)
    shift[np.arange(64), np.arange(64) + 64] = 1.0
    return {"c_ident": np.eye(128, dtype=np.float32), "c_cos": tab(cos), "c_sin": tab(sin), "c_gcaus": gcaus,
            "c_gown": gown, "c_tri": tri, "c_khot": khot, "c_shift": shift, "c_ltri": ltri, "c_jstart": jstart, "c_wbase": wbase}


def host_params(inp, depth):
    def f(a):
        return np.ascontiguousarray(np.asarray(a, dtype=np.float32))

    def chunkvec(v):
        return f(np.asarray(v)[:depth].reshape(depth, 4, 128).transpose(0, 2, 1))

    def bcast(v):
        v = np.asarray(v)[:depth]
        return f(np.broadcast_to(v[:, None, :], (depth, 128, v.shape[-1])))

    def blockdiag(w):
        w = np.asarray(w)[:depth]
        o = np.zeros((depth, 4, 128, 128), np.float32)
        for c in range(4):
            o[:, c, 0:64, 0:64] = w[:, 2 * c]
            o[:, c, 64:128, 64:128] = w[:, 2 * c + 1]
        return o
    p = {}
    p["w_in"] = f(inp["w_in"][:depth])
    p["conv_w"] = f(np.asarray(inp["conv_w"])[:depth].reshape(depth, 4, 4, 128).transpose(0, 3, 2, 1))
    p["conv_b"] = chunkvec(inp["conv_b"])
    p["w_rg_a"] = blockdiag(inp["w_rg_a"])
    p["b_rg_a"] = chunkvec(inp["b_rg_a"])
    p["w_rg_i"] = blockdiag(inp["w_rg_i"])
    p["b_rg_i"] = chunkvec(inp["b_rg_i"])
    p["lru_lambda"] = chunkvec(inp["lru_lambda"])
    p["g_attn_norm"] = bcast(inp["g_attn_norm"])
    p["g_rec_norm"] = chunkvec(inp["g_rec_norm"])
    p["w_out"] = f(inp["w_out"][:depth])
    p["ln1_g"] = bcast(inp["ln1_g"])
    p["ln1_b"] = bcast(inp["ln1_b"])
    p["w_router"] = f(np.concatenate([np.asarray(inp["w_router_group"])[:depth], np.asarray(inp["w_router_expert"])[:depth]], axis=-1))
    p["b_router"] = bcast(np.concatenate([np.asarray(inp["b_router_group"])[:depth], np.asarray(inp["b_router_expert"])[:depth]], axis=-1))
    p["w_gate"] = f(inp["w_gate"][:depth])
    p["w_up"] = f(inp["w_up"][:depth])
    p["w_down"] = f(inp["w_down"][:depth])
    p["ln2_g"] = bcast(inp["ln2_g"])
    p["ln2_b"] = bcast(inp["ln2_b"])
    return p


_CACHE = {}


def kernel(**inputs):
    x = np.asarray(inputs["x"], dtype=np.float32)
    B, S, _ = x.shape
    depth = int(np.asarray(inputs["w_in"]).shape[0])
    key = (S, depth)
    if key not in _CACHE:
        _CACHE[key] = build(S, depth)[0]
    nc = _CACHE[key]
    shared = host_params(inputs, depth)
    shared.update(host_consts(S))
    in_maps = []
    for b in range(B):
        m = dict(shared)
        m["x"] = np.ascontiguousarray(x[b])
        in_maps.append(m)
    res = run_bass_kernel_spmd(nc, in_maps, core_ids=list(range(B)))
    return np.stack([np.asarray(r["out"], dtype=np.float32) for r in res.results], axis=0)
```
